# Optimizing a Trainium2 kernel written in Bass

```python
import math
import jax, jax.numpy as jnp
from jax import lax
import numpy as np


D_MODEL = 1024
BATCH = 2
SEQ = 16384
DEPTH = 2

GRID_W = 64
CTX_LEN = 256
D_MIX = D_MODEL
D_POOL = D_MIX // 2
POOL_WINDOWS = (2, 4, 8, 16)
N_POOL_GROUPS = len(POOL_WINDOWS)
POOL_GC = D_POOL // N_POOL_GROUPS
D_ATTN = D_MIX - D_POOL
ATTN_HEAD_DIM = 64
N_ATTN_HEADS = D_ATTN // (2 * ATTN_HEAD_DIM)
ROPE_THETA = 10000.0
Q_BLOCK = 128
Q_OFF = D_POOL
K_OFF = Q_OFF + D_ATTN
V_OFF = K_OFF + D_ATTN
D_IN = V_OFF + D_ATTN
N_GROUPS = 4
EXPERTS_PER_GROUP = 8
N_EXPERTS = N_GROUPS * EXPERTS_PER_GROUP
TOP_K_FINE = 2
D_EXPERT = 256
EPS = 1e-6

kernel_name = 'hybrid_pool_diffattn_hmoe_dit'


def rmsnorm(x, g):
    xf = x.astype(jnp.float32)
    y = xf * lax.rsqrt(jnp.mean(xf * xf, axis=-1, keepdims=True) + EPS)
    return (y * g.astype(jnp.float32)).astype(x.dtype)


def modulate(h, shift, scale):
    return h * (1 + scale) + shift


def axial_rope_tables(n_tokens, dtype):
    rows = n_tokens // GRID_W
    row = jnp.repeat(jnp.arange(rows, dtype=jnp.int32), GRID_W)
    col = jnp.tile(jnp.arange(GRID_W, dtype=jnp.int32), rows)
    n_freq = ATTN_HEAD_DIM // 4
    inv = ROPE_THETA ** (-jnp.arange(n_freq, dtype=jnp.float32) / n_freq)
    ang_r = (row[:, None].astype(jnp.float32) * inv)[:, None, None, :]
    ang_c = (col[:, None].astype(jnp.float32) * inv)[:, None, None, :]
    return (jnp.cos(ang_r).astype(dtype), jnp.sin(ang_r).astype(dtype),
            jnp.cos(ang_c).astype(dtype), jnp.sin(ang_c).astype(dtype))


def _rope_half(x, cos, sin):
    x1, x2 = jnp.split(x, 2, axis=-1)
    return jnp.concatenate([x1 * cos - x2 * sin, x2 * cos + x1 * sin], axis=-1)


def apply_axial_rope(x, tables):
    cos_r, sin_r, cos_c, sin_c = tables
    half = ATTN_HEAD_DIM // 2
    return jnp.concatenate([_rope_half(x[..., :half], cos_r, sin_r),
                            _rope_half(x[..., half:], cos_c, sin_c)], axis=-1)


def heads_qk(p):
    return p.reshape(*p.shape[:-1], N_ATTN_HEADS, 2, ATTN_HEAD_DIM)


def heads_v(p):
    return p.reshape(*p.shape[:-1], N_ATTN_HEADS, 2 * ATTN_HEAD_DIM)


def pool_mixer(u, pool_w, pool_scale):
    b, n, _ = u.shape
    uf = u.reshape(b, n, N_POOL_GROUPS, POOL_GC).astype(jnp.float32)
    csum = jnp.concatenate([jnp.zeros_like(uf[:, :1]), jnp.cumsum(uf, axis=1)], axis=1)
    t = jnp.arange(n)
    outs = []
    for g, w in enumerate(POOL_WINDOWS):
        left = w // 2
        right = w - 1 - left
        lo = jnp.clip(t - left, 0, n)
        hi = jnp.clip(t + right + 1, 0, n)
        cnt = (hi - lo).astype(jnp.float32)[None, :, None]
        outs.append((csum[:, hi, g] - csum[:, lo, g]) / cnt - uf[:, :, g])
    pooled = jnp.stack(outs, axis=2).astype(u.dtype)
    mixed = jnp.einsum('blgc,gce->blge', pooled, pool_w)
    return mixed.reshape(b, n, D_POOL) * pool_scale


def diff_attend(q, k, v, lam):
    s = jnp.einsum('bqhmd,bkhmd->bhmqk', q, k).astype(jnp.float32) * (ATTN_HEAD_DIM ** -0.5)
    p = jax.nn.softmax(s, axis=-1)
    a = p[:, :, 0] - lam * p[:, :, 1]
    return jnp.einsum('bhqk,bkhe->bqhe', a.astype(v.dtype), v)


def diff_attention_latent(q, k, v, k_ctx, v_ctx, lam):
    keys = jnp.concatenate([k, k_ctx], axis=1)
    vals = jnp.concatenate([v, v_ctx], axis=1)
    b, n = q.shape[:2]
    nb = n // Q_BLOCK
    qb = q.reshape(b, nb, Q_BLOCK, N_ATTN_HEADS, 2, ATTN_HEAD_DIM).transpose(1, 0, 2, 3, 4, 5)
    out = lax.map(lambda qblk: diff_attend(qblk, keys, vals, lam), qb)
    return out.transpose(1, 0, 2, 3, 4).reshape(b, n, N_ATTN_HEADS, 2 * ATTN_HEAD_DIM)


def merge_head_groups(u, attn, pool_w, pool_scale, subln_g, lam_init, w_out):
    pool = pool_mixer(u, pool_w, pool_scale)
    attn = rmsnorm(attn, subln_g) * (1 - lam_init)
    attn = attn.reshape(*attn.shape[:2], D_ATTN)
    return jnp.concatenate([pool, attn], axis=-1) @ w_out


def hier_moe(h, rc_w, rc_b, rf_w, rf_b, w_gate, w_up, w_down):
    shp = h.shape
    t = h.reshape(-1, D_MODEL)
    n = t.shape[0]
    logits_c = (t @ rc_w + rc_b).astype(jnp.float32)
    probs_c = jax.nn.softmax(logits_c, axis=-1)
    _, top_g = lax.top_k(logits_c, 1)
    g_idx = top_g[:, 0]
    p_group = jnp.take_along_axis(probs_c, top_g, axis=-1)
    logits_f = (t @ rf_w + rf_b).astype(jnp.float32).reshape(n, N_GROUPS, EXPERTS_PER_GROUP)
    lf = logits_f[jnp.arange(n), g_idx]
    top_v, top_i = lax.top_k(lf, TOP_K_FINE)
    w_top = jax.nn.softmax(top_v, axis=-1) * p_group
    expert_id = g_idx[:, None] * EXPERTS_PER_GROUP + top_i
    gates = jnp.einsum('nk,nke->ne', w_top, jax.nn.one_hot(expert_id, N_EXPERTS, dtype=jnp.float32))
    gates = gates.reshape(n, N_GROUPS, EXPERTS_PER_GROUP).astype(t.dtype)
    out = jnp.zeros_like(t)
    for g in range(N_GROUPS):
        a = jnp.einsum('nd,edf->nef', t, w_gate[g])
        bu = jnp.einsum('nd,edf->nef', t, w_up[g])
        hid = jax.nn.silu(a) * bu * gates[:, g, :, None]
        out = out + jnp.einsum('nef,efd->nd', hid, w_down[g])
    return out.reshape(shp)


def setup_inputs(seed: int = 0) -> dict:
    key = jax.random.key(seed)
    ks = jax.random.split(key, 26)
    f32 = jnp.float32
    nrm = lambda k, s: jax.random.normal(k, s, f32)
    D = D_MODEL
    return {
        'x': nrm(ks[0], (BATCH, SEQ, D)),
        'c': nrm(ks[1], (BATCH, D)),
        'ctx': nrm(ks[2], (BATCH, CTX_LEN, D)),
        'c_ctx': nrm(ks[3], (D,)),
        'ada_w': nrm(ks[4], (DEPTH, D, 6 * D)) * (0.5 * D ** -0.5),
        'ada_b': nrm(ks[5], (DEPTH, 6 * D)) * 0.01,
        'norm1_g': 1.0 + 0.05 * nrm(ks[6], (DEPTH, D)),
        'norm2_g': 1.0 + 0.05 * nrm(ks[7], (DEPTH, D)),
        'w_in': nrm(ks[8], (DEPTH, D, D_IN)) * D ** -0.5,
        'pool_w': nrm(ks[9], (DEPTH, N_POOL_GROUPS, POOL_GC, POOL_GC)) * POOL_GC ** -0.5,
        'pool_scale': 1.0 + 0.1 * nrm(ks[10], (DEPTH, D_POOL)),
        'lambda_q1': 0.1 * nrm(ks[11], (DEPTH, ATTN_HEAD_DIM)),
        'lambda_k1': 0.1 * nrm(ks[12], (DEPTH, ATTN_HEAD_DIM)),
        'lambda_q2': 0.1 * nrm(ks[13], (DEPTH, ATTN_HEAD_DIM)),
        'lambda_k2': 0.1 * nrm(ks[14], (DEPTH, ATTN_HEAD_DIM)),
        'subln_g': 1.0 + 0.05 * nrm(ks[15], (DEPTH, 2 * ATTN_HEAD_DIM)),
        'w_out': nrm(ks[16], (DEPTH, D_MIX, D)) * D_MIX ** -0.5,
        'router_coarse_w': nrm(ks[17], (DEPTH, D, N_GROUPS)) * D ** -0.5,
        'router_coarse_b': 0.01 * nrm(ks[18], (DEPTH, N_GROUPS)),
        'router_fine_w': nrm(ks[19], (DEPTH, D, N_EXPERTS)) * D ** -0.5,
        'router_fine_b': 0.01 * nrm(ks[20], (DEPTH, N_EXPERTS)),
        'w_gate': nrm(ks[21], (DEPTH, N_GROUPS, EXPERTS_PER_GROUP, D, D_EXPERT)) * D ** -0.5,
        'w_up': nrm(ks[22], (DEPTH, N_GROUPS, EXPERTS_PER_GROUP, D, D_EXPERT)) * D ** -0.5,
        'w_down': nrm(ks[23], (DEPTH, N_GROUPS, EXPERTS_PER_GROUP, D_EXPERT, D)) * D_EXPERT ** -0.5,
        'final_g': 1.0 + 0.05 * nrm(ks[24], (D,)),
    }


def reference(x, c, ctx, c_ctx, ada_w, ada_b, norm1_g, norm2_g, w_in, pool_w, pool_scale,
              lambda_q1, lambda_k1, lambda_q2, lambda_k2, subln_g, w_out,
              router_coarse_w, router_coarse_b, router_fine_w, router_fine_b,
              w_gate, w_up, w_down, final_g):
    n_lat = x.shape[1]
    tables = axial_rope_tables(n_lat, x.dtype)
    xc = ctx
    s_x = jax.nn.silu(c)
    s_c = jax.nn.silu(c_ctx)
    for layer in range(DEPTH):
        last = layer == DEPTH - 1
        lam_init = 0.8 - 0.6 * math.exp(-0.3 * layer)
        mod_x = (s_x @ ada_w[layer] + ada_b[layer])[:, None, :]
        mod_c = (s_c @ ada_w[layer] + ada_b[layer])[None, None, :]
        sh1, sc1, g1, sh2, sc2, g2 = jnp.split(mod_x, 6, axis=-1)
        csh1, csc1, cg1, csh2, csc2, cg2 = jnp.split(mod_c, 6, axis=-1)
        lam = (jnp.exp(jnp.sum(lambda_q1[layer] * lambda_k1[layer]).astype(jnp.float32))
               - jnp.exp(jnp.sum(lambda_q2[layer] * lambda_k2[layer]).astype(jnp.float32))
               + lam_init)

        hx = modulate(rmsnorm(x, norm1_g[layer]), sh1, sc1)
        hc = modulate(rmsnorm(xc, norm1_g[layer]), csh1, csc1)
        px = hx @ w_in[layer]
        u_x = px[..., :Q_OFF]
        q_x = apply_axial_rope(heads_qk(px[..., Q_OFF:K_OFF]), tables)
        k_x = apply_axial_rope(heads_qk(px[..., K_OFF:V_OFF]), tables)
        v_x = heads_v(px[..., V_OFF:])
        col0 = K_OFF if last else 0
        pc = hc @ w_in[layer][:, col0:]
        k_c = heads_qk(pc[..., K_OFF - col0:V_OFF - col0])
        v_c = heads_v(pc[..., V_OFF - col0:])
        attn_x = diff_attention_latent(q_x, k_x, v_x, k_c, v_c, lam)
        mix_x = merge_head_groups(u_x, attn_x, pool_w[layer], pool_scale[layer], subln_g[layer],
                                  lam_init, w_out[layer])
        if not last:
            q_c = heads_qk(pc[..., Q_OFF:K_OFF])
            attn_c = diff_attend(q_c, k_c, v_c, lam)
            mix_c = merge_head_groups(pc[..., :Q_OFF], attn_c, pool_w[layer], pool_scale[layer],
                                      subln_g[layer], lam_init, w_out[layer])
            xc = xc + cg1 * mix_c
        x = x + g1 * mix_x

        hx2 = modulate(rmsnorm(x, norm2_g[layer]), sh2, sc2)
        moe_args = (router_coarse_w[layer], router_coarse_b[layer], router_fine_w[layer],
                    router_fine_b[layer], w_gate[layer], w_up[layer], w_down[layer])
        if last:
            x = x + g2 * hier_moe(hx2, *moe_args)
        else:
            hc2 = modulate(rmsnorm(xc, norm2_g[layer]), csh2, csc2)
            n_ctx = xc.shape[1]
            f = hier_moe(jnp.concatenate([hc2, hx2], axis=1), *moe_args)
            xc = xc + cg2 * f[:, :n_ctx]
            x = x + g2 * f[:, n_ctx:]
    return rmsnorm(x, final_g)
```

```python
import math
from contextlib import ExitStack
import numpy as np
import ml_dtypes
import concourse.bass as bass
import concourse.mybir as mybir
from concourse.bass_utils import run_bass_kernel_spmd

F32 = mybir.dt.float32
BF16 = mybir.dt.bfloat16
AF = mybir.ActivationFunctionType
ALU = mybir.AluOpType

NCORES = 8
D = 1024
L = 16384
NLAT = 4096
NCTX = 256
NT = NLAT + NCTX
VW = 132
EPS = 1e-6
ST = [(s * 512, 512) for s in range(8)] + [(NLAT, NCTX)]
ENGS = ['sp', 'pe', 'act', 'dve', 'pool']


class Buf:
    def __init__(self, name, t=None):
        self.name = name
        self.t = t
        self.w = None
        self.r = {}
        self.dkey = None
        self.small = False

    def __getitem__(self, idx):
        return self.t[idx]


class Sched:
    def __init__(self, nc, stack):
        self.nc = nc
        self.stack = stack
        self.ops = {e: [] for e in ENGS}
        self.sem = {}
        self.cnt = {}
        self.isdma = {}
        self.waited = {e: {} for e in ENGS}
        self.nsem = 0
        self.free_d = []
        for e in ['pe', 'act', 'dve', 'pool']:
            self.newsem(e, False)

    def newsem(self, key, isdma):
        self.nsem += 1
        self.sem[key] = self.stack.enter_context(self.nc.semaphore('s%d' % self.nsem))
        self.cnt[key] = 0
        self.isdma[key] = isdma

    def _deps(self, reads, writes):
        deps = {}

        def add(k, v):
            if deps.get(k, 0) < v:
                deps[k] = v
        for b in reads:
            if b.w is not None:
                add(*b.w)
        for b in writes:
            if b.w is not None:
                add(*b.w)
            for k, v in b.r.items():
                add(k, v)
        return deps

    def _wait(self, eng, deps, strict=()):
        own = 0
        for b in strict:
            if b.w is not None and b.w[0] == eng:
                own = max(own, b.w[1])
        for k, v in deps.items():
            if k == eng:
                if own == 0:
                    continue
                v = own
            if self.isdma[k]:
                v = self.cnt[k]
            if self.waited[eng].get(k, 0) >= v:
                continue
            self.waited[eng][k] = v
            sem = self.sem[k]
            self.ops[eng].append(lambda e, sem=sem, v=v: e.wait_ge(sem, v))

    def _commit(self, key, v, reads, writes):
        for b in writes:
            b.w = (key, v)
            b.r = {}
        for b in reads:
            if b.r.get(key, 0) < v:
                b.r[key] = v

    def op(self, eng, fns, reads=(), writes=(), strict=()):
        if not isinstance(fns, (list, tuple)):
            fns = [fns]
        strict = list(strict) + [b for b in reads if b.small]
        self._wait(eng, self._deps(list(reads) + list(strict), writes), strict)
        self.cnt[eng] += 1
        v = self.cnt[eng]
        sem = self.sem[eng]
        for f in fns[:-1]:
            self.ops[eng].append(f)
        last = fns[-1]
        self.ops[eng].append(lambda e, last=last, sem=sem: last(e).then_inc(sem, 1))
        self._commit(eng, v, reads, writes)

    def dma(self, q, out, in_, reads=(), writes=(), sb=None, **kw):
        if sb.dkey is None:
            if self.free_d:
                sb.dkey = self.free_d.pop()
            else:
                sb.dkey = ('d', len(self.sem))
                self.newsem(sb.dkey, True)
        key = sb.dkey
        self._wait(q, self._deps(reads, writes))
        self.cnt[key] += 16
        v = self.cnt[key]
        sem = self.sem[key]
        self.ops[q].append(lambda e: e.dma_start(out=out, in_=in_, **kw).then_inc(sem, 16))
        self._commit(key, v, reads, writes)

    def cc(self, ins_ap, outs_ap, reads=(), writes=()):
        key = 'cc'
        if key not in self.sem:
            self.newsem(key, True)
        self._wait('pool', self._deps(reads, writes))
        self.cnt[key] += 1
        v = self.cnt[key]
        sem = self.sem[key]
        self.ops['pool'].append(lambda e: e.collective_compute(
            "AllGather", ALU.bypass, replica_groups=[[0, 1, 2, 3], [4, 5, 6, 7]],
            ins=[ins_ap], outs=[outs_ap]).then_inc(sem, 1))
        self._commit(key, v, reads, writes)

    def release(self, bufs):
        for b in bufs:
            if b.dkey is not None:
                self.free_d.append(b.dkey)
                b.dkey = None

    def barrier(self, engines=ENGS):
        for e in engines:
            for k in self.sem:
                if k == e or self.cnt[k] == 0:
                    continue
                v = self.cnt[k]
                if self.waited[e].get(k, 0) >= v:
                    continue
                self.waited[e][k] = v
                sem = self.sem[k]
                self.ops[e].append(lambda en, sem=sem, v=v: en.wait_ge(sem, v))

    def replay(self):
        nc = self.nc
        with nc.Block() as block:
            @block.sync
            def _(e):
                for f in self.ops['sp']:
                    f(e)

            @block.tensor
            def _(e):
                for f in self.ops['pe']:
                    f(e)

            @block.scalar
            def _(e):
                for f in self.ops['act']:
                    f(e)

            @block.vector
            def _(e):
                for f in self.ops['dve']:
                    f(e)

            @block.gpsimd
            def _(e):
                for f in self.ops['pool']:
                    f(e)


class Ctx:
    pass


def dbuf(C, name):
    if name not in C.db:
        C.db[name] = Buf(name)
    return C.db[name]


def sb(C, stack, name, shape, dt):
    if not hasattr(C, 'names'):
        C.names = {}
    k = C.names.get(name, 0)
    C.names[name] = k + 1
    if k:
        name = '%s_r%d' % (name, k)
    t = stack.enter_context(C.nc.sbuf_tensor(name, shape, dt))
    b = Buf(name, t)
    if getattr(C, 'phase_bufs', None) is not None:
        C.phase_bufs.append(b)
    fs = 1
    for d_ in shape[1:]:
        fs *= d_
    b.small = fs < 256
    return b


def ps(C, stack, name, shape, dt):
    t = stack.enter_context(C.nc.psum_tensor(name, shape, dt))
    return Buf(name, t)


def mm(C, out_buf, out_ap, pairs, reads, start=True, stop=True, skip=False):
    fns = []
    n = len(pairs)
    for i, (l_ap, r_ap) in enumerate(pairs):
        st = start and i == 0
        sp_ = stop and i == n - 1
        fns.append(lambda e, l_ap=l_ap, r_ap=r_ap, st=st, sp_=sp_: e.matmul(
            out_ap, l_ap, r_ap, start=st, stop=sp_, skip_group_check=skip))
    C.S.op('pe', fns, reads=reads, writes=[out_buf])


def tr(C, out_buf, out_ap, in_buf, in_ap, ident_ap, extra_reads=()):
    C.S.op('pe', lambda e: e.transpose(out_ap, in_ap, ident_ap),
           reads=[in_buf, C.ident_b] + list(extra_reads), writes=[out_buf])


def act(C, out_ap, in_ap, func, reads, writes, scale=None, bias=None, accum=None, strict=()):
    kw = {}
    if scale is not None:
        kw['scale'] = scale
    if bias is not None:
        kw['bias'] = bias
    if accum is not None:
        kw['accum_out'] = accum
    C.S.op('act', lambda e: e.activation(out_ap, in_ap, func, **kw), reads=reads, writes=writes, strict=strict)


def tsc(C, eng, out_ap, in_ap, s1, s2, op0, op1, reads, writes, accum=None, strict=()):
    if op1 is None:
        C.S.op(eng, lambda e: e.tensor_scalar(out_ap, in_ap, s1, None, op0), reads=reads, writes=writes, strict=strict)
    else:
        C.S.op(eng, lambda e: e.tensor_scalar(out_ap, in_ap, s1, s2, op0, op1), reads=reads, writes=writes, strict=strict)


def stt(C, out_ap, in0, scalar, in1, op0, op1, reads, writes, accum=None, strict=()):
    if accum is None:
        C.S.op('dve', lambda e: e.scalar_tensor_tensor(out_ap, in0, scalar, in1, op0, op1),
               reads=reads, writes=writes, strict=strict)
    else:
        C.S.op('dve', lambda e: e.scalar_tensor_tensor(out_ap, in0, scalar, in1, op0, op1, accum_out=accum),
               reads=reads, writes=writes, strict=strict)


def tt(C, eng, out_ap, in0, in1, op, reads, writes):
    C.S.op(eng, lambda e: e.tensor_tensor(out_ap, in0, in1, op), reads=reads, writes=writes)


def cp(C, eng, out_ap, in_ap, reads, writes):
    if eng == 'act':
        C.S.op('act', lambda e: e.activation(out_ap, in_ap, AF.Copy), reads=reads, writes=writes)
    else:
        C.S.op(eng, lambda e: e.tensor_copy(out_ap, in_ap), reads=reads, writes=writes)


def setup_common(C, stack):
    nc, S = C.nc, C.S
    C.ident_f = sb(C, stack, 'ident_f', [128, 128], F32)
    C.ident_b = sb(C, stack, 'ident_b', [128, 128], BF16)
    C.eps_t = sb(C, stack, 'eps_t', [128, 1], F32)
    S.dma('sp', C.ident_f[:], C.dr['ident'][:, :], reads=[], writes=[C.ident_f], sb=C.ident_f)
    cp(C, 'dve', C.ident_b[:], C.ident_f[:], [C.ident_f], [C.ident_b])
    S.op('dve', lambda e: e.memset(C.eps_t[:], EPS), writes=[C.eps_t])
    C.pb2 = [stack.enter_context(nc.psum_tensor('pb%d' % i, [128, 1024], F32)) for i in range(4)]
    C.pb = []
    for i in range(4):
        for hh in range(2):
            C.pb.append(Buf('pbank%d' % (2 * i + hh), C.pb2[i][:, hh * 512:(hh + 1) * 512]))


def load_modT(C, dst, col, lyr, which, vec, q='sp'):
    src = C.dr['modrow'][lyr, which, vec * 1024:(vec + 1) * 1024].rearrange('(kc p) -> p kc', p=128)
    C.S.dma(q, dst[:, col:col + 8], src, reads=[dbuf(C, 'modrow')], writes=[dst], sb=dst,
            allow_slow_non_contiguous=True)


def load_bc(C, dst_ap, dst_buf, src_row_ap, reads=(), q='sp'):
    C.S.dma(q, dst_ap, src_row_ap.partition_broadcast(128), reads=list(reads), writes=[dst_buf], sb=dst_buf)


def rms_rstd(C, xt, rstd, ss, junk, n=1024, dim=1024):
    stt(C, junk, xt[0], 1.0, xt[0], ALU.mult, ALU.mult, reads=[xt[1]], writes=[ss, C.junk_b], accum=ss[:, 0:1])
    act(C, ss[:, 1:2], ss[:, 0:1], AF.Sqrt, [ss], [ss], scale=1.0 / dim, bias=C.eps_t[:, 0:1], strict=[C.eps_t])
    C.S.op('dve', lambda e: e.reciprocal(rstd[:, 0:1], ss[:, 1:2]), reads=[ss], writes=[rstd])


def norm_mod_T(C, xts, ntile, hxT, gsc, gcol, sh, scol, tpb):
    for i in range(ntile):
        xt = xts[i]
        ss = C.ss[i % 2]
        rstd = C.rstd[i % 2]
        xn = C.xn[i % 2]
        rms_rstd(C, (xt[:, :], xt), rstd, ss, C.junk_b[:, :])
        tsc(C, 'dve', xn[:, :], xt[:, :], rstd[:, 0:1], None, ALU.mult, None, [xt], [xn], strict=[rstd])
        tp = tpb[i % len(tpb)]
        tpv = tp[:, :].bitcast(BF16).rearrange('p (k t) -> p k t', k=8)
        for kc in range(8):
            tr(C, tp, tpv[:, kc, :], xn, xn[:, kc * 128:(kc + 1) * 128], C.ident_b[:])
        for kc in range(8):
            o = hxT[:, kc, i * 128:(i + 1) * 128]
            if kc % 2 == 0:
                act(C, o, tpv[:, kc, :], AF.Identity, [tp], [hxT], strict=[gsc, sh],
                    scale=gsc[:, gcol + kc:gcol + kc + 1], bias=sh[:, scol + kc:scol + kc + 1])
            else:
                tsc(C, 'dve', o, tpv[:, kc, :], gsc[:, gcol + kc:gcol + kc + 1], sh[:, scol + kc:scol + kc + 1],
                    ALU.mult, ALU.add, [tp], [hxT], strict=[gsc, sh])


def load_weight_bf16(C, dst, dst_ap_fn, src_ap_fn, nchunk, stage_bufs, q='sp', cast_eng='pool'):
    for i in range(nchunk):
        stg = stage_bufs[i % len(stage_bufs)]
        src = src_ap_fn(i)
        C.S.dma(q, stg[0](i), src, reads=[], writes=[stg[1]], sb=stg[1])
        cp(C, cast_eng, dst_ap_fn(i), stg[0](i), [stg[1]], [dst])


def mod_vectors(C, lyr, which, stack, pre):
    M = Ctx()
    M.mT = sb(C, stack, pre + 'mT', [128, 48], F32)
    for v in (0, 1, 3, 4):
        load_modT(C, M.mT, v * 8, lyr, which, v)
    M.gsc = sb(C, stack, pre + 'gsc', [128, 16], F32)
    stt(C, M.gsc[:, 0:8], M.mT[:, 8:16], 1.0, C.n1gT[:, lyr * 8:(lyr + 1) * 8], ALU.add, ALU.mult,
        [M.mT, C.n1gT], [M.gsc])
    stt(C, M.gsc[:, 8:16], M.mT[:, 32:40], 1.0, C.n2gT[:, lyr * 8:(lyr + 1) * 8], ALU.add, ALU.mult,
        [M.mT, C.n2gT], [M.gsc])
    return M


def prologue(C):
    S = C.S
    with ExitStack() as stack:
        sT = sb(C, stack, 'sT', [128, 16], F32)
        S.dma('sp', sT[:, :], C.dr['sT_in'][:, :], writes=[sT], sb=sT)
        act(C, sT[:, :], sT[:, :], AF.Silu, [sT], [sT])
        sTv = sT[:, :].rearrange('p (k w) -> p k w', w=2)
        wblk = [sb(C, stack, 'adaw%d' % i, [128, 8, 512], F32) for i in range(2)]
        brow = sb(C, stack, 'brow', [2, 6144], F32)
        mrow = sb(C, stack, 'mrow', [2, 6144], F32)
        n = 0
        for lyr in range(2):
            for w in range(2):
                S.dma('sp', brow[w:w + 1, :], C.dr['ada_b'][lyr:lyr + 1, :], writes=[brow], sb=brow)
            for cb in range(12):
                wb = wblk[n % 2]
                src = C.dr['ada_w'][lyr, :, cb * 512:(cb + 1) * 512].rearrange('(kc p) c -> p kc c', p=128)
                S.dma('sp' if n % 2 == 0 else 'pool', wb[:, :, :], src, writes=[wb], sb=wb)
                pb = C.pb[n % 2]
                mm(C, pb, pb[0:2, :], [(sTv[:, kc, :], wb[:, kc, :]) for kc in range(8)], [sT, wb])
                tt(C, 'dve', mrow[:, cb * 512:(cb + 1) * 512], pb[0:2, :], brow[:, cb * 512:(cb + 1) * 512],
                   ALU.add, [pb, brow], [mrow])
                n += 1
            S.dma('pool', C.dr['modrow'][lyr, :, :], mrow[:, :], reads=[mrow], writes=[dbuf(C, 'modrow')], sb=mrow)
        S.barrier()
        S.release(C.phase_bufs)
        C.phase_bufs = []


def phase_a(C, lyr, xname, last):
    xsrc = C.dr[xname]
    xsrc_bufs = [dbuf(C, '%s#%d' % (xname, i)) for i in range(9)]
    S = C.S
    with ExitStack() as stack:
        w_in = sb(C, stack, 'w_in_sb', [128, 8, 2048], BF16)
        stg = [sb(C, stack, 'stgA%d' % i, [128, 2048], F32) for i in range(2)]
        for kc in range(8):
            st_ = stg[kc % 2]
            S.dma('sp', st_[:, :], C.dr['w_in'][lyr, kc * 128:(kc + 1) * 128, :], writes=[st_], sb=st_)
            cp(C, 'pool', w_in[:, kc, :], st_[:, :], [st_], [w_in])
        pmat = sb(C, stack, 'pmat_sb', [128, 128], BF16)
        S.dma('sp', stg[0][:, 0:128], C.dr['pmat'][:, :], writes=[stg[0]], sb=stg[0])
        cp(C, 'dve', pmat[:, :], stg[0][:, 0:128], [stg[0]], [pmat])
        Mx = mod_vectors(C, lyr, 0, stack, 'ax')
        Mc = mod_vectors(C, lyr, 1, stack, 'ac')
        xts = [[sb(C, stack, 'xtA%d_%d' % (j, i), [128, 1024], F32) for i in range(4)] for j in range(2)]
        hxTs = [sb(C, stack, 'hxTA%d' % j, [128, 8, 512], BF16) for j in range(2)]
        cosb = [sb(C, stack, 'cosA%d' % j, [128, 512], F32) for j in range(2)]
        sinb = [sb(C, stack, 'sinA%d' % j, [128, 512], F32) for j in range(2)]
        uTs = [sb(C, stack, 'uTA%d' % j, [128, 4, 512], F32) for j in range(2)]
        qTs = [sb(C, stack, 'qTA%d' % j, [128, 4, 512], BF16) for j in range(2)]
        kTs = [sb(C, stack, 'kTA%d' % j, [128, 4, 512], BF16) for j in range(2)]
        Vts = [sb(C, stack, 'VtA%d' % j, [128, 4, 4, VW], BF16) for j in range(2)]
        raw = [sb(C, stack, 'rawA%d' % j, [128, 512], BF16) for j in range(2)]
        t1 = [sb(C, stack, 't1A%d' % j, [128, 512], F32) for j in range(2)]
        t2 = [sb(C, stack, 't2A%d' % j, [128, 512], F32) for j in range(2)]
        C.ss = [sb(C, stack, 'ssA%d' % j, [128, 2], F32) for j in range(2)]
        C.rstd = [sb(C, stack, 'rstdA%d' % j, [128, 1], F32) for j in range(2)]
        C.xn = [sb(C, stack, 'xnA%d' % j, [128, 1024], BF16) for j in range(2)]
        C.junk_b = sb(C, stack, 'junkA', [128, 1024], BF16)
        for j in range(2):
            S.op('pool', lambda e, j=j: e.memset(Vts[j][:, :, :, :], 1.0), writes=[Vts[j]])

        def loads(si):
            tok0, ntok = ST[si]
            j = si % 2
            for i in range(ntok // 128):
                S.dma('sp', xts[j][i][:, :], xsrc[tok0 + i * 128: tok0 + (i + 1) * 128, :],
                      reads=[xsrc_bufs[si]], writes=[xts[j][i]], sb=xts[j][i])
            S.dma('sp', cosb[j][:, 0:ntok], C.dr['cosT'][:, tok0:tok0 + ntok], writes=[cosb[j]], sb=cosb[j])
            S.dma('sp', sinb[j][:, 0:ntok], C.dr['sinT'][:, tok0:tok0 + ntok], writes=[sinb[j]], sb=sinb[j])

        loads(0)
        pbi = 0
        for si, (tok0, ntok) in enumerate(ST):
            if si + 1 < len(ST):
                loads(si + 1)
            j = si % 2
            ntile = ntok // 128
            isctx = si == 8
            M = Mc if isctx else Mx
            hxT = hxTs[j]
            norm_mod_T(C, xts[j], ntile, hxT, M.gsc, 0, M.mT, 0, [C.pb[6], C.pb[7]])
            uT, qT, kT, Vt = uTs[j], qTs[j], kTs[j], Vts[j]
            nkb = ntile
            for cc in range(12):
                if last and isctx and cc < 8:
                    continue
                pb = C.pb[pbi % 4]
                pbi += 1
                mm(C, pb, pb[:, 0:ntok],
                   [(w_in[:, kc, cc * 128:(cc + 1) * 128], hxT[:, kc, 0:ntok]) for kc in range(8)], [w_in, hxT])
                if cc < 4:
                    cp(C, 'act', uT[:, cc, 0:ntok], pb[:, 0:ntok], [pb], [uT])
                    continue
                h = cc % 4
                rw = raw[cc % 2]
                cp(C, 'act', rw[:, 0:ntok], pb[:, 0:ntok], [pb], [rw])
                pw = C.pb[4 + (cc % 2)]
                mm(C, pw, pw[:, 0:ntok], [(pmat[:, :], rw[:, 0:ntok])], [pmat, rw])
                a1, a2 = t1[cc % 2], t2[cc % 2]
                tt(C, 'pool', a1[:, 0:ntok], rw[:, 0:ntok], cosb[j][:, 0:ntok], ALU.mult, [rw, cosb[j]], [a1])
                tt(C, 'dve', a2[:, 0:ntok], pw[:, 0:ntok], sinb[j][:, 0:ntok], ALU.mult, [pw, sinb[j]], [a2])
                if cc < 8:
                    tt(C, 'dve', qT[:, h, 0:ntok], a1[:, 0:ntok], a2[:, 0:ntok], ALU.add, [a1, a2], [qT])
                else:
                    o = kT[:, h, 0:ntok].rearrange('d (kb p) -> d p kb', kb=nkb)
                    i1 = a1[:, 0:ntok].rearrange('d (p kb) -> d p kb', kb=nkb)
                    i2 = a2[:, 0:ntok].rearrange('d (p kb) -> d p kb', kb=nkb)
                    tt(C, 'dve', o, i1, i2, ALU.add, [a1, a2], [kT])
            for i in range(ntile):
                pb = C.pb[pbi % 4]
                pbi += 1
                mm(C, pb, pb[:, :],
                   [(hxT[:, kc, i * 128:(i + 1) * 128], w_in[:, kc, 1536:2048]) for kc in range(8)], [w_in, hxT])
                cp(C, 'act' if i % 2 == 0 else 'dve', Vt[:, i, :, 0:128],
                   pb[:, :].rearrange('p (h e) -> p h e', h=4), [pb], [Vt])
            dr = C.dr
            l = lyr
            if not (last and isctx):
                S.dma('pool', dr['uT%d' % l].rearrange('(c p) t -> p c t', p=128)[:, :, tok0:tok0 + ntok],
                      uT[:, :, 0:ntok], reads=[uT], writes=[dbuf(C, 'uT%d#%d' % (l, si))], sb=uT)
                S.dma('pool', dr['qT%d' % l].rearrange('(c p) t -> p c t', p=128)[:, :, tok0:tok0 + ntok],
                      qT[:, :, 0:ntok], reads=[qT], writes=[dbuf(C, 'qT%d#%d' % (l, si))], sb=qT)
                hx = dr['Hx%d' % l].rearrange('p (c t) -> p c t', c=4)
                if si == 0:
                    S.dma('pool', hx[:, :, 0:8], uT[:, :, 0:8], reads=[uT], writes=[dbuf(C, 'Hx%d' % l)], sb=uT)
                if si == 7:
                    S.dma('pool', hx[:, :, 8:16], uT[:, :, 504:512], reads=[uT], writes=[dbuf(C, 'Hx%d' % l)], sb=uT)
                    if C.fused:
                        S.cc(dr['Hx%d' % l][:, :], dr['Hg%d' % l][:, :], reads=[dbuf(C, 'Hx%d' % l)],
                             writes=[dbuf(C, 'Hg%d' % l)])
            if not isctx:
                kn, vn = 'Kx%d_%d' % (l, si), 'Vx%d_%d' % (l, si)
                S.dma('pool', dr[kn].rearrange('(c p) t -> p c t', p=128), kT[:, :, 0:ntok],
                      reads=[kT], writes=[dbuf(C, kn)], sb=kT)
                for i in range(ntile):
                    S.dma('pool', dr[vn].rearrange('(h t) e -> t h e', h=4)[i * 128:(i + 1) * 128, :, :],
                          Vt[:, i, :, :], reads=[Vt], writes=[dbuf(C, vn)], sb=Vt)
                if C.fused:
                    S.cc(dr[kn][:, :], dr['Kg%d_%d' % (l, si)][:, :], reads=[dbuf(C, kn)],
                         writes=[dbuf(C, 'Kg%d_%d' % (l, si))])
                    S.cc(dr[vn][:, :], dr['Vg%d_%d' % (l, si)][:, :], reads=[dbuf(C, vn)],
                         writes=[dbuf(C, 'Vg%d_%d' % (l, si))])
            else:
                S.dma('pool', dr['Kc%d' % l].rearrange('(c p) t -> p c t', p=128), kT[:, :, 0:ntok],
                      reads=[kT], writes=[dbuf(C, 'Kc%d' % l)], sb=kT)
                for i in range(ntile):
                    S.dma('pool', dr['Vc%d' % l].rearrange('(h t) e -> t h e', h=4)[i * 128:(i + 1) * 128, :, :],
                          Vt[:, i, :, :], reads=[Vt], writes=[dbuf(C, 'Vc%d' % l)], sb=Vt)
        S.barrier()
        S.release(C.phase_bufs)
        C.phase_bufs = []


def load_w_bf16_rows(C, dst, src2d, nk, width, stg, q='sp', cast='pool'):
    for kc in range(nk):
        st_ = stg[kc % len(stg)]
        C.S.dma(q, st_[:, 0:width], src2d[kc * 128:(kc + 1) * 128, :], writes=[st_], sb=st_)
        cp(C, cast, dst[:, kc, :], st_[:, 0:width], [st_], [dst])


def phase_b(C, lyr, xname, last):
    S = C.S
    l = lyr
    dr = C.dr
    lam_init = 0.8 - 0.6 * math.exp(-0.3 * lyr)
    sts = list(range(8)) if last else list(range(9))
    with ExitStack() as stack:
        stg = [sb(C, stack, 'stgB%d' % i, [128, 1024], F32) for i in range(2)]
        w_out = sb(C, stack, 'w_out_sb', [128, 8, 1024], BF16)
        load_w_bf16_rows(C, w_out, dr['w_out'][l], 8, 1024, stg)
        pool_w = sb(C, stack, 'pool_w_sb', [128, 4, 128], BF16)
        S.dma('sp', stg[0][:, 0:512].rearrange('p (g e) -> p g e', g=4), dr['pool_w'][l].rearrange('g c e -> c g e'),
              writes=[stg[0]], sb=stg[0])
        cp(C, 'dve', pool_w[:, :, :], stg[0][:, 0:512].rearrange('p (g e) -> p g e', g=4), [stg[0]], [pool_w])
        psT = sb(C, stack, 'psT_sb', [128, 8], F32)
        S.dma('sp', psT[:, :], dr['psT'][:, :], writes=[psT], sb=psT)
        sel = sb(C, stack, 'sel_sb', [128, 8], F32)
        S.dma('sp', sel[:, :], dr['sel'][:, :], writes=[sel], sb=sel)
        subg = sb(C, stack, 'subg', [128, 128], F32)
        load_bc(C, subg[:, :], subg, dr['subln_g'][l, :])
        tsc(C, 'dve', subg[:, :], subg[:, :], 1.0 - lam_init, None, ALU.mult, None, [subg], [subg])
        lamb = sb(C, stack, 'lamb', [128, 4, 64], F32)
        for i, nm in enumerate(['lambda_q1', 'lambda_k1', 'lambda_q2', 'lambda_k2']):
            load_bc(C, lamb[:, i, :], lamb, dr[nm][l, :])
        lsc = sb(C, stack, 'lsc', [128, 8], F32)
        ljunk = sb(C, stack, 'ljunk', [128, 64], F32)
        stt(C, ljunk[:, :], lamb[:, 0, :], 1.0, lamb[:, 1, :], ALU.mult, ALU.mult, [lamb], [ljunk, lsc], accum=lsc[:, 0:1])
        stt(C, ljunk[:, :], lamb[:, 2, :], 1.0, lamb[:, 3, :], ALU.mult, ALU.mult, [lamb], [ljunk, lsc], accum=lsc[:, 1:2])
        act(C, lsc[:, 2:4], lsc[:, 0:2], AF.Exp, [lsc], [lsc])
        tt(C, 'dve', lsc[:, 4:5], lsc[:, 2:3], lsc[:, 3:4], ALU.subtract, [lsc], [lsc])
        nlam = sb(C, stack, 'nlam', [128, 1], F32)
        tsc(C, 'dve', nlam[:, :], lsc[:, 4:5], lam_init, -1.0, ALU.add, ALU.mult, [lsc], [nlam])
        g1x = sb(C, stack, 'g1x', [128, 1024], F32)
        load_bc(C, g1x[:, :], g1x, dr['modrow'][l, 0, 2048:3072], reads=[dbuf(C, 'modrow')])
        g1c = None
        if not last:
            g1c = sb(C, stack, 'g1c', [128, 1024], F32)
            load_bc(C, g1c[:, :], g1c, dr['modrow'][l, 1, 2048:3072], reads=[dbuf(C, 'modrow')])
        hg = sb(C, stack, 'hg_sb', [128, 4, 64], F32)
        S.dma('sp', hg[:, :, :], dr['Hg%d' % l].rearrange('(r p) f -> p r f', p=128), reads=[dbuf(C, 'Hg%d' % l)],
              writes=[hg], sb=hg)
        hgv = hg[:, :, :].rearrange('p r (c t) -> p r c t', c=4)
        halo = sb(C, stack, 'halo', [128, 2, 4, 8], F32)
        for r in range(4):
            for side in range(2):
                src = hgv[:, r, :, 8:16] if side == 0 else hgv[:, r, :, 0:8]
                scol = sel[:, side * 4 + r:side * 4 + r + 1]
                if r == 0:
                    tsc(C, 'dve', halo[:, side, :, :], src, scol, None, ALU.mult, None, [hg, sel], [halo])
                else:
                    stt(C, halo[:, side, :, :], src, scol, halo[:, side, :, :], ALU.mult, ALU.add, [hg, sel, halo], [halo])
        qTt = [sb(C, stack, 'qTB%d' % j, [128, 4, 512], BF16) for j in range(2)]
        NB = 4
        kch = [sb(C, stack, 'kch%d' % j, [128, 512], BF16) for j in range(NB)]
        vch = [sb(C, stack, 'vch%d' % j, [128, 4, VW], BF16) for j in range(NB)]
        Eb = [sb(C, stack, 'Eb%d' % j, [128, 2, 512], BF16) for j in range(2)]
        attn_tm = sb(C, stack, 'attn_tm', [128, 4, 4, 128], BF16)
        catT = sb(C, stack, 'catT', [128, 8, 512], BF16)
        uTe = [sb(C, stack, 'uTe%d' % j, [128, 4, 528], F32) for j in range(2)]
        invc = [sb(C, stack, 'invc%d' % j, [128, 4, 512], F32) for j in range(2)]
        s2 = sb(C, stack, 'ps2', [128, 4, 528], F32)
        s4 = sb(C, stack, 'ps4', [128, 3, 528], F32)
        s8 = sb(C, stack, 'ps8', [128, 2, 528], F32)
        s16 = sb(C, stack, 'ps16', [128, 1, 528], F32)
        ptmp = sb(C, stack, 'ptmp', [128, 512], F32)
        pooledT = sb(C, stack, 'pooledT', [128, 4, 512], BF16)
        xts = [[sb(C, stack, 'xtB%d_%d' % (j, i), [128, 1024], F32) for i in range(4)] for j in range(2)]
        tmpo = [sb(C, stack, 'tmpo%d' % j, [128, 512], F32) for j in range(2)]
        o32 = [sb(C, stack, 'o32_%d' % j, [128, 128], F32) for j in range(2)]
        fsc = [sb(C, stack, 'fsc%d' % j, [128, 8], F32) for j in range(2)]
        fjunk = sb(C, stack, 'fjunk', [128, 128], BF16)
        xsrc = dr[xname]
        xdst = dr['xa%d' % l]
        accb = [C.pb[4], C.pb[5], C.pb[6]]
        misc = C.pb[7]

        def acc_ap(idx):
            return accb[idx // 3][:, (idx % 3) * 132:(idx % 3) * 132 + 129]

        def loads(si):
            tok0, ntok = ST[si]
            j = si % 2
            isctx = si == 8
            S.dma('sp', qTt[j][:, :, 0:ntok], dr['qT%d' % l].rearrange('(c p) t -> p c t', p=128)[:, :, tok0:tok0 + ntok],
                  reads=[dbuf(C, 'qT%d#%d' % (l, si))], writes=[qTt[j]], sb=qTt[j])
            ut = dr['uT%d' % l].rearrange('(c p) t -> p c t', p=128)
            ue = uTe[j]
            lo = 0 if (si == 0 or isctx) else 8
            hi = 0 if (si == 7 or isctx) else 8
            rd = [dbuf(C, 'uT%d#%d' % (l, si))]
            if lo:
                rd.append(dbuf(C, 'uT%d#%d' % (l, si - 1)))
            if hi:
                rd.append(dbuf(C, 'uT%d#%d' % (l, si + 1)))
            S.dma('sp', ue[:, :, 8 - lo:8 + ntok + hi], ut[:, :, tok0 - lo:tok0 + ntok + hi], reads=rd, writes=[ue], sb=ue)
            if isctx:
                S.op('pool', lambda e: e.memset(ue[:, :, 0:8], 0.0), writes=[ue])
                S.op('pool', lambda e: e.memset(ue[:, :, 8 + ntok:16 + ntok], 0.0), writes=[ue])
            else:
                if si == 0:
                    cp(C, 'pool', ue[:, :, 0:8], halo[:, 0, :, :], [halo], [ue])
                if si == 7:
                    cp(C, 'pool', ue[:, :, 8 + ntok:16 + ntok], halo[:, 1, :, :], [halo], [ue])
            S.dma('sp', invc[j][:, :, 0:ntok], dr['invcnt'][:, tok0:tok0 + ntok].partition_broadcast(128),
                  writes=[invc[j]], sb=invc[j])
            for i in range(ntok // 128):
                S.dma('sp', xts[j][i][:, :], xsrc[tok0 + i * 128:tok0 + (i + 1) * 128, :],
                      reads=[dbuf(C, '%s#%d' % (xname, si))], writes=[xts[j][i]], sb=xts[j][i])

        nchunk_issued = [0]

        def chunk_list(si):
            cl = []
            if si != 8:
                for r in range(4):
                    for jj in range(8):
                        cl.append(('lat', r, jj))
            cl.append(('ctx', 0, 0))
            return cl

        def load_chunk(h, ch):
            n = nchunk_issued[0]
            nchunk_issued[0] += 1
            kb_, vb_ = kch[n % NB], vch[n % NB]
            kind, r, jj = ch
            if kind == 'lat':
                kn, vn = 'Kg%d_%d' % (l, jj), 'Vg%d_%d' % (l, jj)
                S.dma('sp', kb_[:, :], dr[kn][r * 512 + h * 128:r * 512 + (h + 1) * 128, :], reads=[dbuf(C, kn)],
                      writes=[kb_], sb=kb_)
                S.dma('sp', vb_[:, :, :],
                      dr[vn][r * 2048 + h * 512:r * 2048 + (h + 1) * 512, :].rearrange('(p kb) e -> p kb e', kb=4),
                      reads=[dbuf(C, vn)], writes=[vb_], sb=vb_)
            else:
                S.dma('sp', kb_[:, 0:256], dr['Kc%d' % l][h * 128:(h + 1) * 128, :], reads=[dbuf(C, 'Kc%d' % l)],
                      writes=[kb_], sb=kb_)
                S.dma('sp', vb_[:, 0:2, :],
                      dr['Vc%d' % l][h * 256:(h + 1) * 256, :].rearrange('(p kb) e -> p kb e', kb=2),
                      reads=[dbuf(C, 'Vc%d' % l)], writes=[vb_], sb=vb_)
            return kb_, vb_

        loads(sts[0])
        ucount = [0]
        for sidx, si in enumerate(sts):
            if sidx + 1 < len(sts):
                loads(sts[sidx + 1])
            tok0, ntok = ST[si]
            j = si % 2
            isctx = si == 8
            ntile = ntok // 128
            qT = qTt[j]
            chunks = chunk_list(si)
            work = [(h, ci) for h in range(4) for ci in range(len(chunks))]
            loaded = {}
            PRE = 3
            for wi in range(min(PRE, len(work))):
                loaded[wi] = load_chunk(work[wi][0], chunks[work[wi][1]])
            units = []
            for wi, (h, ci) in enumerate(work):
                nkb = 4 if chunks[ci][0] == 'lat' else 2
                for kb in range(nkb):
                    units.append((wi, h, ci, kb))

            def emit_S(u):
                wi, h, ci, kb = units[u]
                kb_, vb_ = loaded[wi]
                uu = ucount[0] + u
                b0, b1 = C.pb[(uu % 2) * 2], C.pb[(uu % 2) * 2 + 1]
                o0, o1 = b0[:, 0:ntok], b1[:, 0:ntok]
                l0, l1 = kb_[0:64, kb * 128:(kb + 1) * 128], kb_[64:128, kb * 128:(kb + 1) * 128]
                r0, r1 = qT[0:64, h, 0:ntok], qT[64:128, h, 0:ntok]
                fns = [lambda e, o0=o0, l0=l0, r0=r0: e.matmul(o0, l0, r0, start=True, stop=True),
                       lambda e, o1=o1, l1=l1, r1=r1: e.matmul(o1, l1, r1, start=True, stop=True)]
                S.op('pe', fns, reads=[kb_, qT], writes=[b0, b1])

            started = {}
            emit_S(0)
            for u, (wi, h, ci, kb) in enumerate(units):
                if kb == 0 and wi + PRE < len(work) and (wi + PRE) not in loaded:
                    loaded[wi + PRE] = load_chunk(work[wi + PRE][0], chunks[work[wi + PRE][1]])
                if u + 1 < len(units):
                    emit_S(u + 1)
                uu = ucount[0] + u
                b0, b1 = C.pb[(uu % 2) * 2], C.pb[(uu % 2) * 2 + 1]
                E = Eb[uu % 2]
                pin = C.pb2[uu % 2][:, :].rearrange('p (m q) -> p m q', m=2)[:, :, 0:ntok]
                act(C, E[:, :, 0:ntok], pin, AF.Exp, [b0, b1], [E], scale=0.125)
                kb_, vb_ = loaded[wi]
                fns = []
                for m in range(2):
                    for qb in range(ntile):
                        idx = m * 4 + qb
                        bank = idx // 3
                        st = (h, bank) not in started
                        started[(h, bank)] = True
                        oa, la, ra = acc_ap(idx), E[:, m, qb * 128:(qb + 1) * 128], vb_[:, kb, 0:129]
                        fns.append(lambda e, oa=oa, la=la, ra=ra, st=st: e.matmul(
                            oa, la, ra, start=st, stop=False, skip_group_check=True))
                S.op('pe', fns, reads=[E, vb_], writes=accb)
                last_of_head = (u + 1 == len(units)) or units[u + 1][1] != h
                if last_of_head:
                    for qb in range(ntile):
                        f = fsc[qb % 2]
                        o = o32[qb % 2]
                        a0, a1 = acc_ap(qb), acc_ap(4 + qb)
                        S.op('dve', lambda e, f=f, a0=a0: e.reciprocal(f[:, 0:1], a0[:, 128:129]), reads=accb, writes=[f])
                        S.op('dve', lambda e, f=f, a1=a1: e.reciprocal(f[:, 1:2], a1[:, 128:129]), reads=accb, writes=[f])
                        tt(C, 'dve', f[:, 2:3], f[:, 1:2], nlam[:, 0:1], ALU.mult, [f, nlam], [f])
                        tsc(C, 'dve', o[:, :], a0[:, 0:128], f[:, 0:1], None, ALU.mult, None, accb, [o], strict=[f])
                        stt(C, o[:, :], a1[:, 0:128], f[:, 2:3], o[:, :], ALU.mult, ALU.add, accb + [o], [o], strict=[f])
                        stt(C, fjunk[:, :], o[:, :], 1.0, o[:, :], ALU.mult, ALU.mult, [o], [fjunk, f], accum=f[:, 3:4])
                        act(C, f[:, 4:5], f[:, 3:4], AF.Sqrt, [f], [f], scale=1.0 / 128, bias=C.eps_t[:, 0:1],
                            strict=[C.eps_t])
                        S.op('dve', lambda e, f=f: e.reciprocal(f[:, 5:6], f[:, 4:5]), reads=[f], writes=[f])
                        stt(C, attn_tm[:, qb, h, :], o[:, :], f[:, 5:6], subg[:, :], ALU.mult, ALU.mult,
                            [o, subg], [attn_tm], strict=[f])
            ucount[0] += len(units)
            mv = misc[:, :].bitcast(BF16).rearrange('p (k t) -> p k t', k=8)
            for qb in range(ntile):
                for h in range(4):
                    tr(C, misc, mv[:, h, :], attn_tm, attn_tm[:, qb, h, :], C.ident_b[:])
                cp(C, 'dve' if qb % 2 == 0 else 'act', catT[:, 4:8, qb * 128:(qb + 1) * 128], mv[:, 0:4, :], [misc], [catT])
            ue = uTe[j]
            W = ntok + 16
            tt(C, 'pool', s2[:, :, 0:W - 1], ue[:, :, 0:W - 1], ue[:, :, 1:W], ALU.add, [ue], [s2])
            tt(C, 'pool', s4[:, :, 0:W - 3], s2[:, 1:4, 0:W - 3], s2[:, 1:4, 2:W - 1], ALU.add, [s2], [s4])
            tt(C, 'pool', s8[:, :, 0:W - 7], s4[:, 1:3, 0:W - 7], s4[:, 1:3, 4:W - 3], ALU.add, [s4], [s8])
            tt(C, 'pool', s16[:, :, 0:W - 15], s8[:, 1:2, 0:W - 15], s8[:, 1:2, 8:W - 7], ALU.add, [s8], [s16])
            wsrc = [(s2, 0, 7), (s4, 0, 6), (s8, 0, 4), (s16, 0, 0)]
            for g in range(4):
                buf_, gi, off = wsrc[g]
                tt(C, 'pool', ptmp[:, 0:ntok], buf_[:, gi, off:off + ntok], invc[j][:, g, 0:ntok], ALU.mult,
                   [buf_, invc[j]], [ptmp])
                tt(C, 'pool', pooledT[:, g, 0:ntok], ptmp[:, 0:ntok], ue[:, g, 8:8 + ntok], ALU.subtract,
                   [ptmp, ue], [pooledT])
            for g in range(4):
                pbk = C.pb[g % 4]
                mm(C, pbk, pbk[:, 0:ntok], [(pool_w[:, g, :], pooledT[:, g, 0:ntok])], [pool_w, pooledT])
                tsc(C, 'dve', catT[:, g, 0:ntok], pbk[:, 0:ntok], psT[:, l * 4 + g:l * 4 + g + 1], None, ALU.mult, None,
                    [pbk, psT], [catT])
            gbc = g1c if isctx else g1x
            k = 0
            for i in range(ntile):
                xt = xts[j][i]
                for half in range(2):
                    pbk = C.pb[k % 4]
                    tm = tmpo[k % 2]
                    k += 1
                    mm(C, pbk, pbk[:, :], [(catT[:, kc, i * 128:(i + 1) * 128], w_out[:, kc, half * 512:(half + 1) * 512])
                                           for kc in range(8)], [catT, w_out])
                    tt(C, 'dve', tm[:, :], pbk[:, :], gbc[:, half * 512:(half + 1) * 512], ALU.mult, [pbk, gbc], [tm])
                    tt(C, 'dve', xt[:, half * 512:(half + 1) * 512], xt[:, half * 512:(half + 1) * 512], tm[:, :], ALU.add,
                       [xt, tm], [xt])
                S.dma('pool', xdst[tok0 + i * 128:tok0 + (i + 1) * 128, :], xt[:, :], reads=[xt],
                      writes=[dbuf(C, 'xa%d#%d' % (l, si))], sb=xt)
        S.barrier()
        S.release(C.phase_bufs)
        C.phase_bufs = []


def phase_c(C, lyr, last):
    S = C.S
    l = lyr
    dr = C.dr
    sts = list(range(8)) if last else list(range(9))
    with ExitStack() as stack:
        Mx = mod_vectors(C, lyr, 0, stack, 'cx')
        Mc = mod_vectors(C, lyr, 1, stack, 'cc') if not last else None
        g2x = sb(C, stack, 'g2x', [128, 1024], F32)
        load_bc(C, g2x[:, :], g2x, dr['modrow'][l, 0, 5120:6144], reads=[dbuf(C, 'modrow')])
        g2c = None
        if not last:
            g2c = sb(C, stack, 'g2c', [128, 1024], F32)
            load_bc(C, g2c[:, :], g2c, dr['modrow'][l, 1, 5120:6144], reads=[dbuf(C, 'modrow')])
        fg = None
        if last:
            fg = sb(C, stack, 'fg', [128, 1024], F32)
            load_bc(C, fg[:, :], fg, dr['final_g'][:])
        rstg = sb(C, stack, 'rstg', [128, 8, 36], F32)
        S.dma('sp', rstg[:, :, 0:4], dr['router_coarse_w'][l].rearrange('(kc p) g -> p kc g', p=128), writes=[rstg], sb=rstg)
        S.dma('sp', rstg[:, :, 4:36], dr['router_fine_w'][l].rearrange('(kc p) g -> p kc g', p=128), writes=[rstg], sb=rstg)
        rw = sb(C, stack, 'rw', [128, 8, 36], BF16)
        cp(C, 'dve', rw[:, :, :], rstg[:, :, :], [rstg], [rw])
        rb = sb(C, stack, 'rb', [128, 36], F32)
        load_bc(C, rb[:, 0:4], rb, dr['router_coarse_b'][l, :])
        load_bc(C, rb[:, 4:36], rb, dr['router_fine_b'][l, :])
        NW = 2
        wst = [[sb(C, stack, 'wst%d_%d' % (j, t), [128, 2048], F32) for t in range(3)] for j in range(NW)]
        wg = [sb(C, stack, 'wg%d' % j, [128, 8, 256], BF16) for j in range(NW)]
        wu = [sb(C, stack, 'wu%d' % j, [128, 8, 256], BF16) for j in range(NW)]
        wd = [sb(C, stack, 'wd%d' % j, [128, 2, 1024], BF16) for j in range(NW)]
        xts = [[sb(C, stack, 'xtC%d_%d' % (j, i), [128, 1024], F32) for i in range(4)] for j in range(2)]
        hxTs = [sb(C, stack, 'hxTC%d' % j, [128, 8, 512], BF16) for j in range(2)]
        accs = [sb(C, stack, 'accC%d' % i, [128, 1024], F32) for i in range(4)]
        gates = [sb(C, stack, 'gates%d' % i, [128, 32], F32) for i in range(4)]
        hidT = [sb(C, stack, 'hidT%d' % j, [128, 2, 512], BF16) for j in range(2)]
        sa = [sb(C, stack, 'sa%d' % j, [128, 512], F32) for j in range(2)]
        C.ss = [sb(C, stack, 'ssC%d' % j, [128, 2], F32) for j in range(2)]
        C.rstd = [sb(C, stack, 'rstdC%d' % j, [128, 1], F32) for j in range(2)]
        C.xn = [sb(C, stack, 'xnC%d' % j, [128, 1024], BF16) for j in range(2)]
        C.junk_b = sb(C, stack, 'junkC', [128, 1024], BF16)
        lg = sb(C, stack, 'lg', [128, 36], F32)
        rs = sb(C, stack, 'rsc', [128, 16], F32)
        lfs = sb(C, stack, 'lfs', [128, 8], F32)
        top8 = sb(C, stack, 'top8', [128, 8], F32)
        g8 = sb(C, stack, 'g8', [128, 8], F32)
        g8b = sb(C, stack, 'g8b', [128, 8], F32)
        mk = sb(C, stack, 'mk', [128, 4], F32)
        e4 = sb(C, stack, 'e4', [128, 4], F32)
        xsrc = dr['xa%d' % l]
        xdst = dr['out'] if last else dr['xm%d' % l]

        def loads(si):
            tok0, ntok = ST[si]
            j = si % 2
            for i in range(ntok // 128):
                S.dma('sp', xts[j][i][:, :], xsrc[tok0 + i * 128:tok0 + (i + 1) * 128, :],
                      reads=[dbuf(C, 'xa%d#%d' % (l, si))], writes=[xts[j][i]], sb=xts[j][i])

        nw = [0]

        def load_expert(e):
            n = nw[0]
            nw[0] += 1
            jn = n % NW
            g_, e_ = e // 8, e % 8
            st0, st1, st2 = wst[jn]
            S.dma('sp', st0[:, :].rearrange('p (kc f) -> p kc f', kc=8),
                  dr['w_gate'][l, g_, e_].rearrange('(kc p) f -> p kc f', p=128), writes=[st0], sb=st0)
            S.dma('sp', st1[:, :].rearrange('p (kc f) -> p kc f', kc=8),
                  dr['w_up'][l, g_, e_].rearrange('(kc p) f -> p kc f', p=128), writes=[st1], sb=st1)
            S.dma('sp', st2[:, :].rearrange('p (fc d) -> p fc d', fc=2),
                  dr['w_down'][l, g_, e_].rearrange('(fc p) d -> p fc d', p=128), writes=[st2], sb=st2)
            cp(C, 'pool', wg[jn][:, :, :], st0[:, :].rearrange('p (kc f) -> p kc f', kc=8), [st0], [wg[jn]])
            cp(C, 'pool', wu[jn][:, :, :], st1[:, :].rearrange('p (kc f) -> p kc f', kc=8), [st1], [wu[jn]])
            cp(C, 'pool', wd[jn][:, :, :], st2[:, :].rearrange('p (fc d) -> p fc d', fc=2), [st2], [wd[jn]])
            return wg[jn], wu[jn], wd[jn]

        loads(sts[0])
        hcount = 0
        ocount = 0
        for sidx, si in enumerate(sts):
            if sidx + 1 < len(sts):
                loads(sts[sidx + 1])
            tok0, ntok = ST[si]
            j = si % 2
            isctx = si == 8
            ntile = ntok // 128
            M = Mc if isctx else Mx
            hxT = hxTs[j]
            norm_mod_T(C, xts[j], ntile, hxT, M.gsc, 8, M.mT, 24, [C.pb[6], C.pb[7]])
            for i in range(ntile):
                pr = C.pb[6 + (i % 2)]
                mm(C, pr, pr[:, 0:36], [(hxT[:, kc, i * 128:(i + 1) * 128], rw[:, kc, :]) for kc in range(8)], [hxT, rw])
                tt(C, 'dve', lg[:, :], pr[:, 0:36], rb[:, :], ALU.add, [pr, rb], [lg])
                S.op('dve', lambda e: e.tensor_reduce(rs[:, 0:1], lg[:, 0:4], mybir.AxisListType.X, ALU.max),
                     reads=[lg], writes=[rs])
                tsc(C, 'dve', mk[:, :], lg[:, 0:4], rs[:, 0:1], None, ALU.is_equal, None, [lg], [mk], strict=[rs])
                tsc(C, 'dve', rs[:, 1:2], rs[:, 0:1], -1.0, None, ALU.mult, None, [rs], [rs])
                act(C, e4[:, :], lg[:, 0:4], AF.Exp, [lg, rs], [e4, rs], bias=rs[:, 1:2], accum=rs[:, 2:3])
                S.op('dve', lambda e: e.reciprocal(rs[:, 3:4], rs[:, 2:3]), reads=[rs], writes=[rs])
                for g in range(4):
                    src = lg[:, 4 + 8 * g:12 + 8 * g]
                    if g == 0:
                        tsc(C, 'dve', lfs[:, :], src, mk[:, 0:1], None, ALU.mult, None, [lg], [lfs], strict=[mk])
                    else:
                        stt(C, lfs[:, :], src, mk[:, g:g + 1], lfs[:, :], ALU.mult, ALU.add, [lg, lfs], [lfs], strict=[mk])
                S.op('dve', lambda e: e.max(top8[:, :], lfs[:, :]), reads=[lfs], writes=[top8])
                tt(C, 'dve', rs[:, 4:5], top8[:, 1:2], top8[:, 0:1], ALU.subtract, [top8], [rs])
                act(C, rs[:, 5:6], rs[:, 4:5], AF.Exp, [rs], [rs])
                tsc(C, 'dve', rs[:, 6:7], rs[:, 5:6], 1.0, None, ALU.add, None, [rs], [rs])
                S.op('dve', lambda e: e.reciprocal(rs[:, 7:8], rs[:, 6:7]), reads=[rs], writes=[rs])
                tt(C, 'dve', rs[:, 8:9], rs[:, 7:8], rs[:, 3:4], ALU.mult, [rs], [rs])
                tt(C, 'dve', rs[:, 9:10], rs[:, 8:9], rs[:, 5:6], ALU.mult, [rs], [rs])
                tsc(C, 'dve', g8[:, :], lfs[:, :], top8[:, 0:1], rs[:, 8:9], ALU.is_equal, ALU.mult, [lfs], [g8],
                    strict=[top8, rs])
                tsc(C, 'dve', g8b[:, :], lfs[:, :], top8[:, 1:2], rs[:, 9:10], ALU.is_equal, ALU.mult, [lfs], [g8b],
                    strict=[top8, rs])
                tt(C, 'dve', g8[:, :], g8[:, :], g8b[:, :], ALU.add, [g8, g8b], [g8])
                for g in range(4):
                    tsc(C, 'dve', gates[i][:, 8 * g:8 * g + 8], g8[:, :], mk[:, g:g + 1], None, ALU.mult, None,
                        [g8], [gates[i]], strict=[mk])
            pend = load_expert(0)
            for e in range(32):
                wg_, wu_, wd_ = pend
                if e + 1 < 32:
                    pend = load_expert(e + 1)
                hT = hidT[hcount % 2]
                hcount += 1
                for fc in range(2):
                    pa, pbb = C.pb[fc], C.pb[2 + fc]
                    mm(C, pa, pa[:, 0:ntok], [(wg_[:, kc, fc * 128:(fc + 1) * 128], hxT[:, kc, 0:ntok]) for kc in range(8)],
                       [wg_, hxT])
                    mm(C, pbb, pbb[:, 0:ntok], [(wu_[:, kc, fc * 128:(fc + 1) * 128], hxT[:, kc, 0:ntok]) for kc in range(8)],
                       [wu_, hxT])
                    sa_ = sa[fc]
                    act(C, sa_[:, 0:ntok], pa[:, 0:ntok], AF.Silu, [pa], [sa_])
                    tt(C, 'dve', hT[:, fc, 0:ntok], sa_[:, 0:ntok], pbb[:, 0:ntok], ALU.mult, [sa_, pbb], [hT])
                for i in range(ntile):
                    for half in range(2):
                        po = C.pb[4 + (ocount % 2)]
                        ocount += 1
                        mm(C, po, po[:, :], [(hT[:, fc, i * 128:(i + 1) * 128], wd_[:, fc, half * 512:(half + 1) * 512])
                                             for fc in range(2)], [hT, wd_])
                        a_ = accs[i][:, half * 512:(half + 1) * 512]
                        if e == 0:
                            tsc(C, 'dve', a_, po[:, :], gates[i][:, 0:1], None, ALU.mult, None, [po], [accs[i]],
                                strict=[gates[i]])
                        else:
                            stt(C, a_, po[:, :], gates[i][:, e:e + 1], a_, ALU.mult, ALU.add, [po, accs[i]], [accs[i]],
                                strict=[gates[i]])
            gbc = g2c if isctx else g2x
            for i in range(ntile):
                xt = xts[j][i]
                tt(C, 'pool', accs[i][:, :], accs[i][:, :], gbc[:, :], ALU.mult, [accs[i], gbc], [accs[i]])
                tt(C, 'pool', xt[:, :], xt[:, :], accs[i][:, :], ALU.add, [xt, accs[i]], [xt])
                if last:
                    ss = C.ss[i % 2]
                    rstd = C.rstd[i % 2]
                    rms_rstd(C, (xt[:, :], xt), rstd, ss, C.junk_b[:, :])
                    stt(C, xt[:, :], xt[:, :], rstd[:, 0:1], fg[:, :], ALU.mult, ALU.mult, [xt, fg], [xt], strict=[rstd])
                    S.dma('pool', xdst[tok0 + i * 128:tok0 + (i + 1) * 128, :], xt[:, :], reads=[xt],
                          writes=[dbuf(C, 'out')], sb=xt)
                else:
                    S.dma('pool', xdst[tok0 + i * 128:tok0 + (i + 1) * 128, :], xt[:, :], reads=[xt],
                          writes=[dbuf(C, 'xm%d#%d' % (l, si))], sb=xt)
        S.barrier()
        S.release(C.phase_bufs)
        C.phase_bufs = []


TAGGED = {'ada_w', 'w_in', 'w_out', 'w_gate', 'w_up', 'w_down'}
TAGN = 64


def declare(C, name, shape, dt, role):
    kind = {'in': 'ExternalInput', 'out': 'ExternalOutput', 'tmp': 'Internal'}[role]
    if name in TAGGED:
        n = 1
        for d_ in shape:
            n *= d_
        flat = C.nc.dram_tensor(name, [n + TAGN], dt, kind=kind).ap()
        letters = 'abcdefg'[:len(shape)]
        pat = '(%s) -> %s' % (' '.join(letters), ' '.join(letters))
        C.dr[name] = flat[0:n].rearrange(pat, **{letters[i]: shape[i] for i in range(1, len(shape))})
        return
    if role == 'tmp' and name.startswith(('Kg', 'Vg', 'Hg')):
        C.dr[name] = C.nc.dram_tensor(name, list(shape), dt, kind=kind, addr_space='Local').ap()
    else:
        C.dr[name] = C.nc.dram_tensor(name, list(shape), dt, kind=kind).ap()


CONST_IN = [('ident', [128, 128]), ('pmat', [128, 128]), ('cosT', [128, NT]), ('sinT', [128, NT]),
            ('n1gT', [128, 16]), ('n2gT', [128, 16]), ('psT', [128, 8]), ('sel', [128, 8]), ('invcnt', [4, NT])]
PARAM_IN = [('w_in', [2, D, 2048]), ('w_out', [2, D, D]), ('pool_w', [2, 4, 128, 128]), ('subln_g', [2, 128]),
            ('lambda_q1', [2, 64]), ('lambda_k1', [2, 64]), ('lambda_q2', [2, 64]), ('lambda_k2', [2, 64]),
            ('router_coarse_w', [2, D, 4]), ('router_coarse_b', [2, 4]), ('router_fine_w', [2, D, 32]),
            ('router_fine_b', [2, 32]), ('w_gate', [2, 4, 8, D, 256]), ('w_up', [2, 4, 8, D, 256]),
            ('w_down', [2, 4, 8, 256, D]), ('final_g', [D])]


_ALLP = [nm for nm, _ in PARAM_IN]
MODE_PARAMS = {0: set(_ALLP), 1: {'w_in'}, 2: set(_ALLP) - {'final_g'}, 3: set(_ALLP) - {'w_in'}}


def layer_tensors(l):
    t = [('uT%d' % l, [512, NT], F32), ('qT%d' % l, [512, NT], BF16), ('Hx%d' % l, [128, 64], F32),
         ('Kc%d' % l, [512, NCTX], BF16), ('Vc%d' % l, [4 * NCTX, VW], BF16)]
    for s in range(8):
        t.append(('Kx%d_%d' % (l, s), [512, 512], BF16))
        t.append(('Vx%d_%d' % (l, s), [2048, VW], BF16))
    return t


def gathered_tensors(l):
    t = [('Hg%d' % l, [4 * 128, 64], F32)]
    for s in range(8):
        t.append(('Kg%d_%d' % (l, s), [4 * 512, 512], BF16))
        t.append(('Vg%d_%d' % (l, s), [4 * 2048, VW], BF16))
    return t


def build(mode):
    nc = bass.Bass("TRN2", target_bir_lowering=False)
    C = Ctx()
    C.nc = nc
    C.dr = {}
    C.db = {}
    C.fused = mode == 0
    with ExitStack() as stack:
        C.S = Sched(nc, stack)
        for nm, shp in CONST_IN:
            declare(C, nm, shp, F32, 'in')
        for nm, shp in PARAM_IN:
            if nm in MODE_PARAMS[mode]:
                declare(C, nm, shp, F32, 'in')
        if mode in (0, 1):
            declare(C, 'x_in', [NT, D], F32, 'in')
            declare(C, 'sT_in', [128, 16], F32, 'in')
            declare(C, 'ada_w', [2, D, 6 * D], F32, 'in')
            declare(C, 'ada_b', [2, 6 * D], F32, 'in')
        declare(C, 'modrow', [2, 2, 6 * D], F32, {0: 'tmp', 1: 'out', 2: 'in', 3: 'in'}[mode])
        for l in range(2):
            prod = 1 if l == 0 else 2
            cons = prod + 1
            for nm, shp, dt in layer_tensors(l):
                if mode == 0:
                    declare(C, nm, shp, dt, 'tmp')
                elif mode == prod:
                    declare(C, nm, shp, dt, 'out')
                elif mode == cons and not nm.startswith(('Kx', 'Vx', 'Hx')):
                    declare(C, nm, shp, dt, 'in')
            for nm, shp, dt in gathered_tensors(l):
                if mode == 0:
                    declare(C, nm, shp, dt, 'tmp')
                elif mode == cons:
                    declare(C, nm, shp, dt, 'in')
        if mode == 0:
            for nm in ['xa0', 'xm0', 'xa1']:
                declare(C, nm, [NT, D], F32, 'tmp')
            declare(C, 'out', [NLAT, D], F32, 'out')
        elif mode == 2:
            declare(C, 'x_in', [NT, D], F32, 'in')
            declare(C, 'xa0', [NT, D], F32, 'out')
            declare(C, 'xm0', [NT, D], F32, 'out')
        elif mode == 3:
            declare(C, 'xm0', [NT, D], F32, 'in')
            declare(C, 'xa1', [NT, D], F32, 'tmp')
            declare(C, 'out', [NLAT, D], F32, 'out')
        setup_common(C, stack)
        C.n1gT = sb(C, stack, 'n1gT_sb', [128, 16], F32)
        C.n2gT = sb(C, stack, 'n2gT_sb', [128, 16], F32)
        C.S.dma('sp', C.n1gT[:, :], C.dr['n1gT'][:, :], writes=[C.n1gT], sb=C.n1gT)
        C.S.dma('sp', C.n2gT[:, :], C.dr['n2gT'][:, :], writes=[C.n2gT], sb=C.n2gT)
        C.phase_bufs = []
        if mode in (0, 1):
            prologue(C)
            phase_a(C, 0, 'x_in', last=False)
        if mode in (0, 2):
            phase_b(C, 0, 'x_in', last=False)
            phase_c(C, 0, last=False)
            phase_a(C, 1, 'xm0', last=True)
        if mode in (0, 3):
            phase_b(C, 1, 'xm0', last=True)
            phase_c(C, 1, last=True)
        C.S.barrier(['sp'])
        C.S.replay()
    return nc


def rope_tables_T(core):
    qq = core % 4
    tok = np.arange(NLAT) + qq * NLAT
    row = (tok // 64).astype(np.float32)
    col = (tok % 64).astype(np.float32)
    inv = (10000.0 ** (-np.arange(16, dtype=np.float32) / 16)).astype(np.float32)
    cosT = np.ones((64, NT), np.float32)
    sinT = np.zeros((64, NT), np.float32)
    for d in range(64):
        f = d % 16
        pos = row if d < 32 else col
        ang = (pos * inv[f]).astype(np.float32)
        sgn = -1.0 if (d % 32) < 16 else 1.0
        cosT[d, :NLAT] = np.cos(ang)
        sinT[d, :NLAT] = sgn * np.sin(ang)
    return np.tile(cosT, (2, 1)), np.tile(sinT, (2, 1))


def pmat_const():
    p = np.zeros((128, 128), np.float32)
    for i in range(128):
        partner = i + 16 if (i % 32) < 16 else i - 16
        p[partner, i] = 1.0
    return p


def invcnt_const(core):
    qq = core % 4
    out = np.zeros((4, NT), np.float32)
    for g, w in enumerate((2, 4, 8, 16)):
        left = w // 2
        right = w - 1 - left
        t = np.arange(NLAT) + qq * NLAT
        lo = np.clip(t - left, 0, L)
        hi = np.clip(t + right + 1, 0, L)
        out[g, :NLAT] = 1.0 / (hi - lo)
        t = np.arange(NCTX)
        lo = np.clip(t - left, 0, NCTX)
        hi = np.clip(t + right + 1, 0, NCTX)
        out[g, NLAT:] = 1.0 / (hi - lo)
    return out


def const_inputs(core, inp):
    cosT, sinT = rope_tables_T(core)
    qq = core % 4
    sel = np.zeros((128, 8), np.float32)
    if qq > 0:
        sel[:, qq - 1] = 1.0
    if qq < 3:
        sel[:, 4 + qq + 1] = 1.0
    m = {
        'ident': np.eye(128, dtype=np.float32),
        'pmat': pmat_const(),
        'cosT': cosT, 'sinT': sinT,
        'n1gT': np.ascontiguousarray(inp['norm1_g'].reshape(2, 8, 128).transpose(2, 0, 1).reshape(128, 16)),
        'n2gT': np.ascontiguousarray(inp['norm2_g'].reshape(2, 8, 128).transpose(2, 0, 1).reshape(128, 16)),
        'psT': np.ascontiguousarray(inp['pool_scale'].reshape(2, 4, 128).transpose(2, 0, 1).reshape(128, 8)),
        'sel': sel,
        'invcnt': invcnt_const(core),
    }
    return m


def first_inputs(core, inp):
    b, qq = core // 4, core % 4
    m = {}
    m['x_in'] = np.ascontiguousarray(np.concatenate([inp['x'][b, qq * NLAT:(qq + 1) * NLAT], inp['ctx'][b]], 0))
    s = np.stack([inp['c'][b], inp['c_ctx']], -1)
    m['sT_in'] = np.ascontiguousarray(s.reshape(8, 128, 2).transpose(1, 0, 2).reshape(128, 16))
    m['ada_w'] = tagged(inp['ada_w'], core)
    m['ada_b'] = inp['ada_b']
    return m


def gather_host(results, l):
    outs = []
    for c in range(NCORES):
        grp = [4 * (c // 4) + r for r in range(4)]
        m = {'Hg%d' % l: np.concatenate([results[r]['Hx%d' % l] for r in grp], 0)}
        for s in range(8):
            m['Kg%d_%d' % (l, s)] = np.concatenate([results[r]['Kx%d_%d' % (l, s)] for r in grp], 0)
            m['Vg%d_%d' % (l, s)] = np.concatenate([results[r]['Vx%d_%d' % (l, s)] for r in grp], 0)
        outs.append(m)
    return outs


_NC_CACHE = {}


def get_nc(mode):
    if mode not in _NC_CACHE:
        _NC_CACHE[mode] = build(mode)
    return _NC_CACHE[mode]


def tagged(a, core):
    return np.concatenate([np.asarray(a, np.float32).ravel(), np.full(TAGN, float(core), np.float32)])


def params_for(mode, inp, core=0):
    return {nm: (tagged(inp[nm], core) if nm in TAGGED else inp[nm]) for nm in _ALLP if nm in MODE_PARAMS[mode]}


def run_multi(inp, cores=None):
    consts = [const_inputs(c, inp) for c in range(NCORES)]
    ids = list(range(NCORES)) if cores is None else cores
    in1 = [dict(consts[c], **first_inputs(c, inp), **params_for(1, inp, c)) for c in ids]
    r1 = run_bass_kernel_spmd(get_nc(1), in1, core_ids=ids).results
    g0 = gather_host(r1, 0)
    in2 = []
    for c in ids:
        m = dict(consts[c], **params_for(2, inp, c))
        m['x_in'] = in1[c]['x_in']
        m['modrow'] = r1[c]['modrow']
        for nm in ['uT0', 'qT0', 'Kc0', 'Vc0']:
            m[nm] = r1[c][nm]
        m.update(g0[c])
        in2.append(m)
    r2 = run_bass_kernel_spmd(get_nc(2), in2, core_ids=ids).results
    g1 = gather_host(r2, 1)
    in3 = []
    for c in ids:
        m = dict(consts[c], **params_for(3, inp, c))
        m['modrow'] = r1[c]['modrow']
        m['xm0'] = r2[c]['xm0']
        for nm in ['uT1', 'qT1', 'Kc1', 'Vc1']:
            m[nm] = r2[c][nm]
        m.update(g1[c])
        in3.append(m)
    r3 = run_bass_kernel_spmd(get_nc(3), in3, core_ids=ids).results
    return r3


def run_fused(inp):
    ids = list(range(NCORES))
    ins = [dict(const_inputs(c, inp), **first_inputs(c, inp), **params_for(0, inp, c)) for c in ids]
    return run_bass_kernel_spmd(get_nc(0), ins, core_ids=ids).results


FUSED = True


def kernel(**inputs):
    inp = {k: np.ascontiguousarray(np.asarray(v, dtype=np.float32)) for k, v in inputs.items()}
    res = run_fused(inp) if FUSED else run_multi(inp)
    out = np.zeros((2, L, D), np.float32)
    for c in range(NCORES):
        b, qq = c // 4, c % 4
        out[b, qq * NLAT:(qq + 1) * NLAT] = np.asarray(res[c]['out'], dtype=np.float32)
    return out
```

```python
import math
from contextlib import ExitStack
import numpy as np
import ml_dtypes
import concourse.bass as bass
import concourse.mybir as mybir
from concourse.bass_utils import run_bass_kernel_spmd

F32 = mybir.dt.float32
BF16 = mybir.dt.bfloat16
AF = mybir.ActivationFunctionType
ALU = mybir.AluOpType

NCORES = 8
D = 1024
L = 16384
NLAT = 4096
NCTX = 256
NT = NLAT + NCTX
VW = 132
EPS = 1e-6
ST = [(s * 512, 512) for s in range(8)] + [(NLAT, NCTX)]
ENGS = ['sp', 'pe', 'act', 'dve', 'pool']


class Buf:
    def __init__(self, name, t=None):
        self.name = name
        self.t = t
        self.w = None
        self.r = {}
        self.dkey = None
        self.small = False

    def __getitem__(self, idx):
        return self.t[idx]


class Sched:
    def __init__(self, nc, stack):
        self.nc = nc
        self.stack = stack
        self.ops = {e: [] for e in ENGS}
        self.sem = {}
        self.cnt = {}
        self.isdma = {}
        self.waited = {e: {} for e in ENGS}
        self.nsem = 0
        self.free_d = []
        for e in ['pe', 'act', 'dve', 'pool']:
            self.newsem(e, False)

    def newsem(self, key, isdma):
        self.nsem += 1
        self.sem[key] = self.stack.enter_context(self.nc.semaphore('s%d' % self.nsem))
        self.cnt[key] = 0
        self.isdma[key] = isdma

    def _deps(self, reads, writes):
        deps = {}

        def add(k, v):
            if deps.get(k, 0) < v:
                deps[k] = v
        for b in reads:
            if b.w is not None:
                add(*b.w)
        for b in writes:
            if b.w is not None:
                add(*b.w)
            for k, v in b.r.items():
                add(k, v)
        return deps

    def _wait(self, eng, deps, strict=()):
        own = 0
        for b in strict:
            if b.w is not None and b.w[0] == eng:
                own = max(own, b.w[1])
        for k, v in deps.items():
            if k == eng:
                if own == 0:
                    continue
                v = own
            if self.isdma[k]:
                v = self.cnt[k]
            if self.waited[eng].get(k, 0) >= v:
                continue
            self.waited[eng][k] = v
            sem = self.sem[k]
            self.ops[eng].append(lambda e, sem=sem, v=v: e.wait_ge(sem, v))

    def _commit(self, key, v, reads, writes):
        for b in writes:
            b.w = (key, v)
            b.r = {}
        for b in reads:
            if b.r.get(key, 0) < v:
                b.r[key] = v

    def op(self, eng, fns, reads=(), writes=(), strict=()):
        if not isinstance(fns, (list, tuple)):
            fns = [fns]
        strict = list(strict) + [b for b in reads if b.small]
        self._wait(eng, self._deps(list(reads) + list(strict), writes), strict)
        self.cnt[eng] += 1
        v = self.cnt[eng]
        sem = self.sem[eng]
        for f in fns[:-1]:
            self.ops[eng].append(f)
        last = fns[-1]
        self.ops[eng].append(lambda e, last=last, sem=sem: last(e).then_inc(sem, 1))
        self._commit(eng, v, reads, writes)

    def dma(self, q, out, in_, reads=(), writes=(), sb=None, **kw):
        if sb.dkey is None:
            if self.free_d:
                sb.dkey = self.free_d.pop()
            else:
                sb.dkey = ('d', len(self.sem))
                self.newsem(sb.dkey, True)
        key = sb.dkey
        self._wait(q, self._deps(reads, writes))
        self.cnt[key] += 16
        v = self.cnt[key]
        sem = self.sem[key]
        self.ops[q].append(lambda e: e.dma_start(out=out, in_=in_, **kw).then_inc(sem, 16))
        self._commit(key, v, reads, writes)

    def cc(self, ins_ap, outs_ap, reads=(), writes=()):
        key = 'cc'
        if key not in self.sem:
            self.newsem(key, True)
        self._wait('pool', self._deps(reads, writes))
        self.cnt[key] += 1
        v = self.cnt[key]
        sem = self.sem[key]
        self.ops['pool'].append(lambda e: e.collective_compute(
            "AllGather", ALU.bypass, replica_groups=[[0, 1, 2, 3], [4, 5, 6, 7]],
            ins=[ins_ap], outs=[outs_ap]).then_inc(sem, 1))
        self._commit(key, v, reads, writes)

    def release(self, bufs):
        for b in bufs:
            if b.dkey is not None:
                self.free_d.append(b.dkey)
                b.dkey = None

    def barrier(self, engines=ENGS):
        for e in engines:
            for k in self.sem:
                if k == e or self.cnt[k] == 0:
                    continue
                v = self.cnt[k]
                if self.waited[e].get(k, 0) >= v:
                    continue
                self.waited[e][k] = v
                sem = self.sem[k]
                self.ops[e].append(lambda en, sem=sem, v=v: en.wait_ge(sem, v))

    def replay(self):
        nc = self.nc
        with nc.Block() as block:
            @block.sync
            def _(e):
                for f in self.ops['sp']:
                    f(e)

            @block.tensor
            def _(e):
                for f in self.ops['pe']:
                    f(e)

            @block.scalar
            def _(e):
                for f in self.ops['act']:
                    f(e)

            @block.vector
            def _(e):
                for f in self.ops['dve']:
                    f(e)

            @block.gpsimd
            def _(e):
                for f in self.ops['pool']:
                    f(e)


class Ctx:
    pass


def dbuf(C, name):
    if name not in C.db:
        C.db[name] = Buf(name)
    return C.db[name]


def sb(C, stack, name, shape, dt):
    if not hasattr(C, 'names'):
        C.names = {}
    k = C.names.get(name, 0)
    C.names[name] = k + 1
    if k:
        name = '%s_r%d' % (name, k)
    t = stack.enter_context(C.nc.sbuf_tensor(name, shape, dt))
    b = Buf(name, t)
    if getattr(C, 'phase_bufs', None) is not None:
        C.phase_bufs.append(b)
    fs = 1
    for d_ in shape[1:]:
        fs *= d_
    b.small = fs < 256
    return b


def ps(C, stack, name, shape, dt):
    t = stack.enter_context(C.nc.psum_tensor(name, shape, dt))
    return Buf(name, t)


def mm(C, out_buf, out_ap, pairs, reads, start=True, stop=True, skip=False):
    fns = []
    n = len(pairs)
    for i, (l_ap, r_ap) in enumerate(pairs):
        st = start and i == 0
        sp_ = stop and i == n - 1
        fns.append(lambda e, l_ap=l_ap, r_ap=r_ap, st=st, sp_=sp_: e.matmul(
            out_ap, l_ap, r_ap, start=st, stop=sp_, skip_group_check=skip))
    C.S.op('pe', fns, reads=reads, writes=[out_buf])


def tr(C, out_buf, out_ap, in_buf, in_ap, ident_ap, extra_reads=()):
    C.S.op('pe', lambda e: e.transpose(out_ap, in_ap, ident_ap),
           reads=[in_buf, C.ident_b] + list(extra_reads), writes=[out_buf])


def act(C, out_ap, in_ap, func, reads, writes, scale=None, bias=None, accum=None, strict=()):
    kw = {}
    if scale is not None:
        kw['scale'] = scale
    if bias is not None:
        kw['bias'] = bias
    if accum is not None:
        kw['accum_out'] = accum
    C.S.op('act', lambda e: e.activation(out_ap, in_ap, func, **kw), reads=reads, writes=writes, strict=strict)


def tsc(C, eng, out_ap, in_ap, s1, s2, op0, op1, reads, writes, accum=None, strict=()):
    if op1 is None:
        C.S.op(eng, lambda e: e.tensor_scalar(out_ap, in_ap, s1, None, op0), reads=reads, writes=writes, strict=strict)
    else:
        C.S.op(eng, lambda e: e.tensor_scalar(out_ap, in_ap, s1, s2, op0, op1), reads=reads, writes=writes, strict=strict)


def stt(C, out_ap, in0, scalar, in1, op0, op1, reads, writes, accum=None, strict=()):
    if accum is None:
        C.S.op('dve', lambda e: e.scalar_tensor_tensor(out_ap, in0, scalar, in1, op0, op1),
               reads=reads, writes=writes, strict=strict)
    else:
        C.S.op('dve', lambda e: e.scalar_tensor_tensor(out_ap, in0, scalar, in1, op0, op1, accum_out=accum),
               reads=reads, writes=writes, strict=strict)


def tt(C, eng, out_ap, in0, in1, op, reads, writes):
    C.S.op(eng, lambda e: e.tensor_tensor(out_ap, in0, in1, op), reads=reads, writes=writes)


def cp(C, eng, out_ap, in_ap, reads, writes):
    if eng == 'act':
        C.S.op('act', lambda e: e.activation(out_ap, in_ap, AF.Copy), reads=reads, writes=writes)
    else:
        C.S.op(eng, lambda e: e.tensor_copy(out_ap, in_ap), reads=reads, writes=writes)


def setup_common(C, stack):
    nc, S = C.nc, C.S
    C.ident_f = sb(C, stack, 'ident_f', [128, 128], F32)
    C.ident_b = sb(C, stack, 'ident_b', [128, 128], BF16)
    C.eps_t = sb(C, stack, 'eps_t', [128, 1], F32)
    S.dma('sp', C.ident_f[:], C.dr['ident'][:, :], reads=[], writes=[C.ident_f], sb=C.ident_f)
    cp(C, 'dve', C.ident_b[:], C.ident_f[:], [C.ident_f], [C.ident_b])
    S.op('dve', lambda e: e.memset(C.eps_t[:], EPS), writes=[C.eps_t])
    C.pb2 = [stack.enter_context(nc.psum_tensor('pb%d' % i, [128, 1024], F32)) for i in range(4)]
    C.pb = []
    for i in range(4):
        for hh in range(2):
            C.pb.append(Buf('pbank%d' % (2 * i + hh), C.pb2[i][:, hh * 512:(hh + 1) * 512]))


def load_modT(C, dst, col, lyr, which, vec, q='sp'):
    src = C.dr['modrow'][lyr, which, vec * 1024:(vec + 1) * 1024].rearrange('(kc p) -> p kc', p=128)
    C.S.dma(q, dst[:, col:col + 8], src, reads=[dbuf(C, 'modrow')], writes=[dst], sb=dst,
            allow_slow_non_contiguous=True)


def load_bc(C, dst_ap, dst_buf, src_row_ap, reads=(), q='sp'):
    C.S.dma(q, dst_ap, src_row_ap.partition_broadcast(128), reads=list(reads), writes=[dst_buf], sb=dst_buf)


def rms_rstd(C, xt, rstd, ss, junk, n=1024, dim=1024):
    stt(C, junk, xt[0], 1.0, xt[0], ALU.mult, ALU.mult, reads=[xt[1]], writes=[ss, C.junk_b], accum=ss[:, 0:1])
    act(C, ss[:, 1:2], ss[:, 0:1], AF.Sqrt, [ss], [ss], scale=1.0 / dim, bias=C.eps_t[:, 0:1], strict=[C.eps_t])
    C.S.op('dve', lambda e: e.reciprocal(rstd[:, 0:1], ss[:, 1:2]), reads=[ss], writes=[rstd])


def norm_mod_T(C, xts, ntile, hxT, gsc, gcol, sh, scol, tpb):
    for i in range(ntile):
        xt = xts[i]
        ss = C.ss[i % 2]
        rstd = C.rstd[i % 2]
        xn = C.xn[i % 2]
        rms_rstd(C, (xt[:, :], xt), rstd, ss, C.junk_b[:, :])
        tsc(C, 'dve', xn[:, :], xt[:, :], rstd[:, 0:1], None, ALU.mult, None, [xt], [xn], strict=[rstd])
        tp = tpb[i % len(tpb)]
        tpv = tp[:, :].bitcast(BF16).rearrange('p (k t) -> p k t', k=8)
        for kc in range(8):
            tr(C, tp, tpv[:, kc, :], xn, xn[:, kc * 128:(kc + 1) * 128], C.ident_b[:])
        for kc in range(8):
            o = hxT[:, kc, i * 128:(i + 1) * 128]
            if kc % 2 == 0:
                act(C, o, tpv[:, kc, :], AF.Identity, [tp], [hxT], strict=[gsc, sh],
                    scale=gsc[:, gcol + kc:gcol + kc + 1], bias=sh[:, scol + kc:scol + kc + 1])
            else:
                tsc(C, 'dve', o, tpv[:, kc, :], gsc[:, gcol + kc:gcol + kc + 1], sh[:, scol + kc:scol + kc + 1],
                    ALU.mult, ALU.add, [tp], [hxT], strict=[gsc, sh])


def load_weight_bf16(C, dst, dst_ap_fn, src_ap_fn, nchunk, stage_bufs, q='sp', cast_eng='pool'):
    for i in range(nchunk):
        stg = stage_bufs[i % len(stage_bufs)]
        src = src_ap_fn(i)
        C.S.dma(q, stg[0](i), src, reads=[], writes=[stg[1]], sb=stg[1])
        cp(C, cast_eng, dst_ap_fn(i), stg[0](i), [stg[1]], [dst])


def mod_vectors(C, lyr, which, stack, pre):
    M = Ctx()
    M.mT = sb(C, stack, pre + 'mT', [128, 48], F32)
    for v in (0, 1, 3, 4):
        load_modT(C, M.mT, v * 8, lyr, which, v)
    M.gsc = sb(C, stack, pre + 'gsc', [128, 16], F32)
    stt(C, M.gsc[:, 0:8], M.mT[:, 8:16], 1.0, C.n1gT[:, lyr * 8:(lyr + 1) * 8], ALU.add, ALU.mult,
        [M.mT, C.n1gT], [M.gsc])
    stt(C, M.gsc[:, 8:16], M.mT[:, 32:40], 1.0, C.n2gT[:, lyr * 8:(lyr + 1) * 8], ALU.add, ALU.mult,
        [M.mT, C.n2gT], [M.gsc])
    return M


def prologue(C):
    S = C.S
    with ExitStack() as stack:
        sT = sb(C, stack, 'sT', [128, 16], F32)
        S.dma('sp', sT[:, :], C.dr['sT_in'][:, :], writes=[sT], sb=sT)
        act(C, sT[:, :], sT[:, :], AF.Silu, [sT], [sT])
        sTv = sT[:, :].rearrange('p (k w) -> p k w', w=2)
        wblk = [sb(C, stack, 'adaw%d' % i, [128, 8, 512], F32) for i in range(2)]
        brow = sb(C, stack, 'brow', [2, 6144], F32)
        mrow = sb(C, stack, 'mrow', [2, 6144], F32)
        n = 0
        for lyr in range(2):
            for w in range(2):
                S.dma('sp', brow[w:w + 1, :], C.dr['ada_b'][lyr:lyr + 1, :], writes=[brow], sb=brow)
            for cb in range(12):
                wb = wblk[n % 2]
                src = C.dr['ada_w'][lyr, :, cb * 512:(cb + 1) * 512].rearrange('(kc p) c -> p kc c', p=128)
                S.dma('sp' if n % 2 == 0 else 'pool', wb[:, :, :], src, writes=[wb], sb=wb)
                pb = C.pb[n % 2]
                mm(C, pb, pb[0:2, :], [(sTv[:, kc, :], wb[:, kc, :]) for kc in range(8)], [sT, wb])
                tt(C, 'dve', mrow[:, cb * 512:(cb + 1) * 512], pb[0:2, :], brow[:, cb * 512:(cb + 1) * 512],
                   ALU.add, [pb, brow], [mrow])
                n += 1
            S.dma('pool', C.dr['modrow'][lyr, :, :], mrow[:, :], reads=[mrow], writes=[dbuf(C, 'modrow')], sb=mrow)
        S.barrier()
        S.release(C.phase_bufs)
        C.phase_bufs = []


def phase_a(C, lyr, xname, last):
    xsrc = C.dr[xname]
    xsrc_bufs = [dbuf(C, '%s#%d' % (xname, i)) for i in range(9)]
    S = C.S
    with ExitStack() as stack:
        w_in = sb(C, stack, 'w_in_sb', [128, 8, 2048], BF16)
        stg = [sb(C, stack, 'stgA%d' % i, [128, 2048], F32) for i in range(2)]
        for kc in range(8):
            st_ = stg[kc % 2]
            S.dma('sp', st_[:, :], C.dr['w_in'][lyr, kc * 128:(kc + 1) * 128, :], writes=[st_], sb=st_)
            cp(C, 'pool', w_in[:, kc, :], st_[:, :], [st_], [w_in])
        pmat = sb(C, stack, 'pmat_sb', [128, 128], BF16)
        S.dma('sp', stg[0][:, 0:128], C.dr['pmat'][:, :], writes=[stg[0]], sb=stg[0])
        cp(C, 'dve', pmat[:, :], stg[0][:, 0:128], [stg[0]], [pmat])
        Mx = mod_vectors(C, lyr, 0, stack, 'ax')
        Mc = mod_vectors(C, lyr, 1, stack, 'ac')
        xts = [[sb(C, stack, 'xtA%d_%d' % (j, i), [128, 1024], F32) for i in range(4)] for j in range(2)]
        hxTs = [sb(C, stack, 'hxTA%d' % j, [128, 8, 512], BF16) for j in range(2)]
        cosb = [sb(C, stack, 'cosA%d' % j, [128, 512], F32) for j in range(2)]
        sinb = [sb(C, stack, 'sinA%d' % j, [128, 512], F32) for j in range(2)]
        uTs = [sb(C, stack, 'uTA%d' % j, [128, 4, 512], F32) for j in range(2)]
        qTs = [sb(C, stack, 'qTA%d' % j, [128, 4, 512], BF16) for j in range(2)]
        kTs = [sb(C, stack, 'kTA%d' % j, [128, 4, 512], BF16) for j in range(2)]
        Vts = [sb(C, stack, 'VtA%d' % j, [128, 4, 4, VW], BF16) for j in range(2)]
        raw = [sb(C, stack, 'rawA%d' % j, [128, 512], BF16) for j in range(2)]
        t1 = [sb(C, stack, 't1A%d' % j, [128, 512], F32) for j in range(2)]
        t2 = [sb(C, stack, 't2A%d' % j, [128, 512], F32) for j in range(2)]
        C.ss = [sb(C, stack, 'ssA%d' % j, [128, 2], F32) for j in range(2)]
        C.rstd = [sb(C, stack, 'rstdA%d' % j, [128, 1], F32) for j in range(2)]
        C.xn = [sb(C, stack, 'xnA%d' % j, [128, 1024], BF16) for j in range(2)]
        C.junk_b = sb(C, stack, 'junkA', [128, 1024], BF16)
        for j in range(2):
            S.op('pool', lambda e, j=j: e.memset(Vts[j][:, :, :, :], 1.0), writes=[Vts[j]])

        def loads(si):
            tok0, ntok = ST[si]
            j = si % 2
            for i in range(ntok // 128):
                S.dma('sp', xts[j][i][:, :], xsrc[tok0 + i * 128: tok0 + (i + 1) * 128, :],
                      reads=[xsrc_bufs[si]], writes=[xts[j][i]], sb=xts[j][i])
            S.dma('sp', cosb[j][:, 0:ntok], C.dr['cosT'][:, tok0:tok0 + ntok], writes=[cosb[j]], sb=cosb[j])
            S.dma('sp', sinb[j][:, 0:ntok], C.dr['sinT'][:, tok0:tok0 + ntok], writes=[sinb[j]], sb=sinb[j])

        loads(0)
        pbi = 0
        for si, (tok0, ntok) in enumerate(ST):
            if si + 1 < len(ST):
                loads(si + 1)
            j = si % 2
            ntile = ntok // 128
            isctx = si == 8
            M = Mc if isctx else Mx
            hxT = hxTs[j]
            norm_mod_T(C, xts[j], ntile, hxT, M.gsc, 0, M.mT, 0, [C.pb[6], C.pb[7]])
            uT, qT, kT, Vt = uTs[j], qTs[j], kTs[j], Vts[j]
            nkb = ntile
            for cc in range(12):
                if last and isctx and cc < 8:
                    continue
                pb = C.pb[pbi % 4]
                pbi += 1
                mm(C, pb, pb[:, 0:ntok],
                   [(w_in[:, kc, cc * 128:(cc + 1) * 128], hxT[:, kc, 0:ntok]) for kc in range(8)], [w_in, hxT])
                if cc < 4:
                    cp(C, 'act', uT[:, cc, 0:ntok], pb[:, 0:ntok], [pb], [uT])
                    continue
                h = cc % 4
                rw = raw[cc % 2]
                cp(C, 'act', rw[:, 0:ntok], pb[:, 0:ntok], [pb], [rw])
                pw = C.pb[4 + (cc % 2)]
                mm(C, pw, pw[:, 0:ntok], [(pmat[:, :], rw[:, 0:ntok])], [pmat, rw])
                a1, a2 = t1[cc % 2], t2[cc % 2]
                tt(C, 'pool', a1[:, 0:ntok], rw[:, 0:ntok], cosb[j][:, 0:ntok], ALU.mult, [rw, cosb[j]], [a1])
                tt(C, 'dve', a2[:, 0:ntok], pw[:, 0:ntok], sinb[j][:, 0:ntok], ALU.mult, [pw, sinb[j]], [a2])
                if cc < 8:
                    tt(C, 'dve', qT[:, h, 0:ntok], a1[:, 0:ntok], a2[:, 0:ntok], ALU.add, [a1, a2], [qT])
                else:
                    o = kT[:, h, 0:ntok].rearrange('d (kb p) -> d p kb', kb=nkb)
                    i1 = a1[:, 0:ntok].rearrange('d (p kb) -> d p kb', kb=nkb)
                    i2 = a2[:, 0:ntok].rearrange('d (p kb) -> d p kb', kb=nkb)
                    tt(C, 'dve', o, i1, i2, ALU.add, [a1, a2], [kT])
            for i in range(ntile):
                pb = C.pb[pbi % 4]
                pbi += 1
                mm(C, pb, pb[:, :],
                   [(hxT[:, kc, i * 128:(i + 1) * 128], w_in[:, kc, 1536:2048]) for kc in range(8)], [w_in, hxT])
                cp(C, 'act' if i % 2 == 0 else 'dve', Vt[:, i, :, 0:128],
                   pb[:, :].rearrange('p (h e) -> p h e', h=4), [pb], [Vt])
            dr = C.dr
            l = lyr
            if not (last and isctx):
                S.dma('pool', dr['uT%d' % l].rearrange('(c p) t -> p c t', p=128)[:, :, tok0:tok0 + ntok],
                      uT[:, :, 0:ntok], reads=[uT], writes=[dbuf(C, 'uT%d#%d' % (l, si))], sb=uT)
                S.dma('pool', dr['qT%d' % l].rearrange('(c p) t -> p c t', p=128)[:, :, tok0:tok0 + ntok],
                      qT[:, :, 0:ntok], reads=[qT], writes=[dbuf(C, 'qT%d#%d' % (l, si))], sb=qT)
                hx = dr['Hx%d' % l].rearrange('p (c t) -> p c t', c=4)
                if si == 0:
                    S.dma('pool', hx[:, :, 0:8], uT[:, :, 0:8], reads=[uT], writes=[dbuf(C, 'Hx%d' % l)], sb=uT)
                if si == 7:
                    S.dma('pool', hx[:, :, 8:16], uT[:, :, 504:512], reads=[uT], writes=[dbuf(C, 'Hx%d' % l)], sb=uT)
                    if C.fused:
                        S.cc(dr['Hx%d' % l][:, :], dr['Hg%d' % l][:, :], reads=[dbuf(C, 'Hx%d' % l)],
                             writes=[dbuf(C, 'Hg%d' % l)])
            if not isctx:
                kn, vn = 'Kx%d_%d' % (l, si), 'Vx%d_%d' % (l, si)
                S.dma('pool', dr[kn].rearrange('(c p) t -> p c t', p=128), kT[:, :, 0:ntok],
                      reads=[kT], writes=[dbuf(C, kn)], sb=kT)
                for i in range(ntile):
                    S.dma('pool', dr[vn].rearrange('(h t) e -> t h e', h=4)[i * 128:(i + 1) * 128, :, :],
                          Vt[:, i, :, :], reads=[Vt], writes=[dbuf(C, vn)], sb=Vt)
                if C.fused:
                    S.cc(dr[kn][:, :], dr['Kg%d_%d' % (l, si)][:, :], reads=[dbuf(C, kn)],
                         writes=[dbuf(C, 'Kg%d_%d' % (l, si))])
                    S.cc(dr[vn][:, :], dr['Vg%d_%d' % (l, si)][:, :], reads=[dbuf(C, vn)],
                         writes=[dbuf(C, 'Vg%d_%d' % (l, si))])
            else:
                S.dma('pool', dr['Kc%d' % l].rearrange('(c p) t -> p c t', p=128), kT[:, :, 0:ntok],
                      reads=[kT], writes=[dbuf(C, 'Kc%d' % l)], sb=kT)
                for i in range(ntile):
                    S.dma('pool', dr['Vc%d' % l].rearrange('(h t) e -> t h e', h=4)[i * 128:(i + 1) * 128, :, :],
                          Vt[:, i, :, :], reads=[Vt], writes=[dbuf(C, 'Vc%d' % l)], sb=Vt)
        S.barrier()
        S.release(C.phase_bufs)
        C.phase_bufs = []


def load_w_bf16_rows(C, dst, src2d, nk, width, stg, q='sp', cast='pool'):
    for kc in range(nk):
        st_ = stg[kc % len(stg)]
        C.S.dma(q, st_[:, 0:width], src2d[kc * 128:(kc + 1) * 128, :], writes=[st_], sb=st_)
        cp(C, cast, dst[:, kc, :], st_[:, 0:width], [st_], [dst])


def phase_b(C, lyr, xname, last):
    S = C.S
    l = lyr
    dr = C.dr
    lam_init = 0.8 - 0.6 * math.exp(-0.3 * lyr)
    sts = list(range(8)) if last else list(range(9))
    with ExitStack() as stack:
        stg = [sb(C, stack, 'stgB%d' % i, [128, 1024], F32) for i in range(2)]
        w_out = sb(C, stack, 'w_out_sb', [128, 8, 1024], BF16)
        load_w_bf16_rows(C, w_out, dr['w_out'][l], 8, 1024, stg)
        pool_w = sb(C, stack, 'pool_w_sb', [128, 4, 128], BF16)
        S.dma('sp', stg[0][:, 0:512].rearrange('p (g e) -> p g e', g=4), dr['pool_w'][l].rearrange('g c e -> c g e'),
              writes=[stg[0]], sb=stg[0])
        cp(C, 'dve', pool_w[:, :, :], stg[0][:, 0:512].rearrange('p (g e) -> p g e', g=4), [stg[0]], [pool_w])
        psT = sb(C, stack, 'psT_sb', [128, 8], F32)
        S.dma('sp', psT[:, :], dr['psT'][:, :], writes=[psT], sb=psT)
        sel = sb(C, stack, 'sel_sb', [128, 8], F32)
        S.dma('sp', sel[:, :], dr['sel'][:, :], writes=[sel], sb=sel)
        subg = sb(C, stack, 'subg', [128, 128], F32)
        load_bc(C, subg[:, :], subg, dr['subln_g'][l, :])
        tsc(C, 'dve', subg[:, :], subg[:, :], 1.0 - lam_init, None, ALU.mult, None, [subg], [subg])
        lamb = sb(C, stack, 'lamb', [128, 4, 64], F32)
        for i, nm in enumerate(['lambda_q1', 'lambda_k1', 'lambda_q2', 'lambda_k2']):
            load_bc(C, lamb[:, i, :], lamb, dr[nm][l, :])
        lsc = sb(C, stack, 'lsc', [128, 8], F32)
        ljunk = sb(C, stack, 'ljunk', [128, 64], F32)
        stt(C, ljunk[:, :], lamb[:, 0, :], 1.0, lamb[:, 1, :], ALU.mult, ALU.mult, [lamb], [ljunk, lsc], accum=lsc[:, 0:1])
        stt(C, ljunk[:, :], lamb[:, 2, :], 1.0, lamb[:, 3, :], ALU.mult, ALU.mult, [lamb], [ljunk, lsc], accum=lsc[:, 1:2])
        act(C, lsc[:, 2:4], lsc[:, 0:2], AF.Exp, [lsc], [lsc])
        tt(C, 'dve', lsc[:, 4:5], lsc[:, 2:3], lsc[:, 3:4], ALU.subtract, [lsc], [lsc])
        nlam = sb(C, stack, 'nlam', [128, 1], F32)
        tsc(C, 'dve', nlam[:, :], lsc[:, 4:5], lam_init, -1.0, ALU.add, ALU.mult, [lsc], [nlam])
        g1x = sb(C, stack, 'g1x', [128, 1024], F32)
        load_bc(C, g1x[:, :], g1x, dr['modrow'][l, 0, 2048:3072], reads=[dbuf(C, 'modrow')])
        g1c = None
        if not last:
            g1c = sb(C, stack, 'g1c', [128, 1024], F32)
            load_bc(C, g1c[:, :], g1c, dr['modrow'][l, 1, 2048:3072], reads=[dbuf(C, 'modrow')])
        hg = sb(C, stack, 'hg_sb', [128, 4, 64], F32)
        S.dma('sp', hg[:, :, :], dr['Hg%d' % l].rearrange('(r p) f -> p r f', p=128), reads=[dbuf(C, 'Hg%d' % l)],
              writes=[hg], sb=hg)
        hgv = hg[:, :, :].rearrange('p r (c t) -> p r c t', c=4)
        halo = sb(C, stack, 'halo', [128, 2, 4, 8], F32)
        for r in range(4):
            for side in range(2):
                src = hgv[:, r, :, 8:16] if side == 0 else hgv[:, r, :, 0:8]
                scol = sel[:, side * 4 + r:side * 4 + r + 1]
                if r == 0:
                    tsc(C, 'dve', halo[:, side, :, :], src, scol, None, ALU.mult, None, [hg, sel], [halo])
                else:
                    stt(C, halo[:, side, :, :], src, scol, halo[:, side, :, :], ALU.mult, ALU.add, [hg, sel, halo], [halo])
        qTt = [sb(C, stack, 'qTB%d' % j, [128, 4, 512], BF16) for j in range(2)]
        NB = 4
        kch = [sb(C, stack, 'kch%d' % j, [128, 512], BF16) for j in range(NB)]
        vch = [sb(C, stack, 'vch%d' % j, [128, 4, VW], BF16) for j in range(NB)]
        Eb = [sb(C, stack, 'Eb%d' % j, [128, 2, 512], BF16) for j in range(2)]
        attn_tm = sb(C, stack, 'attn_tm', [128, 4, 4, 128], BF16)
        catT = sb(C, stack, 'catT', [128, 8, 512], BF16)
        uTe = [sb(C, stack, 'uTe%d' % j, [128, 4, 528], F32) for j in range(2)]
        invc = [sb(C, stack, 'invc%d' % j, [128, 4, 512], F32) for j in range(2)]
        s2 = sb(C, stack, 'ps2', [128, 4, 528], F32)
        s4 = sb(C, stack, 'ps4', [128, 3, 528], F32)
        s8 = sb(C, stack, 'ps8', [128, 2, 528], F32)
        s16 = sb(C, stack, 'ps16', [128, 1, 528], F32)
        ptmp = sb(C, stack, 'ptmp', [128, 512], F32)
        pooledT = sb(C, stack, 'pooledT', [128, 4, 512], BF16)
        xts = [[sb(C, stack, 'xtB%d_%d' % (j, i), [128, 1024], F32) for i in range(4)] for j in range(2)]
        tmpo = [sb(C, stack, 'tmpo%d' % j, [128, 512], F32) for j in range(2)]
        o32 = [sb(C, stack, 'o32_%d' % j, [128, 128], F32) for j in range(2)]
        fsc = [sb(C, stack, 'fsc%d' % j, [128, 8], F32) for j in range(2)]
        fjunk = sb(C, stack, 'fjunk', [128, 128], BF16)
        xsrc = dr[xname]
        xdst = dr['xa%d' % l]
        accb = [C.pb[4], C.pb[5], C.pb[6]]
        misc = C.pb[7]

        def acc_ap(idx):
            return accb[idx // 3][:, (idx % 3) * 132:(idx % 3) * 132 + 129]

        def loads(si):
            tok0, ntok = ST[si]
            j = si % 2
            isctx = si == 8
            S.dma('sp', qTt[j][:, :, 0:ntok], dr['qT%d' % l].rearrange('(c p) t -> p c t', p=128)[:, :, tok0:tok0 + ntok],
                  reads=[dbuf(C, 'qT%d#%d' % (l, si))], writes=[qTt[j]], sb=qTt[j])
            ut = dr['uT%d' % l].rearrange('(c p) t -> p c t', p=128)
            ue = uTe[j]
            lo = 0 if (si == 0 or isctx) else 8
            hi = 0 if (si == 7 or isctx) else 8
            rd = [dbuf(C, 'uT%d#%d' % (l, si))]
            if lo:
                rd.append(dbuf(C, 'uT%d#%d' % (l, si - 1)))
            if hi:
                rd.append(dbuf(C, 'uT%d#%d' % (l, si + 1)))
            S.dma('sp', ue[:, :, 8 - lo:8 + ntok + hi], ut[:, :, tok0 - lo:tok0 + ntok + hi], reads=rd, writes=[ue], sb=ue)
            if isctx:
                S.op('pool', lambda e: e.memset(ue[:, :, 0:8], 0.0), writes=[ue])
                S.op('pool', lambda e: e.memset(ue[:, :, 8 + ntok:16 + ntok], 0.0), writes=[ue])
            else:
                if si == 0:
                    cp(C, 'pool', ue[:, :, 0:8], halo[:, 0, :, :], [halo], [ue])
                if si == 7:
                    cp(C, 'pool', ue[:, :, 8 + ntok:16 + ntok], halo[:, 1, :, :], [halo], [ue])
            S.dma('sp', invc[j][:, :, 0:ntok], dr['invcnt'][:, tok0:tok0 + ntok].partition_broadcast(128),
                  writes=[invc[j]], sb=invc[j])
            for i in range(ntok // 128):
                S.dma('sp', xts[j][i][:, :], xsrc[tok0 + i * 128:tok0 + (i + 1) * 128, :],
                      reads=[dbuf(C, '%s#%d' % (xname, si))], writes=[xts[j][i]], sb=xts[j][i])

        nchunk_issued = [0]

        def chunk_list(si):
            cl = []
            if si != 8:
                for r in range(4):
                    for jj in range(8):
                        cl.append(('lat', r, jj))
            cl.append(('ctx', 0, 0))
            return cl

        def load_chunk(h, ch):
            n = nchunk_issued[0]
            nchunk_issued[0] += 1
            kb_, vb_ = kch[n % NB], vch[n % NB]
            kind, r, jj = ch
            if kind == 'lat':
                kn, vn = 'Kg%d_%d' % (l, jj), 'Vg%d_%d' % (l, jj)
                S.dma('sp', kb_[:, :], dr[kn][r * 512 + h * 128:r * 512 + (h + 1) * 128, :], reads=[dbuf(C, kn)],
                      writes=[kb_], sb=kb_)
                S.dma('sp', vb_[:, :, :],
                      dr[vn][r * 2048 + h * 512:r * 2048 + (h + 1) * 512, :].rearrange('(p kb) e -> p kb e', kb=4),
                      reads=[dbuf(C, vn)], writes=[vb_], sb=vb_)
            else:
                S.dma('sp', kb_[:, 0:256], dr['Kc%d' % l][h * 128:(h + 1) * 128, :], reads=[dbuf(C, 'Kc%d' % l)],
                      writes=[kb_], sb=kb_)
                S.dma('sp', vb_[:, 0:2, :],
                      dr['Vc%d' % l][h * 256:(h + 1) * 256, :].rearrange('(p kb) e -> p kb e', kb=2),
                      reads=[dbuf(C, 'Vc%d' % l)], writes=[vb_], sb=vb_)
            return kb_, vb_

        loads(sts[0])
        ucount = [0]
        for sidx, si in enumerate(sts):
            if sidx + 1 < len(sts):
                loads(sts[sidx + 1])
            tok0, ntok = ST[si]
            j = si % 2
            isctx = si == 8
            ntile = ntok // 128
            qT = qTt[j]
            chunks = chunk_list(si)
            work = [(h, ci) for h in range(4) for ci in range(len(chunks))]
            loaded = {}
            PRE = 3
            for wi in range(min(PRE, len(work))):
                loaded[wi] = load_chunk(work[wi][0], chunks[work[wi][1]])
            units = []
            for wi, (h, ci) in enumerate(work):
                nkb = 4 if chunks[ci][0] == 'lat' else 2
                for kb in range(nkb):
                    units.append((wi, h, ci, kb))

            def emit_S(u):
                wi, h, ci, kb = units[u]
                kb_, vb_ = loaded[wi]
                uu = ucount[0] + u
                b0, b1 = C.pb[(uu % 2) * 2], C.pb[(uu % 2) * 2 + 1]
                o0, o1 = b0[:, 0:ntok], b1[:, 0:ntok]
                l0, l1 = kb_[0:64, kb * 128:(kb + 1) * 128], kb_[64:128, kb * 128:(kb + 1) * 128]
                r0, r1 = qT[0:64, h, 0:ntok], qT[64:128, h, 0:ntok]
                fns = [lambda e, o0=o0, l0=l0, r0=r0: e.matmul(o0, l0, r0, start=True, stop=True),
                       lambda e, o1=o1, l1=l1, r1=r1: e.matmul(o1, l1, r1, start=True, stop=True)]
                S.op('pe', fns, reads=[kb_, qT], writes=[b0, b1])

            started = {}
            emit_S(0)
            for u, (wi, h, ci, kb) in enumerate(units):
                if kb == 0 and wi + PRE < len(work) and (wi + PRE) not in loaded:
                    loaded[wi + PRE] = load_chunk(work[wi + PRE][0], chunks[work[wi + PRE][1]])
                if u + 1 < len(units):
                    emit_S(u + 1)
                uu = ucount[0] + u
                b0, b1 = C.pb[(uu % 2) * 2], C.pb[(uu % 2) * 2 + 1]
                E = Eb[uu % 2]
                pin = C.pb2[uu % 2][:, :].rearrange('p (m q) -> p m q', m=2)[:, :, 0:ntok]
                act(C, E[:, :, 0:ntok], pin, AF.Exp, [b0, b1], [E], scale=0.125)
                kb_, vb_ = loaded[wi]
                fns = []
                for m in range(2):
                    for qb in range(ntile):
                        idx = m * 4 + qb
                        bank = idx // 3
                        st = (h, bank) not in started
                        started[(h, bank)] = True
                        oa, la, ra = acc_ap(idx), E[:, m, qb * 128:(qb + 1) * 128], vb_[:, kb, 0:129]
                        fns.append(lambda e, oa=oa, la=la, ra=ra, st=st: e.matmul(
                            oa, la, ra, start=st, stop=False, skip_group_check=True))
                S.op('pe', fns, reads=[E, vb_], writes=accb)
                last_of_head = (u + 1 == len(units)) or units[u + 1][1] != h
                if last_of_head:
                    for qb in range(ntile):
                        f = fsc[qb % 2]
                        o = o32[qb % 2]
                        a0, a1 = acc_ap(qb), acc_ap(4 + qb)
                        S.op('dve', lambda e, f=f, a0=a0: e.reciprocal(f[:, 0:1], a0[:, 128:129]), reads=accb, writes=[f])
                        S.op('dve', lambda e, f=f, a1=a1: e.reciprocal(f[:, 1:2], a1[:, 128:129]), reads=accb, writes=[f])
                        tt(C, 'dve', f[:, 2:3], f[:, 1:2], nlam[:, 0:1], ALU.mult, [f, nlam], [f])
                        tsc(C, 'dve', o[:, :], a0[:, 0:128], f[:, 0:1], None, ALU.mult, None, accb, [o], strict=[f])
                        stt(C, o[:, :], a1[:, 0:128], f[:, 2:3], o[:, :], ALU.mult, ALU.add, accb + [o], [o], strict=[f])
                        stt(C, fjunk[:, :], o[:, :], 1.0, o[:, :], ALU.mult, ALU.mult, [o], [fjunk, f], accum=f[:, 3:4])
                        act(C, f[:, 4:5], f[:, 3:4], AF.Sqrt, [f], [f], scale=1.0 / 128, bias=C.eps_t[:, 0:1],
                            strict=[C.eps_t])
                        S.op('dve', lambda e, f=f: e.reciprocal(f[:, 5:6], f[:, 4:5]), reads=[f], writes=[f])
                        stt(C, attn_tm[:, qb, h, :], o[:, :], f[:, 5:6], subg[:, :], ALU.mult, ALU.mult,
                            [o, subg], [attn_tm], strict=[f])
            ucount[0] += len(units)
            mv = misc[:, :].bitcast(BF16).rearrange('p (k t) -> p k t', k=8)
            for qb in range(ntile):
                for h in range(4):
                    tr(C, misc, mv[:, h, :], attn_tm, attn_tm[:, qb, h, :], C.ident_b[:])
                cp(C, 'dve' if qb % 2 == 0 else 'act', catT[:, 4:8, qb * 128:(qb + 1) * 128], mv[:, 0:4, :], [misc], [catT])
            ue = uTe[j]
            W = ntok + 16
            tt(C, 'pool', s2[:, :, 0:W - 1], ue[:, :, 0:W - 1], ue[:, :, 1:W], ALU.add, [ue], [s2])
            tt(C, 'pool', s4[:, :, 0:W - 3], s2[:, 1:4, 0:W - 3], s2[:, 1:4, 2:W - 1], ALU.add, [s2], [s4])
            tt(C, 'pool', s8[:, :, 0:W - 7], s4[:, 1:3, 0:W - 7], s4[:, 1:3, 4:W - 3], ALU.add, [s4], [s8])
            tt(C, 'pool', s16[:, :, 0:W - 15], s8[:, 1:2, 0:W - 15], s8[:, 1:2, 8:W - 7], ALU.add, [s8], [s16])
            wsrc = [(s2, 0, 7), (s4, 0, 6), (s8, 0, 4), (s16, 0, 0)]
            for g in range(4):
                buf_, gi, off = wsrc[g]
                tt(C, 'pool', ptmp[:, 0:ntok], buf_[:, gi, off:off + ntok], invc[j][:, g, 0:ntok], ALU.mult,
                   [buf_, invc[j]], [ptmp])
                tt(C, 'pool', pooledT[:, g, 0:ntok], ptmp[:, 0:ntok], ue[:, g, 8:8 + ntok], ALU.subtract,
                   [ptmp, ue], [pooledT])
            for g in range(4):
                pbk = C.pb[g % 4]
                mm(C, pbk, pbk[:, 0:ntok], [(pool_w[:, g, :], pooledT[:, g, 0:ntok])], [pool_w, pooledT])
                tsc(C, 'dve', catT[:, g, 0:ntok], pbk[:, 0:ntok], psT[:, l * 4 + g:l * 4 + g + 1], None, ALU.mult, None,
                    [pbk, psT], [catT])
            gbc = g1c if isctx else g1x
            k = 0
            for i in range(ntile):
                xt = xts[j][i]
                for half in range(2):
                    pbk = C.pb[k % 4]
                    tm = tmpo[k % 2]
                    k += 1
                    mm(C, pbk, pbk[:, :], [(catT[:, kc, i * 128:(i + 1) * 128], w_out[:, kc, half * 512:(half + 1) * 512])
                                           for kc in range(8)], [catT, w_out])
                    tt(C, 'dve', tm[:, :], pbk[:, :], gbc[:, half * 512:(half + 1) * 512], ALU.mult, [pbk, gbc], [tm])
                    tt(C, 'dve', xt[:, half * 512:(half + 1) * 512], xt[:, half * 512:(half + 1) * 512], tm[:, :], ALU.add,
                       [xt, tm], [xt])
                S.dma('pool', xdst[tok0 + i * 128:tok0 + (i + 1) * 128, :], xt[:, :], reads=[xt],
                      writes=[dbuf(C, 'xa%d#%d' % (l, si))], sb=xt)
        S.barrier()
        S.release(C.phase_bufs)
        C.phase_bufs = []


def phase_c(C, lyr, last):
    S = C.S
    l = lyr
    dr = C.dr
    sts = list(range(8)) if last else list(range(9))
    GS = 3
    groups = [sts[i:i + GS] for i in range(0, len(sts), GS)]
    with ExitStack() as stack:
        Mx = mod_vectors(C, lyr, 0, stack, 'cx')
        Mc = mod_vectors(C, lyr, 1, stack, 'cc') if not last else None
        g2x = sb(C, stack, 'g2x', [128, 1024], F32)
        load_bc(C, g2x[:, :], g2x, dr['modrow'][l, 0, 5120:6144], reads=[dbuf(C, 'modrow')])
        g2c = None
        if not last:
            g2c = sb(C, stack, 'g2c', [128, 1024], F32)
            load_bc(C, g2c[:, :], g2c, dr['modrow'][l, 1, 5120:6144], reads=[dbuf(C, 'modrow')])
        fg = None
        if last:
            fg = sb(C, stack, 'fg', [128, 1024], F32)
            load_bc(C, fg[:, :], fg, dr['final_g'][:])
        rstg = sb(C, stack, 'rstg', [128, 8, 36], F32)
        S.dma('sp', rstg[:, :, 0:4], dr['router_coarse_w'][l].rearrange('(kc p) g -> p kc g', p=128), writes=[rstg], sb=rstg)
        S.dma('sp', rstg[:, :, 4:36], dr['router_fine_w'][l].rearrange('(kc p) g -> p kc g', p=128), writes=[rstg], sb=rstg)
        rw = sb(C, stack, 'rw', [128, 8, 36], BF16)
        cp(C, 'dve', rw[:, :, :], rstg[:, :, :], [rstg], [rw])
        rb = sb(C, stack, 'rb', [128, 36], F32)
        load_bc(C, rb[:, 0:4], rb, dr['router_coarse_b'][l, :])
        load_bc(C, rb[:, 4:36], rb, dr['router_fine_b'][l, :])
        NW = 2
        wst = [sb(C, stack, 'wst%d' % t, [128, 2048], F32) for t in range(3)]
        wg = [sb(C, stack, 'wg%d' % j, [128, 8, 256], BF16) for j in range(NW)]
        wu = [sb(C, stack, 'wu%d' % j, [128, 8, 256], BF16) for j in range(NW)]
        wd = [sb(C, stack, 'wd%d' % j, [128, 2, 1024], BF16) for j in range(NW)]
        xts = [sb(C, stack, 'xtC%d' % i, [128, 1024], F32) for i in range(4)]
        hxTs = [sb(C, stack, 'hxTC%d' % j, [128, 8, 512], BF16) for j in range(GS)]
        accs = [sb(C, stack, 'accC%d' % i, [128, 1024], F32) for i in range(4 * GS)]
        gates = [sb(C, stack, 'gates%d' % i, [128, 32], F32) for i in range(4 * GS)]
        hidT = [sb(C, stack, 'hidT%d' % j, [128, 2, 512], BF16) for j in range(2)]
        sa = [sb(C, stack, 'sa%d' % j, [128, 512], F32) for j in range(2)]
        C.ss = [sb(C, stack, 'ssC%d' % j, [128, 2], F32) for j in range(2)]
        C.rstd = [sb(C, stack, 'rstdC%d' % j, [128, 1], F32) for j in range(2)]
        C.xn = [sb(C, stack, 'xnC%d' % j, [128, 1024], BF16) for j in range(2)]
        C.junk_b = sb(C, stack, 'junkC', [128, 1024], BF16)
        lg = sb(C, stack, 'lg', [128, 36], F32)
        rs = sb(C, stack, 'rsc', [128, 16], F32)
        lfs = sb(C, stack, 'lfs', [128, 8], F32)
        top8 = sb(C, stack, 'top8', [128, 8], F32)
        g8 = sb(C, stack, 'g8', [128, 8], F32)
        g8b = sb(C, stack, 'g8b', [128, 8], F32)
        mk = sb(C, stack, 'mk', [128, 4], F32)
        e4 = sb(C, stack, 'e4', [128, 4], F32)
        xsrc = dr['xa%d' % l]
        xdst = dr['out'] if last else dr['xm%d' % l]
        nx = [0]

        def load_x(si, i):
            tok0, ntok = ST[si]
            xt = xts[nx[0] % 4]
            nx[0] += 1
            S.dma('sp', xt[:, :], xsrc[tok0 + i * 128:tok0 + (i + 1) * 128, :],
                  reads=[dbuf(C, 'xa%d#%d' % (l, si))], writes=[xt], sb=xt)
            return xt

        nw = [0]

        def load_expert(e):
            n = nw[0]
            nw[0] += 1
            jn = n % NW
            g_, e_ = e // 8, e % 8
            st0, st1, st2 = wst
            S.dma('sp', st0[:, :].rearrange('p (kc f) -> p kc f', kc=8),
                  dr['w_gate'][l, g_, e_].rearrange('(kc p) f -> p kc f', p=128), writes=[st0], sb=st0)
            S.dma('sp', st1[:, :].rearrange('p (kc f) -> p kc f', kc=8),
                  dr['w_up'][l, g_, e_].rearrange('(kc p) f -> p kc f', p=128), writes=[st1], sb=st1)
            S.dma('sp', st2[:, :].rearrange('p (fc d) -> p fc d', fc=2),
                  dr['w_down'][l, g_, e_].rearrange('(fc p) d -> p fc d', p=128), writes=[st2], sb=st2)
            cp(C, 'act', wg[jn][:, :, :], st0[:, :].rearrange('p (kc f) -> p kc f', kc=8), [st0], [wg[jn]])
            cp(C, 'pool', wu[jn][:, :, :], st1[:, :].rearrange('p (kc f) -> p kc f', kc=8), [st1], [wu[jn]])
            cp(C, 'act', wd[jn][:, :, :], st2[:, :].rearrange('p (fc d) -> p fc d', fc=2), [st2], [wd[jn]])
            return wg[jn], wu[jn], wd[jn]

        hcount = 0
        ocount = 0
        for grp in groups:
            pend = load_expert(0)
            for gi, si in enumerate(grp):
                tok0, ntok = ST[si]
                isctx = si == 8
                ntile = ntok // 128
                M = Mc if isctx else Mx
                hxT = hxTs[gi]
                xl = [load_x(si, i) for i in range(ntile)]
                norm_mod_T(C, xl, ntile, hxT, M.gsc, 8, M.mT, 24, [C.pb[6], C.pb[7]])
                for i in range(ntile):
                    gt = gates[gi * 4 + i]
                    pr = C.pb[6 + (i % 2)]
                    mm(C, pr, pr[:, 0:36], [(hxT[:, kc, i * 128:(i + 1) * 128], rw[:, kc, :]) for kc in range(8)], [hxT, rw])
                    tt(C, 'dve', lg[:, :], pr[:, 0:36], rb[:, :], ALU.add, [pr, rb], [lg])
                    S.op('dve', lambda e: e.tensor_reduce(rs[:, 0:1], lg[:, 0:4], mybir.AxisListType.X, ALU.max),
                         reads=[lg], writes=[rs])
                    tsc(C, 'dve', mk[:, :], lg[:, 0:4], rs[:, 0:1], None, ALU.is_equal, None, [lg], [mk], strict=[rs])
                    tsc(C, 'dve', rs[:, 1:2], rs[:, 0:1], -1.0, None, ALU.mult, None, [rs], [rs])
                    act(C, e4[:, :], lg[:, 0:4], AF.Exp, [lg, rs], [e4, rs], bias=rs[:, 1:2], accum=rs[:, 2:3])
                    S.op('dve', lambda e: e.reciprocal(rs[:, 3:4], rs[:, 2:3]), reads=[rs], writes=[rs])
                    for g in range(4):
                        src = lg[:, 4 + 8 * g:12 + 8 * g]
                        if g == 0:
                            tsc(C, 'dve', lfs[:, :], src, mk[:, 0:1], None, ALU.mult, None, [lg], [lfs], strict=[mk])
                        else:
                            stt(C, lfs[:, :], src, mk[:, g:g + 1], lfs[:, :], ALU.mult, ALU.add, [lg, lfs], [lfs], strict=[mk])
                    S.op('dve', lambda e: e.max(top8[:, :], lfs[:, :]), reads=[lfs], writes=[top8])
                    tt(C, 'dve', rs[:, 4:5], top8[:, 1:2], top8[:, 0:1], ALU.subtract, [top8], [rs])
                    act(C, rs[:, 5:6], rs[:, 4:5], AF.Exp, [rs], [rs])
                    tsc(C, 'dve', rs[:, 6:7], rs[:, 5:6], 1.0, None, ALU.add, None, [rs], [rs])
                    S.op('dve', lambda e: e.reciprocal(rs[:, 7:8], rs[:, 6:7]), reads=[rs], writes=[rs])
                    tt(C, 'dve', rs[:, 8:9], rs[:, 7:8], rs[:, 3:4], ALU.mult, [rs], [rs])
                    tt(C, 'dve', rs[:, 9:10], rs[:, 8:9], rs[:, 5:6], ALU.mult, [rs], [rs])
                    tsc(C, 'dve', g8[:, :], lfs[:, :], top8[:, 0:1], rs[:, 8:9], ALU.is_equal, ALU.mult, [lfs], [g8],
                        strict=[top8, rs])
                    tsc(C, 'dve', g8b[:, :], lfs[:, :], top8[:, 1:2], rs[:, 9:10], ALU.is_equal, ALU.mult, [lfs], [g8b],
                        strict=[top8, rs])
                    tt(C, 'dve', g8[:, :], g8[:, :], g8b[:, :], ALU.add, [g8, g8b], [g8])
                    for g in range(4):
                        tsc(C, 'dve', gt[:, 8 * g:8 * g + 8], g8[:, :], mk[:, g:g + 1], None, ALU.mult, None,
                            [g8], [gt], strict=[mk])
            for e in range(32):
                wg_, wu_, wd_ = pend
                if e + 1 < 32:
                    pend = load_expert(e + 1)
                for gi, si in enumerate(grp):
                    tok0, ntok = ST[si]
                    ntile = ntok // 128
                    hxT = hxTs[gi]
                    hT = hidT[hcount % 2]
                    hcount += 1
                    for fc in range(2):
                        pa, pbb = C.pb[fc], C.pb[2 + fc]
                        mm(C, pa, pa[:, 0:ntok], [(wg_[:, kc, fc * 128:(fc + 1) * 128], hxT[:, kc, 0:ntok]) for kc in range(8)],
                           [wg_, hxT])
                        mm(C, pbb, pbb[:, 0:ntok], [(wu_[:, kc, fc * 128:(fc + 1) * 128], hxT[:, kc, 0:ntok]) for kc in range(8)],
                           [wu_, hxT])
                        sa_ = sa[fc]
                        act(C, sa_[:, 0:ntok], pa[:, 0:ntok], AF.Silu, [pa], [sa_])
                        tt(C, 'dve', hT[:, fc, 0:ntok], sa_[:, 0:ntok], pbb[:, 0:ntok], ALU.mult, [sa_, pbb], [hT])
                    for i in range(ntile):
                        ac = accs[gi * 4 + i]
                        gt = gates[gi * 4 + i]
                        for half in range(2):
                            po = C.pb[4 + (ocount % 2)]
                            ocount += 1
                            mm(C, po, po[:, :], [(hT[:, fc, i * 128:(i + 1) * 128], wd_[:, fc, half * 512:(half + 1) * 512])
                                                 for fc in range(2)], [hT, wd_])
                            a_ = ac[:, half * 512:(half + 1) * 512]
                            if e == 0:
                                tsc(C, 'dve', a_, po[:, :], gt[:, 0:1], None, ALU.mult, None, [po], [ac], strict=[gt])
                            else:
                                stt(C, a_, po[:, :], gt[:, e:e + 1], a_, ALU.mult, ALU.add, [po, ac], [ac], strict=[gt])
            for gi, si in enumerate(grp):
                tok0, ntok = ST[si]
                isctx = si == 8
                gbc = g2c if isctx else g2x
                for i in range(ntok // 128):
                    ac = accs[gi * 4 + i]
                    xt = load_x(si, i)
                    tt(C, 'pool', ac[:, :], ac[:, :], gbc[:, :], ALU.mult, [ac, gbc], [ac])
                    tt(C, 'pool', xt[:, :], xt[:, :], ac[:, :], ALU.add, [xt, ac], [xt])
                    if last:
                        ss = C.ss[i % 2]
                        rstd = C.rstd[i % 2]
                        rms_rstd(C, (xt[:, :], xt), rstd, ss, C.junk_b[:, :])
                        stt(C, xt[:, :], xt[:, :], rstd[:, 0:1], fg[:, :], ALU.mult, ALU.mult, [xt, fg], [xt], strict=[rstd])
                        S.dma('pool', xdst[tok0 + i * 128:tok0 + (i + 1) * 128, :], xt[:, :], reads=[xt],
                              writes=[dbuf(C, 'out')], sb=xt)
                    else:
                        S.dma('pool', xdst[tok0 + i * 128:tok0 + (i + 1) * 128, :], xt[:, :], reads=[xt],
                              writes=[dbuf(C, 'xm%d#%d' % (l, si))], sb=xt)
        S.barrier()
        S.release(C.phase_bufs)
        C.phase_bufs = []


TAGGED = {'ada_w', 'w_in', 'w_out', 'w_gate', 'w_up', 'w_down'}
TAGN = 64


def declare(C, name, shape, dt, role):
    kind = {'in': 'ExternalInput', 'out': 'ExternalOutput', 'tmp': 'Internal'}[role]
    if name in TAGGED:
        n = 1
        for d_ in shape:
            n *= d_
        flat = C.nc.dram_tensor(name, [n + TAGN], dt, kind=kind).ap()
        letters = 'abcdefg'[:len(shape)]
        pat = '(%s) -> %s' % (' '.join(letters), ' '.join(letters))
        C.dr[name] = flat[0:n].rearrange(pat, **{letters[i]: shape[i] for i in range(1, len(shape))})
        return
    if role == 'tmp' and name.startswith(('Kg', 'Vg', 'Hg')):
        C.dr[name] = C.nc.dram_tensor(name, list(shape), dt, kind=kind, addr_space='Local').ap()
    else:
        C.dr[name] = C.nc.dram_tensor(name, list(shape), dt, kind=kind).ap()


CONST_IN = [('ident', [128, 128]), ('pmat', [128, 128]), ('cosT', [128, NT]), ('sinT', [128, NT]),
            ('n1gT', [128, 16]), ('n2gT', [128, 16]), ('psT', [128, 8]), ('sel', [128, 8]), ('invcnt', [4, NT])]
PARAM_IN = [('w_in', [2, D, 2048]), ('w_out', [2, D, D]), ('pool_w', [2, 4, 128, 128]), ('subln_g', [2, 128]),
            ('lambda_q1', [2, 64]), ('lambda_k1', [2, 64]), ('lambda_q2', [2, 64]), ('lambda_k2', [2, 64]),
            ('router_coarse_w', [2, D, 4]), ('router_coarse_b', [2, 4]), ('router_fine_w', [2, D, 32]),
            ('router_fine_b', [2, 32]), ('w_gate', [2, 4, 8, D, 256]), ('w_up', [2, 4, 8, D, 256]),
            ('w_down', [2, 4, 8, 256, D]), ('final_g', [D])]


_ALLP = [nm for nm, _ in PARAM_IN]
MODE_PARAMS = {0: set(_ALLP), 1: {'w_in'}, 2: set(_ALLP) - {'final_g'}, 3: set(_ALLP) - {'w_in'}}


def layer_tensors(l):
    t = [('uT%d' % l, [512, NT], F32), ('qT%d' % l, [512, NT], BF16), ('Hx%d' % l, [128, 64], F32),
         ('Kc%d' % l, [512, NCTX], BF16), ('Vc%d' % l, [4 * NCTX, VW], BF16)]
    for s in range(8):
        t.append(('Kx%d_%d' % (l, s), [512, 512], BF16))
        t.append(('Vx%d_%d' % (l, s), [2048, VW], BF16))
    return t


def gathered_tensors(l):
    t = [('Hg%d' % l, [4 * 128, 64], F32)]
    for s in range(8):
        t.append(('Kg%d_%d' % (l, s), [4 * 512, 512], BF16))
        t.append(('Vg%d_%d' % (l, s), [4 * 2048, VW], BF16))
    return t


def build(mode):
    nc = bass.Bass("TRN2", target_bir_lowering=False)
    C = Ctx()
    C.nc = nc
    C.dr = {}
    C.db = {}
    C.fused = mode == 0
    with ExitStack() as stack:
        C.S = Sched(nc, stack)
        for nm, shp in CONST_IN:
            declare(C, nm, shp, F32, 'in')
        for nm, shp in PARAM_IN:
            if nm in MODE_PARAMS[mode]:
                declare(C, nm, shp, F32, 'in')
        if mode in (0, 1):
            declare(C, 'x_in', [NT, D], F32, 'in')
            declare(C, 'sT_in', [128, 16], F32, 'in')
            declare(C, 'ada_w', [2, D, 6 * D], F32, 'in')
            declare(C, 'ada_b', [2, 6 * D], F32, 'in')
        declare(C, 'modrow', [2, 2, 6 * D], F32, {0: 'tmp', 1: 'out', 2: 'in', 3: 'in'}[mode])
        for l in range(2):
            prod = 1 if l == 0 else 2
            cons = prod + 1
            for nm, shp, dt in layer_tensors(l):
                if mode == 0:
                    declare(C, nm, shp, dt, 'tmp')
                elif mode == prod:
                    declare(C, nm, shp, dt, 'out')
                elif mode == cons and not nm.startswith(('Kx', 'Vx', 'Hx')):
                    declare(C, nm, shp, dt, 'in')
            for nm, shp, dt in gathered_tensors(l):
                if mode == 0:
                    declare(C, nm, shp, dt, 'tmp')
                elif mode == cons:
                    declare(C, nm, shp, dt, 'in')
        if mode == 0:
            for nm in ['xa0', 'xm0', 'xa1']:
                declare(C, nm, [NT, D], F32, 'tmp')
            declare(C, 'out', [NLAT, D], F32, 'out')
        elif mode == 2:
            declare(C, 'x_in', [NT, D], F32, 'in')
            declare(C, 'xa0', [NT, D], F32, 'out')
            declare(C, 'xm0', [NT, D], F32, 'out')
        elif mode == 3:
            declare(C, 'xm0', [NT, D], F32, 'in')
            declare(C, 'xa1', [NT, D], F32, 'tmp')
            declare(C, 'out', [NLAT, D], F32, 'out')
        setup_common(C, stack)
        C.n1gT = sb(C, stack, 'n1gT_sb', [128, 16], F32)
        C.n2gT = sb(C, stack, 'n2gT_sb', [128, 16], F32)
        C.S.dma('sp', C.n1gT[:, :], C.dr['n1gT'][:, :], writes=[C.n1gT], sb=C.n1gT)
        C.S.dma('sp', C.n2gT[:, :], C.dr['n2gT'][:, :], writes=[C.n2gT], sb=C.n2gT)
        C.phase_bufs = []
        if mode in (0, 1):
            prologue(C)
            phase_a(C, 0, 'x_in', last=False)
        if mode in (0, 2):
            phase_b(C, 0, 'x_in', last=False)
            phase_c(C, 0, last=False)
            phase_a(C, 1, 'xm0', last=True)
        if mode in (0, 3):
            phase_b(C, 1, 'xm0', last=True)
            phase_c(C, 1, last=True)
        C.S.barrier(['sp'])
        C.S.replay()
    return nc


def rope_tables_T(core):
    qq = core % 4
    tok = np.arange(NLAT) + qq * NLAT
    row = (tok // 64).astype(np.float32)
    col = (tok % 64).astype(np.float32)
    inv = (10000.0 ** (-np.arange(16, dtype=np.float32) / 16)).astype(np.float32)
    cosT = np.ones((64, NT), np.float32)
    sinT = np.zeros((64, NT), np.float32)
    for d in range(64):
        f = d % 16
        pos = row if d < 32 else col
        ang = (pos * inv[f]).astype(np.float32)
        sgn = -1.0 if (d % 32) < 16 else 1.0
        cosT[d, :NLAT] = np.cos(ang)
        sinT[d, :NLAT] = sgn * np.sin(ang)
    return np.tile(cosT, (2, 1)), np.tile(sinT, (2, 1))


def pmat_const():
    p = np.zeros((128, 128), np.float32)
    for i in range(128):
        partner = i + 16 if (i % 32) < 16 else i - 16
        p[partner, i] = 1.0
    return p


def invcnt_const(core):
    qq = core % 4
    out = np.zeros((4, NT), np.float32)
    for g, w in enumerate((2, 4, 8, 16)):
        left = w // 2
        right = w - 1 - left
        t = np.arange(NLAT) + qq * NLAT
        lo = np.clip(t - left, 0, L)
        hi = np.clip(t + right + 1, 0, L)
        out[g, :NLAT] = 1.0 / (hi - lo)
        t = np.arange(NCTX)
        lo = np.clip(t - left, 0, NCTX)
        hi = np.clip(t + right + 1, 0, NCTX)
        out[g, NLAT:] = 1.0 / (hi - lo)
    return out


def const_inputs(core, inp):
    cosT, sinT = rope_tables_T(core)
    qq = core % 4
    sel = np.zeros((128, 8), np.float32)
    if qq > 0:
        sel[:, qq - 1] = 1.0
    if qq < 3:
        sel[:, 4 + qq + 1] = 1.0
    m = {
        'ident': np.eye(128, dtype=np.float32),
        'pmat': pmat_const(),
        'cosT': cosT, 'sinT': sinT,
        'n1gT': np.ascontiguousarray(inp['norm1_g'].reshape(2, 8, 128).transpose(2, 0, 1).reshape(128, 16)),
        'n2gT': np.ascontiguousarray(inp['norm2_g'].reshape(2, 8, 128).transpose(2, 0, 1).reshape(128, 16)),
        'psT': np.ascontiguousarray(inp['pool_scale'].reshape(2, 4, 128).transpose(2, 0, 1).reshape(128, 8)),
        'sel': sel,
        'invcnt': invcnt_const(core),
    }
    return m


def first_inputs(core, inp):
    b, qq = core // 4, core % 4
    m = {}
    m['x_in'] = np.ascontiguousarray(np.concatenate([inp['x'][b, qq * NLAT:(qq + 1) * NLAT], inp['ctx'][b]], 0))
    s = np.stack([inp['c'][b], inp['c_ctx']], -1)
    m['sT_in'] = np.ascontiguousarray(s.reshape(8, 128, 2).transpose(1, 0, 2).reshape(128, 16))
    m['ada_w'] = tagged(inp['ada_w'], core)
    m['ada_b'] = inp['ada_b']
    return m


def gather_host(results, l):
    outs = []
    for c in range(NCORES):
        grp = [4 * (c // 4) + r for r in range(4)]
        m = {'Hg%d' % l: np.concatenate([results[r]['Hx%d' % l] for r in grp], 0)}
        for s in range(8):
            m['Kg%d_%d' % (l, s)] = np.concatenate([results[r]['Kx%d_%d' % (l, s)] for r in grp], 0)
            m['Vg%d_%d' % (l, s)] = np.concatenate([results[r]['Vx%d_%d' % (l, s)] for r in grp], 0)
        outs.append(m)
    return outs


_NC_CACHE = {}


def get_nc(mode):
    if mode not in _NC_CACHE:
        _NC_CACHE[mode] = build(mode)
    return _NC_CACHE[mode]


def tagged(a, core):
    return np.concatenate([np.asarray(a, np.float32).ravel(), np.full(TAGN, float(core), np.float32)])


def params_for(mode, inp, core=0):
    return {nm: (tagged(inp[nm], core) if nm in TAGGED else inp[nm]) for nm in _ALLP if nm in MODE_PARAMS[mode]}


def run_multi(inp, cores=None):
    consts = [const_inputs(c, inp) for c in range(NCORES)]
    ids = list(range(NCORES)) if cores is None else cores
    in1 = [dict(consts[c], **first_inputs(c, inp), **params_for(1, inp, c)) for c in ids]
    r1 = run_bass_kernel_spmd(get_nc(1), in1, core_ids=ids).results
    g0 = gather_host(r1, 0)
    in2 = []
    for c in ids:
        m = dict(consts[c], **params_for(2, inp, c))
        m['x_in'] = in1[c]['x_in']
        m['modrow'] = r1[c]['modrow']
        for nm in ['uT0', 'qT0', 'Kc0', 'Vc0']:
            m[nm] = r1[c][nm]
        m.update(g0[c])
        in2.append(m)
    r2 = run_bass_kernel_spmd(get_nc(2), in2, core_ids=ids).results
    g1 = gather_host(r2, 1)
    in3 = []
    for c in ids:
        m = dict(consts[c], **params_for(3, inp, c))
        m['modrow'] = r1[c]['modrow']
        m['xm0'] = r2[c]['xm0']
        for nm in ['uT1', 'qT1', 'Kc1', 'Vc1']:
            m[nm] = r2[c][nm]
        m.update(g1[c])
        in3.append(m)
    r3 = run_bass_kernel_spmd(get_nc(3), in3, core_ids=ids).results
    return r3


def run_fused(inp):
    ids = list(range(NCORES))
    ins = [dict(const_inputs(c, inp), **first_inputs(c, inp), **params_for(0, inp, c)) for c in ids]
    return run_bass_kernel_spmd(get_nc(0), ins, core_ids=ids).results


FUSED = True


def kernel(**inputs):
    inp = {k: np.ascontiguousarray(np.asarray(v, dtype=np.float32)) for k, v in inputs.items()}
    res = run_fused(inp) if FUSED else run_multi(inp)
    out = np.zeros((2, L, D), np.float32)
    for c in range(NCORES):
        b, qq = c // 4, c % 4
        out[b, qq * NLAT:(qq + 1) * NLAT] = np.asarray(res[c]['out'], dtype=np.float32)
    return out
```

```python
import math
from contextlib import ExitStack
import numpy as np
import ml_dtypes
import concourse.bass as bass
import concourse.mybir as mybir
from concourse.bass_utils import run_bass_kernel_spmd

F32 = mybir.dt.float32
BF16 = mybir.dt.bfloat16
AF = mybir.ActivationFunctionType
ALU = mybir.AluOpType

NCORES = 8
D = 1024
L = 16384
NLAT = 4096
NCTX = 256
NT = NLAT + NCTX
VW = 132
EPS = 1e-6
ST = [(s * 512, 512) for s in range(8)] + [(NLAT, NCTX)]
ENGS = ['sp', 'pe', 'act', 'dve', 'pool']


class Buf:
    def __init__(self, name, t=None):
        self.name = name
        self.t = t
        self.w = None
        self.r = {}
        self.dkey = None
        self.small = False

    def __getitem__(self, idx):
        return self.t[idx]


class Sched:
    def __init__(self, nc, stack):
        self.nc = nc
        self.stack = stack
        self.ops = {e: [] for e in ENGS}
        self.sem = {}
        self.cnt = {}
        self.isdma = {}
        self.waited = {e: {} for e in ENGS}
        self.nsem = 0
        self.free_d = []
        for e in ['pe', 'act', 'dve', 'pool']:
            self.newsem(e, False)

    def newsem(self, key, isdma):
        self.nsem += 1
        self.sem[key] = self.stack.enter_context(self.nc.semaphore('s%d' % self.nsem))
        self.cnt[key] = 0
        self.isdma[key] = isdma

    def _deps(self, reads, writes):
        deps = {}

        def add(k, v):
            if deps.get(k, 0) < v:
                deps[k] = v
        for b in reads:
            if b.w is not None:
                add(*b.w)
        for b in writes:
            if b.w is not None:
                add(*b.w)
            for k, v in b.r.items():
                add(k, v)
        return deps

    def _wait(self, eng, deps, strict=()):
        own = 0
        for b in strict:
            if b.w is not None and b.w[0] == eng:
                own = max(own, b.w[1])
        for k, v in deps.items():
            if k == eng:
                if own == 0:
                    continue
                v = own
            if self.isdma[k]:
                v = self.cnt[k]
            if self.waited[eng].get(k, 0) >= v:
                continue
            self.waited[eng][k] = v
            sem = self.sem[k]
            self.ops[eng].append(lambda e, sem=sem, v=v: e.wait_ge(sem, v))

    def _commit(self, key, v, reads, writes):
        for b in writes:
            b.w = (key, v)
            b.r = {}
        for b in reads:
            if b.r.get(key, 0) < v:
                b.r[key] = v

    def op(self, eng, fns, reads=(), writes=(), strict=()):
        if not isinstance(fns, (list, tuple)):
            fns = [fns]
        strict = list(strict) + [b for b in reads if b.small]
        self._wait(eng, self._deps(list(reads) + list(strict), writes), strict)
        self.cnt[eng] += 1
        v = self.cnt[eng]
        sem = self.sem[eng]
        for f in fns[:-1]:
            self.ops[eng].append(f)
        last = fns[-1]
        self.ops[eng].append(lambda e, last=last, sem=sem: last(e).then_inc(sem, 1))
        self._commit(eng, v, reads, writes)

    def dma(self, q, out, in_, reads=(), writes=(), sb=None, **kw):
        if sb.dkey is None:
            if self.free_d:
                sb.dkey = self.free_d.pop()
            else:
                sb.dkey = ('d', len(self.sem))
                self.newsem(sb.dkey, True)
        key = sb.dkey
        self._wait(q, self._deps(reads, writes))
        self.cnt[key] += 16
        v = self.cnt[key]
        sem = self.sem[key]
        self.ops[q].append(lambda e: e.dma_start(out=out, in_=in_, **kw).then_inc(sem, 16))
        self._commit(key, v, reads, writes)

    def cc(self, ins_ap, outs_ap, reads=(), writes=()):
        key = 'cc'
        if key not in self.sem:
            self.newsem(key, True)
        self._wait('pool', self._deps(reads, writes))
        self.cnt[key] += 1
        v = self.cnt[key]
        sem = self.sem[key]
        self.ops['pool'].append(lambda e: e.collective_compute(
            "AllGather", ALU.bypass, replica_groups=[[0, 1, 2, 3], [4, 5, 6, 7]],
            ins=[ins_ap], outs=[outs_ap]).then_inc(sem, 1))
        self._commit(key, v, reads, writes)

    def release(self, bufs):
        for b in bufs:
            if b.dkey is not None:
                self.free_d.append(b.dkey)
                b.dkey = None

    def barrier(self, engines=ENGS):
        for e in engines:
            for k in self.sem:
                if k == e or self.cnt[k] == 0:
                    continue
                v = self.cnt[k]
                if self.waited[e].get(k, 0) >= v:
                    continue
                self.waited[e][k] = v
                sem = self.sem[k]
                self.ops[e].append(lambda en, sem=sem, v=v: en.wait_ge(sem, v))

    def replay(self):
        nc = self.nc
        with nc.Block() as block:
            @block.sync
            def _(e):
                for f in self.ops['sp']:
                    f(e)

            @block.tensor
            def _(e):
                for f in self.ops['pe']:
                    f(e)

            @block.scalar
            def _(e):
                for f in self.ops['act']:
                    f(e)

            @block.vector
            def _(e):
                for f in self.ops['dve']:
                    f(e)

            @block.gpsimd
            def _(e):
                for f in self.ops['pool']:
                    f(e)


class Ctx:
    pass


def dbuf(C, name):
    if name not in C.db:
        C.db[name] = Buf(name)
    return C.db[name]


def sb(C, stack, name, shape, dt):
    if not hasattr(C, 'names'):
        C.names = {}
    k = C.names.get(name, 0)
    C.names[name] = k + 1
    if k:
        name = '%s_r%d' % (name, k)
    t = stack.enter_context(C.nc.sbuf_tensor(name, shape, dt))
    b = Buf(name, t)
    if getattr(C, 'phase_bufs', None) is not None:
        C.phase_bufs.append(b)
    fs = 1
    for d_ in shape[1:]:
        fs *= d_
    b.small = fs < 256
    return b


def ps(C, stack, name, shape, dt):
    t = stack.enter_context(C.nc.psum_tensor(name, shape, dt))
    return Buf(name, t)


def mm(C, out_buf, out_ap, pairs, reads, start=True, stop=True, skip=False):
    fns = []
    n = len(pairs)
    for i, (l_ap, r_ap) in enumerate(pairs):
        st = start and i == 0
        sp_ = stop and i == n - 1
        fns.append(lambda e, l_ap=l_ap, r_ap=r_ap, st=st, sp_=sp_: e.matmul(
            out_ap, l_ap, r_ap, start=st, stop=sp_, skip_group_check=skip))
    C.S.op('pe', fns, reads=reads, writes=[out_buf])


def tr(C, out_buf, out_ap, in_buf, in_ap, ident_ap, extra_reads=()):
    C.S.op('pe', lambda e: e.transpose(out_ap, in_ap, ident_ap),
           reads=[in_buf, C.ident_b] + list(extra_reads), writes=[out_buf])


def act(C, out_ap, in_ap, func, reads, writes, scale=None, bias=None, accum=None, strict=()):
    kw = {}
    if scale is not None:
        kw['scale'] = scale
    if bias is not None:
        kw['bias'] = bias
    if accum is not None:
        kw['accum_out'] = accum
    C.S.op('act', lambda e: e.activation(out_ap, in_ap, func, **kw), reads=reads, writes=writes, strict=strict)


def tsc(C, eng, out_ap, in_ap, s1, s2, op0, op1, reads, writes, accum=None, strict=()):
    if op1 is None:
        C.S.op(eng, lambda e: e.tensor_scalar(out_ap, in_ap, s1, None, op0), reads=reads, writes=writes, strict=strict)
    else:
        C.S.op(eng, lambda e: e.tensor_scalar(out_ap, in_ap, s1, s2, op0, op1), reads=reads, writes=writes, strict=strict)


def stt(C, out_ap, in0, scalar, in1, op0, op1, reads, writes, accum=None, strict=()):
    if accum is None:
        C.S.op('dve', lambda e: e.scalar_tensor_tensor(out_ap, in0, scalar, in1, op0, op1),
               reads=reads, writes=writes, strict=strict)
    else:
        C.S.op('dve', lambda e: e.scalar_tensor_tensor(out_ap, in0, scalar, in1, op0, op1, accum_out=accum),
               reads=reads, writes=writes, strict=strict)


def tt(C, eng, out_ap, in0, in1, op, reads, writes):
    C.S.op(eng, lambda e: e.tensor_tensor(out_ap, in0, in1, op), reads=reads, writes=writes)


def cp(C, eng, out_ap, in_ap, reads, writes):
    if eng == 'act':
        C.S.op('act', lambda e: e.activation(out_ap, in_ap, AF.Copy), reads=reads, writes=writes)
    else:
        C.S.op(eng, lambda e: e.tensor_copy(out_ap, in_ap), reads=reads, writes=writes)


def setup_common(C, stack):
    nc, S = C.nc, C.S
    C.ident_f = sb(C, stack, 'ident_f', [128, 128], F32)
    C.ident_b = sb(C, stack, 'ident_b', [128, 128], BF16)
    C.eps_t = sb(C, stack, 'eps_t', [128, 1], F32)
    S.dma('sp', C.ident_f[:], C.dr['ident'][:, :], reads=[], writes=[C.ident_f], sb=C.ident_f)
    cp(C, 'dve', C.ident_b[:], C.ident_f[:], [C.ident_f], [C.ident_b])
    S.op('dve', lambda e: e.memset(C.eps_t[:], EPS), writes=[C.eps_t])
    C.pb2 = [stack.enter_context(nc.psum_tensor('pb%d' % i, [128, 1024], F32)) for i in range(4)]
    C.pb = []
    for i in range(4):
        for hh in range(2):
            C.pb.append(Buf('pbank%d' % (2 * i + hh), C.pb2[i][:, hh * 512:(hh + 1) * 512]))


def load_modT(C, dst, col, lyr, which, vec, q='sp'):
    src = C.dr['modrow'][lyr, which, vec * 1024:(vec + 1) * 1024].rearrange('(kc p) -> p kc', p=128)
    C.S.dma(q, dst[:, col:col + 8], src, reads=[dbuf(C, 'modrow')], writes=[dst], sb=dst,
            allow_slow_non_contiguous=True)


def load_bc(C, dst_ap, dst_buf, src_row_ap, reads=(), q='sp'):
    C.S.dma(q, dst_ap, src_row_ap.partition_broadcast(128), reads=list(reads), writes=[dst_buf], sb=dst_buf)


def rms_rstd(C, xt, rstd, ss, junk, n=1024, dim=1024):
    stt(C, junk, xt[0], 1.0, xt[0], ALU.mult, ALU.mult, reads=[xt[1]], writes=[ss, C.junk_b], accum=ss[:, 0:1])
    act(C, ss[:, 1:2], ss[:, 0:1], AF.Sqrt, [ss], [ss], scale=1.0 / dim, bias=C.eps_t[:, 0:1], strict=[C.eps_t])
    C.S.op('dve', lambda e: e.reciprocal(rstd[:, 0:1], ss[:, 1:2]), reads=[ss], writes=[rstd])


def norm_mod_T(C, xts, ntile, hxT, gsc, gcol, sh, scol, tpb):
    for i in range(ntile):
        xt = xts[i]
        ss = C.ss[i % 2]
        rstd = C.rstd[i % 2]
        xn = C.xn[i % 2]
        rms_rstd(C, (xt[:, :], xt), rstd, ss, C.junk_b[:, :])
        tsc(C, 'dve', xn[:, :], xt[:, :], rstd[:, 0:1], None, ALU.mult, None, [xt], [xn], strict=[rstd])
        tp = tpb[i % len(tpb)]
        tpv = tp[:, :].bitcast(BF16).rearrange('p (k t) -> p k t', k=8)
        for kc in range(8):
            tr(C, tp, tpv[:, kc, :], xn, xn[:, kc * 128:(kc + 1) * 128], C.ident_b[:])
        for kc in range(8):
            o = hxT[:, kc, i * 128:(i + 1) * 128]
            if kc % 2 == 0:
                act(C, o, tpv[:, kc, :], AF.Identity, [tp], [hxT], strict=[gsc, sh],
                    scale=gsc[:, gcol + kc:gcol + kc + 1], bias=sh[:, scol + kc:scol + kc + 1])
            else:
                tsc(C, 'dve', o, tpv[:, kc, :], gsc[:, gcol + kc:gcol + kc + 1], sh[:, scol + kc:scol + kc + 1],
                    ALU.mult, ALU.add, [tp], [hxT], strict=[gsc, sh])


def load_weight_bf16(C, dst, dst_ap_fn, src_ap_fn, nchunk, stage_bufs, q='sp', cast_eng='pool'):
    for i in range(nchunk):
        stg = stage_bufs[i % len(stage_bufs)]
        src = src_ap_fn(i)
        C.S.dma(q, stg[0](i), src, reads=[], writes=[stg[1]], sb=stg[1])
        cp(C, cast_eng, dst_ap_fn(i), stg[0](i), [stg[1]], [dst])


def mod_vectors(C, lyr, which, stack, pre):
    M = Ctx()
    M.mT = sb(C, stack, pre + 'mT', [128, 48], F32)
    for v in (0, 1, 3, 4):
        load_modT(C, M.mT, v * 8, lyr, which, v)
    M.gsc = sb(C, stack, pre + 'gsc', [128, 16], F32)
    stt(C, M.gsc[:, 0:8], M.mT[:, 8:16], 1.0, C.n1gT[:, lyr * 8:(lyr + 1) * 8], ALU.add, ALU.mult,
        [M.mT, C.n1gT], [M.gsc])
    stt(C, M.gsc[:, 8:16], M.mT[:, 32:40], 1.0, C.n2gT[:, lyr * 8:(lyr + 1) * 8], ALU.add, ALU.mult,
        [M.mT, C.n2gT], [M.gsc])
    return M


def prologue(C):
    S = C.S
    with ExitStack() as stack:
        sT = sb(C, stack, 'sT', [128, 16], F32)
        S.dma('sp', sT[:, :], C.dr['sT_in'][:, :], writes=[sT], sb=sT)
        act(C, sT[:, :], sT[:, :], AF.Silu, [sT], [sT])
        sTv = sT[:, :].rearrange('p (k w) -> p k w', w=2)
        wblk = [sb(C, stack, 'adaw%d' % i, [128, 8, 512], F32) for i in range(2)]
        brow = sb(C, stack, 'brow', [2, 6144], F32)
        mrow = sb(C, stack, 'mrow', [2, 6144], F32)
        n = 0
        for lyr in range(2):
            for w in range(2):
                S.dma('sp', brow[w:w + 1, :], C.dr['ada_b'][lyr:lyr + 1, :], writes=[brow], sb=brow)
            for cb in range(12):
                wb = wblk[n % 2]
                src = C.dr['ada_w'][lyr, :, cb * 512:(cb + 1) * 512].rearrange('(kc p) c -> p kc c', p=128)
                S.dma('sp' if n % 2 == 0 else 'pool', wb[:, :, :], src, writes=[wb], sb=wb)
                pb = C.pb[n % 2]
                mm(C, pb, pb[0:2, :], [(sTv[:, kc, :], wb[:, kc, :]) for kc in range(8)], [sT, wb])
                tt(C, 'dve', mrow[:, cb * 512:(cb + 1) * 512], pb[0:2, :], brow[:, cb * 512:(cb + 1) * 512],
                   ALU.add, [pb, brow], [mrow])
                n += 1
            S.dma('pool', C.dr['modrow'][lyr, :, :], mrow[:, :], reads=[mrow], writes=[dbuf(C, 'modrow')], sb=mrow)
        S.barrier()
        S.release(C.phase_bufs)
        C.phase_bufs = []


def phase_a(C, lyr, xname, last):
    xsrc = C.dr[xname]
    xsrc_bufs = [dbuf(C, '%s#%d' % (xname, i)) for i in range(9)]
    S = C.S
    with ExitStack() as stack:
        w_in = sb(C, stack, 'w_in_sb', [128, 8, 2048], BF16)
        stg = [sb(C, stack, 'stgA%d' % i, [128, 2048], F32) for i in range(2)]
        for kc in range(8):
            st_ = stg[kc % 2]
            S.dma('sp', st_[:, :], C.dr['w_in'][lyr, kc * 128:(kc + 1) * 128, :], writes=[st_], sb=st_)
            cp(C, 'pool', w_in[:, kc, :], st_[:, :], [st_], [w_in])
        pmat = sb(C, stack, 'pmat_sb', [128, 128], BF16)
        S.dma('sp', stg[0][:, 0:128], C.dr['pmat'][:, :], writes=[stg[0]], sb=stg[0])
        cp(C, 'dve', pmat[:, :], stg[0][:, 0:128], [stg[0]], [pmat])
        Mx = mod_vectors(C, lyr, 0, stack, 'ax')
        Mc = mod_vectors(C, lyr, 1, stack, 'ac')
        xts = [[sb(C, stack, 'xtA%d_%d' % (j, i), [128, 1024], F32) for i in range(4)] for j in range(2)]
        hxTs = [sb(C, stack, 'hxTA%d' % j, [128, 8, 512], BF16) for j in range(2)]
        cosb = [sb(C, stack, 'cosA%d' % j, [128, 512], F32) for j in range(2)]
        sinb = [sb(C, stack, 'sinA%d' % j, [128, 512], F32) for j in range(2)]
        uTs = [sb(C, stack, 'uTA%d' % j, [128, 4, 512], F32) for j in range(2)]
        qTs = [sb(C, stack, 'qTA%d' % j, [128, 4, 512], BF16) for j in range(2)]
        kTs = [sb(C, stack, 'kTA%d' % j, [128, 4, 512], BF16) for j in range(2)]
        Vts = [sb(C, stack, 'VtA%d' % j, [128, 4, 4, VW], BF16) for j in range(2)]
        raw = [sb(C, stack, 'rawA%d' % j, [128, 512], BF16) for j in range(2)]
        t1 = [sb(C, stack, 't1A%d' % j, [128, 512], F32) for j in range(2)]
        t2 = [sb(C, stack, 't2A%d' % j, [128, 512], F32) for j in range(2)]
        C.ss = [sb(C, stack, 'ssA%d' % j, [128, 2], F32) for j in range(2)]
        C.rstd = [sb(C, stack, 'rstdA%d' % j, [128, 1], F32) for j in range(2)]
        C.xn = [sb(C, stack, 'xnA%d' % j, [128, 1024], BF16) for j in range(2)]
        C.junk_b = sb(C, stack, 'junkA', [128, 1024], BF16)
        for j in range(2):
            S.op('pool', lambda e, j=j: e.memset(Vts[j][:, :, :, :], 1.0), writes=[Vts[j]])

        def loads(si):
            tok0, ntok = ST[si]
            j = si % 2
            for i in range(ntok // 128):
                S.dma('sp', xts[j][i][:, :], xsrc[tok0 + i * 128: tok0 + (i + 1) * 128, :],
                      reads=[xsrc_bufs[si]], writes=[xts[j][i]], sb=xts[j][i])
            S.dma('sp', cosb[j][:, 0:ntok], C.dr['cosT'][:, tok0:tok0 + ntok], writes=[cosb[j]], sb=cosb[j])
            S.dma('sp', sinb[j][:, 0:ntok], C.dr['sinT'][:, tok0:tok0 + ntok], writes=[sinb[j]], sb=sinb[j])

        loads(0)
        pbi = 0
        for si, (tok0, ntok) in enumerate(ST):
            if si + 1 < len(ST):
                loads(si + 1)
            j = si % 2
            ntile = ntok // 128
            isctx = si == 8
            M = Mc if isctx else Mx
            hxT = hxTs[j]
            norm_mod_T(C, xts[j], ntile, hxT, M.gsc, 0, M.mT, 0, [C.pb[6], C.pb[7]])
            uT, qT, kT, Vt = uTs[j], qTs[j], kTs[j], Vts[j]
            nkb = ntile
            for cc in range(12):
                if last and isctx and cc < 8:
                    continue
                pb = C.pb[pbi % 4]
                pbi += 1
                mm(C, pb, pb[:, 0:ntok],
                   [(w_in[:, kc, cc * 128:(cc + 1) * 128], hxT[:, kc, 0:ntok]) for kc in range(8)], [w_in, hxT])
                if cc < 4:
                    cp(C, 'act', uT[:, cc, 0:ntok], pb[:, 0:ntok], [pb], [uT])
                    continue
                h = cc % 4
                rw = raw[cc % 2]
                cp(C, 'act', rw[:, 0:ntok], pb[:, 0:ntok], [pb], [rw])
                pw = C.pb[4 + (cc % 2)]
                mm(C, pw, pw[:, 0:ntok], [(pmat[:, :], rw[:, 0:ntok])], [pmat, rw])
                a1, a2 = t1[cc % 2], t2[cc % 2]
                tt(C, 'pool', a1[:, 0:ntok], rw[:, 0:ntok], cosb[j][:, 0:ntok], ALU.mult, [rw, cosb[j]], [a1])
                tt(C, 'dve', a2[:, 0:ntok], pw[:, 0:ntok], sinb[j][:, 0:ntok], ALU.mult, [pw, sinb[j]], [a2])
                if cc < 8:
                    tt(C, 'dve', qT[:, h, 0:ntok], a1[:, 0:ntok], a2[:, 0:ntok], ALU.add, [a1, a2], [qT])
                else:
                    o = kT[:, h, 0:ntok].rearrange('d (kb p) -> d p kb', kb=nkb)
                    i1 = a1[:, 0:ntok].rearrange('d (p kb) -> d p kb', kb=nkb)
                    i2 = a2[:, 0:ntok].rearrange('d (p kb) -> d p kb', kb=nkb)
                    tt(C, 'dve', o, i1, i2, ALU.add, [a1, a2], [kT])
            for i in range(ntile):
                pb = C.pb[pbi % 4]
                pbi += 1
                mm(C, pb, pb[:, :],
                   [(hxT[:, kc, i * 128:(i + 1) * 128], w_in[:, kc, 1536:2048]) for kc in range(8)], [w_in, hxT])
                cp(C, 'act' if i % 2 == 0 else 'dve', Vt[:, i, :, 0:128],
                   pb[:, :].rearrange('p (h e) -> p h e', h=4), [pb], [Vt])
            dr = C.dr
            l = lyr
            if not (last and isctx):
                S.dma('pool', dr['uT%d' % l].rearrange('(c p) t -> p c t', p=128)[:, :, tok0:tok0 + ntok],
                      uT[:, :, 0:ntok], reads=[uT], writes=[dbuf(C, 'uT%d#%d' % (l, si))], sb=uT)
                S.dma('pool', dr['qT%d' % l].rearrange('(c p) t -> p c t', p=128)[:, :, tok0:tok0 + ntok],
                      qT[:, :, 0:ntok], reads=[qT], writes=[dbuf(C, 'qT%d#%d' % (l, si))], sb=qT)
                hx = dr['Hx%d' % l].rearrange('p (c t) -> p c t', c=4)
                if si == 0:
                    S.dma('pool', hx[:, :, 0:8], uT[:, :, 0:8], reads=[uT], writes=[dbuf(C, 'Hx%d' % l)], sb=uT)
                if si == 7:
                    S.dma('pool', hx[:, :, 8:16], uT[:, :, 504:512], reads=[uT], writes=[dbuf(C, 'Hx%d' % l)], sb=uT)
                    if C.fused:
                        S.cc(dr['Hx%d' % l][:, :], dr['Hg%d' % l][:, :], reads=[dbuf(C, 'Hx%d' % l)],
                             writes=[dbuf(C, 'Hg%d' % l)])
            if not isctx:
                kn, vn = 'Kx%d_%d' % (l, si), 'Vx%d_%d' % (l, si)
                S.dma('pool', dr[kn].rearrange('(c p) t -> p c t', p=128), kT[:, :, 0:ntok],
                      reads=[kT], writes=[dbuf(C, kn)], sb=kT)
                for i in range(ntile):
                    S.dma('pool', dr[vn].rearrange('(h t) e -> t h e', h=4)[i * 128:(i + 1) * 128, :, :],
                          Vt[:, i, :, :], reads=[Vt], writes=[dbuf(C, vn)], sb=Vt)
                if C.fused:
                    S.cc(dr[kn][:, :], dr['Kg%d_%d' % (l, si)][:, :], reads=[dbuf(C, kn)],
                         writes=[dbuf(C, 'Kg%d_%d' % (l, si))])
                    S.cc(dr[vn][:, :], dr['Vg%d_%d' % (l, si)][:, :], reads=[dbuf(C, vn)],
                         writes=[dbuf(C, 'Vg%d_%d' % (l, si))])
            else:
                S.dma('pool', dr['Kc%d' % l].rearrange('(c p) t -> p c t', p=128), kT[:, :, 0:ntok],
                      reads=[kT], writes=[dbuf(C, 'Kc%d' % l)], sb=kT)
                for i in range(ntile):
                    S.dma('pool', dr['Vc%d' % l].rearrange('(h t) e -> t h e', h=4)[i * 128:(i + 1) * 128, :, :],
                          Vt[:, i, :, :], reads=[Vt], writes=[dbuf(C, 'Vc%d' % l)], sb=Vt)
        S.barrier()
        S.release(C.phase_bufs)
        C.phase_bufs = []


def load_w_bf16_rows(C, dst, src2d, nk, width, stg, q='sp', cast='pool'):
    for kc in range(nk):
        st_ = stg[kc % len(stg)]
        C.S.dma(q, st_[:, 0:width], src2d[kc * 128:(kc + 1) * 128, :], writes=[st_], sb=st_)
        cp(C, cast, dst[:, kc, :], st_[:, 0:width], [st_], [dst])


def phase_b(C, lyr, xname, last):
    S = C.S
    l = lyr
    dr = C.dr
    lam_init = 0.8 - 0.6 * math.exp(-0.3 * lyr)
    sts = list(range(8)) if last else list(range(9))
    with ExitStack() as stack:
        stg = [sb(C, stack, 'stgB%d' % i, [128, 1024], F32) for i in range(2)]
        w_out = sb(C, stack, 'w_out_sb', [128, 8, 1024], BF16)
        load_w_bf16_rows(C, w_out, dr['w_out'][l], 8, 1024, stg)
        pool_w = sb(C, stack, 'pool_w_sb', [128, 4, 128], BF16)
        S.dma('sp', stg[0][:, 0:512].rearrange('p (g e) -> p g e', g=4), dr['pool_w'][l].rearrange('g c e -> c g e'),
              writes=[stg[0]], sb=stg[0])
        cp(C, 'dve', pool_w[:, :, :], stg[0][:, 0:512].rearrange('p (g e) -> p g e', g=4), [stg[0]], [pool_w])
        psT = sb(C, stack, 'psT_sb', [128, 8], F32)
        S.dma('sp', psT[:, :], dr['psT'][:, :], writes=[psT], sb=psT)
        sel = sb(C, stack, 'sel_sb', [128, 8], F32)
        S.dma('sp', sel[:, :], dr['sel'][:, :], writes=[sel], sb=sel)
        subg = sb(C, stack, 'subg', [128, 128], F32)
        load_bc(C, subg[:, :], subg, dr['subln_g'][l, :])
        tsc(C, 'dve', subg[:, :], subg[:, :], 1.0 - lam_init, None, ALU.mult, None, [subg], [subg])
        lamb = sb(C, stack, 'lamb', [128, 4, 64], F32)
        for i, nm in enumerate(['lambda_q1', 'lambda_k1', 'lambda_q2', 'lambda_k2']):
            load_bc(C, lamb[:, i, :], lamb, dr[nm][l, :])
        lsc = sb(C, stack, 'lsc', [128, 8], F32)
        ljunk = sb(C, stack, 'ljunk', [128, 64], F32)
        stt(C, ljunk[:, :], lamb[:, 0, :], 1.0, lamb[:, 1, :], ALU.mult, ALU.mult, [lamb], [ljunk, lsc], accum=lsc[:, 0:1])
        stt(C, ljunk[:, :], lamb[:, 2, :], 1.0, lamb[:, 3, :], ALU.mult, ALU.mult, [lamb], [ljunk, lsc], accum=lsc[:, 1:2])
        act(C, lsc[:, 2:4], lsc[:, 0:2], AF.Exp, [lsc], [lsc])
        tt(C, 'dve', lsc[:, 4:5], lsc[:, 2:3], lsc[:, 3:4], ALU.subtract, [lsc], [lsc])
        nlam = sb(C, stack, 'nlam', [128, 1], F32)
        tsc(C, 'dve', nlam[:, :], lsc[:, 4:5], lam_init, -1.0, ALU.add, ALU.mult, [lsc], [nlam])
        g1x = sb(C, stack, 'g1x', [128, 1024], F32)
        load_bc(C, g1x[:, :], g1x, dr['modrow'][l, 0, 2048:3072], reads=[dbuf(C, 'modrow')])
        g1c = None
        if not last:
            g1c = sb(C, stack, 'g1c', [128, 1024], F32)
            load_bc(C, g1c[:, :], g1c, dr['modrow'][l, 1, 2048:3072], reads=[dbuf(C, 'modrow')])
        hg = sb(C, stack, 'hg_sb', [128, 4, 64], F32)
        S.dma('sp', hg[:, :, :], dr['Hg%d' % l].rearrange('(r p) f -> p r f', p=128), reads=[dbuf(C, 'Hg%d' % l)],
              writes=[hg], sb=hg)
        hgv = hg[:, :, :].rearrange('p r (c t) -> p r c t', c=4)
        halo = sb(C, stack, 'halo', [128, 2, 4, 8], F32)
        for r in range(4):
            for side in range(2):
                src = hgv[:, r, :, 8:16] if side == 0 else hgv[:, r, :, 0:8]
                scol = sel[:, side * 4 + r:side * 4 + r + 1]
                if r == 0:
                    tsc(C, 'dve', halo[:, side, :, :], src, scol, None, ALU.mult, None, [hg, sel], [halo])
                else:
                    stt(C, halo[:, side, :, :], src, scol, halo[:, side, :, :], ALU.mult, ALU.add, [hg, sel, halo], [halo])
        qTt = [sb(C, stack, 'qTB%d' % j, [128, 4, 512], BF16) for j in range(2)]
        NB = 4
        kch = [sb(C, stack, 'kch%d' % j, [128, 512], BF16) for j in range(NB)]
        vch = [sb(C, stack, 'vch%d' % j, [128, 4, VW], BF16) for j in range(NB)]
        Eb = [sb(C, stack, 'Eb%d' % j, [128, 2, 512], BF16) for j in range(2)]
        attn_tm = sb(C, stack, 'attn_tm', [128, 4, 4, 128], BF16)
        catT = sb(C, stack, 'catT', [128, 8, 512], BF16)
        uTe = [sb(C, stack, 'uTe%d' % j, [128, 4, 528], F32) for j in range(2)]
        invc = [sb(C, stack, 'invc%d' % j, [128, 4, 512], F32) for j in range(2)]
        s2 = sb(C, stack, 'ps2', [128, 4, 528], F32)
        s4 = sb(C, stack, 'ps4', [128, 3, 528], F32)
        s8 = sb(C, stack, 'ps8', [128, 2, 528], F32)
        s16 = sb(C, stack, 'ps16', [128, 1, 528], F32)
        ptmp = sb(C, stack, 'ptmp', [128, 512], F32)
        pooledT = sb(C, stack, 'pooledT', [128, 4, 512], BF16)
        xts = [[sb(C, stack, 'xtB%d_%d' % (j, i), [128, 1024], F32) for i in range(4)] for j in range(2)]
        tmpo = [sb(C, stack, 'tmpo%d' % j, [128, 512], F32) for j in range(2)]
        o32 = [sb(C, stack, 'o32_%d' % j, [128, 128], F32) for j in range(2)]
        fsc = [sb(C, stack, 'fsc%d' % j, [128, 8], F32) for j in range(2)]
        fjunk = sb(C, stack, 'fjunk', [128, 128], BF16)
        xsrc = dr[xname]
        xdst = dr['xa%d' % l]
        accb = [C.pb[4], C.pb[5], C.pb[6]]
        misc = C.pb[7]

        def acc_ap(idx):
            return accb[idx // 3][:, (idx % 3) * 132:(idx % 3) * 132 + 129]

        def loads(si):
            tok0, ntok = ST[si]
            j = si % 2
            isctx = si == 8
            S.dma('sp', qTt[j][:, :, 0:ntok], dr['qT%d' % l].rearrange('(c p) t -> p c t', p=128)[:, :, tok0:tok0 + ntok],
                  reads=[dbuf(C, 'qT%d#%d' % (l, si))], writes=[qTt[j]], sb=qTt[j])
            ut = dr['uT%d' % l].rearrange('(c p) t -> p c t', p=128)
            ue = uTe[j]
            lo = 0 if (si == 0 or isctx) else 8
            hi = 0 if (si == 7 or isctx) else 8
            rd = [dbuf(C, 'uT%d#%d' % (l, si))]
            if lo:
                rd.append(dbuf(C, 'uT%d#%d' % (l, si - 1)))
            if hi:
                rd.append(dbuf(C, 'uT%d#%d' % (l, si + 1)))
            S.dma('sp', ue[:, :, 8 - lo:8 + ntok + hi], ut[:, :, tok0 - lo:tok0 + ntok + hi], reads=rd, writes=[ue], sb=ue)
            if isctx:
                S.op('pool', lambda e: e.memset(ue[:, :, 0:8], 0.0), writes=[ue])
                S.op('pool', lambda e: e.memset(ue[:, :, 8 + ntok:16 + ntok], 0.0), writes=[ue])
            else:
                if si == 0:
                    cp(C, 'pool', ue[:, :, 0:8], halo[:, 0, :, :], [halo], [ue])
                if si == 7:
                    cp(C, 'pool', ue[:, :, 8 + ntok:16 + ntok], halo[:, 1, :, :], [halo], [ue])
            S.dma('sp', invc[j][:, :, 0:ntok], dr['invcnt'][:, tok0:tok0 + ntok].partition_broadcast(128),
                  writes=[invc[j]], sb=invc[j])
            for i in range(ntok // 128):
                S.dma('sp', xts[j][i][:, :], xsrc[tok0 + i * 128:tok0 + (i + 1) * 128, :],
                      reads=[dbuf(C, '%s#%d' % (xname, si))], writes=[xts[j][i]], sb=xts[j][i])

        nchunk_issued = [0]

        def chunk_list(si):
            cl = []
            if si != 8:
                for r in range(4):
                    for jj in range(8):
                        cl.append(('lat', r, jj))
            cl.append(('ctx', 0, 0))
            return cl

        def load_chunk(h, ch):
            n = nchunk_issued[0]
            nchunk_issued[0] += 1
            kb_, vb_ = kch[n % NB], vch[n % NB]
            kind, r, jj = ch
            if kind == 'lat':
                kn, vn = 'Kg%d_%d' % (l, jj), 'Vg%d_%d' % (l, jj)
                S.dma('sp', kb_[:, :], dr[kn][r * 512 + h * 128:r * 512 + (h + 1) * 128, :], reads=[dbuf(C, kn)],
                      writes=[kb_], sb=kb_)
                S.dma('sp', vb_[:, :, :],
                      dr[vn][r * 2048 + h * 512:r * 2048 + (h + 1) * 512, :].rearrange('(p kb) e -> p kb e', kb=4),
                      reads=[dbuf(C, vn)], writes=[vb_], sb=vb_)
            else:
                S.dma('sp', kb_[:, 0:256], dr['Kc%d' % l][h * 128:(h + 1) * 128, :], reads=[dbuf(C, 'Kc%d' % l)],
                      writes=[kb_], sb=kb_)
                S.dma('sp', vb_[:, 0:2, :],
                      dr['Vc%d' % l][h * 256:(h + 1) * 256, :].rearrange('(p kb) e -> p kb e', kb=2),
                      reads=[dbuf(C, 'Vc%d' % l)], writes=[vb_], sb=vb_)
            return kb_, vb_

        loads(sts[0])
        ucount = [0]
        for sidx, si in enumerate(sts):
            if sidx + 1 < len(sts):
                loads(sts[sidx + 1])
            tok0, ntok = ST[si]
            j = si % 2
            isctx = si == 8
            ntile = ntok // 128
            qT = qTt[j]
            chunks = chunk_list(si)
            work = [(h, ci) for h in range(4) for ci in range(len(chunks))]
            loaded = {}
            PRE = 3
            for wi in range(min(PRE, len(work))):
                loaded[wi] = load_chunk(work[wi][0], chunks[work[wi][1]])
            units = []
            for wi, (h, ci) in enumerate(work):
                nkb = 4 if chunks[ci][0] == 'lat' else 2
                for kb in range(nkb):
                    units.append((wi, h, ci, kb))

            def emit_S(u):
                wi, h, ci, kb = units[u]
                kb_, vb_ = loaded[wi]
                uu = ucount[0] + u
                b0, b1 = C.pb[(uu % 2) * 2], C.pb[(uu % 2) * 2 + 1]
                o0, o1 = b0[:, 0:ntok], b1[:, 0:ntok]
                l0, l1 = kb_[0:64, kb * 128:(kb + 1) * 128], kb_[64:128, kb * 128:(kb + 1) * 128]
                r0, r1 = qT[0:64, h, 0:ntok], qT[64:128, h, 0:ntok]
                fns = [lambda e, o0=o0, l0=l0, r0=r0: e.matmul(o0, l0, r0, start=True, stop=True),
                       lambda e, o1=o1, l1=l1, r1=r1: e.matmul(o1, l1, r1, start=True, stop=True)]
                S.op('pe', fns, reads=[kb_, qT], writes=[b0, b1])

            started = {}
            emit_S(0)
            for u, (wi, h, ci, kb) in enumerate(units):
                if kb == 0 and wi + PRE < len(work) and (wi + PRE) not in loaded:
                    loaded[wi + PRE] = load_chunk(work[wi + PRE][0], chunks[work[wi + PRE][1]])
                if u + 1 < len(units):
                    emit_S(u + 1)
                uu = ucount[0] + u
                b0, b1 = C.pb[(uu % 2) * 2], C.pb[(uu % 2) * 2 + 1]
                E = Eb[uu % 2]
                pin = C.pb2[uu % 2][:, :].rearrange('p (m q) -> p m q', m=2)[:, :, 0:ntok]
                act(C, E[:, :, 0:ntok], pin, AF.Exp, [b0, b1], [E], scale=0.125)
                kb_, vb_ = loaded[wi]
                fns = []
                for m in range(2):
                    for qb in range(ntile):
                        idx = m * 4 + qb
                        bank = idx // 3
                        st = (h, bank) not in started
                        started[(h, bank)] = True
                        oa, la, ra = acc_ap(idx), E[:, m, qb * 128:(qb + 1) * 128], vb_[:, kb, 0:129]
                        fns.append(lambda e, oa=oa, la=la, ra=ra, st=st: e.matmul(
                            oa, la, ra, start=st, stop=False, skip_group_check=True))
                S.op('pe', fns, reads=[E, vb_], writes=accb)
                last_of_head = (u + 1 == len(units)) or units[u + 1][1] != h
                if last_of_head:
                    for qb in range(ntile):
                        f = fsc[qb % 2]
                        o = o32[qb % 2]
                        a0, a1 = acc_ap(qb), acc_ap(4 + qb)
                        S.op('dve', lambda e, f=f, a0=a0: e.reciprocal(f[:, 0:1], a0[:, 128:129]), reads=accb, writes=[f])
                        S.op('dve', lambda e, f=f, a1=a1: e.reciprocal(f[:, 1:2], a1[:, 128:129]), reads=accb, writes=[f])
                        tt(C, 'dve', f[:, 2:3], f[:, 1:2], nlam[:, 0:1], ALU.mult, [f, nlam], [f])
                        tsc(C, 'dve', o[:, :], a0[:, 0:128], f[:, 0:1], None, ALU.mult, None, accb, [o], strict=[f])
                        stt(C, o[:, :], a1[:, 0:128], f[:, 2:3], o[:, :], ALU.mult, ALU.add, accb + [o], [o], strict=[f])
                        stt(C, fjunk[:, :], o[:, :], 1.0, o[:, :], ALU.mult, ALU.mult, [o], [fjunk, f], accum=f[:, 3:4])
                        act(C, f[:, 4:5], f[:, 3:4], AF.Sqrt, [f], [f], scale=1.0 / 128, bias=C.eps_t[:, 0:1],
                            strict=[C.eps_t])
                        S.op('dve', lambda e, f=f: e.reciprocal(f[:, 5:6], f[:, 4:5]), reads=[f], writes=[f])
                        stt(C, attn_tm[:, qb, h, :], o[:, :], f[:, 5:6], subg[:, :], ALU.mult, ALU.mult,
                            [o, subg], [attn_tm], strict=[f])
            ucount[0] += len(units)
            mv = misc[:, :].bitcast(BF16).rearrange('p (k t) -> p k t', k=8)
            for qb in range(ntile):
                for h in range(4):
                    tr(C, misc, mv[:, h, :], attn_tm, attn_tm[:, qb, h, :], C.ident_b[:])
                cp(C, 'dve' if qb % 2 == 0 else 'act', catT[:, 4:8, qb * 128:(qb + 1) * 128], mv[:, 0:4, :], [misc], [catT])
            ue = uTe[j]
            W = ntok + 16
            tt(C, 'pool', s2[:, :, 0:W - 1], ue[:, :, 0:W - 1], ue[:, :, 1:W], ALU.add, [ue], [s2])
            tt(C, 'pool', s4[:, :, 0:W - 3], s2[:, 1:4, 0:W - 3], s2[:, 1:4, 2:W - 1], ALU.add, [s2], [s4])
            tt(C, 'pool', s8[:, :, 0:W - 7], s4[:, 1:3, 0:W - 7], s4[:, 1:3, 4:W - 3], ALU.add, [s4], [s8])
            tt(C, 'pool', s16[:, :, 0:W - 15], s8[:, 1:2, 0:W - 15], s8[:, 1:2, 8:W - 7], ALU.add, [s8], [s16])
            wsrc = [(s2, 0, 7), (s4, 0, 6), (s8, 0, 4), (s16, 0, 0)]
            for g in range(4):
                buf_, gi, off = wsrc[g]
                tt(C, 'pool', ptmp[:, 0:ntok], buf_[:, gi, off:off + ntok], invc[j][:, g, 0:ntok], ALU.mult,
                   [buf_, invc[j]], [ptmp])
                tt(C, 'pool', pooledT[:, g, 0:ntok], ptmp[:, 0:ntok], ue[:, g, 8:8 + ntok], ALU.subtract,
                   [ptmp, ue], [pooledT])
            for g in range(4):
                pbk = C.pb[g % 4]
                mm(C, pbk, pbk[:, 0:ntok], [(pool_w[:, g, :], pooledT[:, g, 0:ntok])], [pool_w, pooledT])
                tsc(C, 'dve', catT[:, g, 0:ntok], pbk[:, 0:ntok], psT[:, l * 4 + g:l * 4 + g + 1], None, ALU.mult, None,
                    [pbk, psT], [catT])
            gbc = g1c if isctx else g1x
            k = 0
            for i in range(ntile):
                xt = xts[j][i]
                for half in range(2):
                    pbk = C.pb[k % 4]
                    tm = tmpo[k % 2]
                    k += 1
                    mm(C, pbk, pbk[:, :], [(catT[:, kc, i * 128:(i + 1) * 128], w_out[:, kc, half * 512:(half + 1) * 512])
                                           for kc in range(8)], [catT, w_out])
                    tt(C, 'dve', tm[:, :], pbk[:, :], gbc[:, half * 512:(half + 1) * 512], ALU.mult, [pbk, gbc], [tm])
                    tt(C, 'dve', xt[:, half * 512:(half + 1) * 512], xt[:, half * 512:(half + 1) * 512], tm[:, :], ALU.add,
                       [xt, tm], [xt])
                S.dma('pool', xdst[tok0 + i * 128:tok0 + (i + 1) * 128, :], xt[:, :], reads=[xt],
                      writes=[dbuf(C, 'xa%d#%d' % (l, si))], sb=xt)
        S.barrier()
        S.release(C.phase_bufs)
        C.phase_bufs = []


def phase_c(C, lyr, last):
    S = C.S
    l = lyr
    dr = C.dr
    sts = list(range(8)) if last else list(range(9))
    GS = 3
    groups = [sts[i:i + GS] for i in range(0, len(sts), GS)]
    with ExitStack() as stack:
        Mx = mod_vectors(C, lyr, 0, stack, 'cx')
        Mc = mod_vectors(C, lyr, 1, stack, 'cc') if not last else None
        g2x = sb(C, stack, 'g2x', [128, 1024], F32)
        load_bc(C, g2x[:, :], g2x, dr['modrow'][l, 0, 5120:6144], reads=[dbuf(C, 'modrow')])
        g2c = None
        if not last:
            g2c = sb(C, stack, 'g2c', [128, 1024], F32)
            load_bc(C, g2c[:, :], g2c, dr['modrow'][l, 1, 5120:6144], reads=[dbuf(C, 'modrow')])
        fg = None
        if last:
            fg = sb(C, stack, 'fg', [128, 1024], F32)
            load_bc(C, fg[:, :], fg, dr['final_g'][:])
        rstg = sb(C, stack, 'rstg', [128, 8, 36], F32)
        S.dma('sp', rstg[:, :, 0:4], dr['router_coarse_w'][l].rearrange('(kc p) g -> p kc g', p=128), writes=[rstg], sb=rstg)
        S.dma('sp', rstg[:, :, 4:36], dr['router_fine_w'][l].rearrange('(kc p) g -> p kc g', p=128), writes=[rstg], sb=rstg)
        rw = sb(C, stack, 'rw', [128, 8, 36], BF16)
        cp(C, 'dve', rw[:, :, :], rstg[:, :, :], [rstg], [rw])
        rb = sb(C, stack, 'rb', [128, 36], F32)
        load_bc(C, rb[:, 0:4], rb, dr['router_coarse_b'][l, :])
        load_bc(C, rb[:, 4:36], rb, dr['router_fine_b'][l, :])
        NW = 2
        wst = [sb(C, stack, 'wst%d' % t, [128, 2048], F32) for t in range(3)]
        wg = [sb(C, stack, 'wg%d' % j, [128, 8, 256], BF16) for j in range(NW)]
        wu = [sb(C, stack, 'wu%d' % j, [128, 8, 256], BF16) for j in range(NW)]
        wd = [sb(C, stack, 'wd%d' % j, [128, 2, 1024], BF16) for j in range(NW)]
        xts = [sb(C, stack, 'xtC%d' % i, [128, 1024], F32) for i in range(4)]
        hxTs = [sb(C, stack, 'hxTC%d' % j, [128, 8, 512], BF16) for j in range(GS)]
        accs = [sb(C, stack, 'accC%d' % i, [128, 1024], F32) for i in range(4 * GS)]
        gates = [sb(C, stack, 'gates%d' % i, [128, 32], F32) for i in range(4 * GS)]
        hidT = [sb(C, stack, 'hidT%d' % j, [128, 2, 512], BF16) for j in range(2)]
        sa = [sb(C, stack, 'sa%d' % j, [128, 512], F32) for j in range(2)]
        C.ss = [sb(C, stack, 'ssC%d' % j, [128, 2], F32) for j in range(2)]
        C.rstd = [sb(C, stack, 'rstdC%d' % j, [128, 1], F32) for j in range(2)]
        C.xn = [sb(C, stack, 'xnC%d' % j, [128, 1024], BF16) for j in range(2)]
        C.junk_b = sb(C, stack, 'junkC', [128, 1024], BF16)
        lg = sb(C, stack, 'lg', [128, 36], F32)
        rs = sb(C, stack, 'rsc', [128, 16], F32)
        lfs = sb(C, stack, 'lfs', [128, 8], F32)
        top8 = sb(C, stack, 'top8', [128, 8], F32)
        g8 = sb(C, stack, 'g8', [128, 8], F32)
        g8b = sb(C, stack, 'g8b', [128, 8], F32)
        mk = sb(C, stack, 'mk', [128, 4], F32)
        e4 = sb(C, stack, 'e4', [128, 4], F32)
        xsrc = dr['xa%d' % l]
        xdst = dr['out'] if last else dr['xm%d' % l]
        nx = [0]

        def load_x(si, i):
            tok0, ntok = ST[si]
            xt = xts[nx[0] % 4]
            nx[0] += 1
            S.dma('sp', xt[:, :], xsrc[tok0 + i * 128:tok0 + (i + 1) * 128, :],
                  reads=[dbuf(C, 'xa%d#%d' % (l, si))], writes=[xt], sb=xt)
            return xt

        nw = [0]

        def load_expert(e):
            n = nw[0]
            nw[0] += 1
            jn = n % NW
            g_, e_ = e // 8, e % 8
            st0, st1, st2 = wst
            S.dma('sp', st0[:, :].rearrange('p (kc f) -> p kc f', kc=8),
                  dr['w_gate'][l, g_, e_].rearrange('(kc p) f -> p kc f', p=128), writes=[st0], sb=st0)
            S.dma('sp', st1[:, :].rearrange('p (kc f) -> p kc f', kc=8),
                  dr['w_up'][l, g_, e_].rearrange('(kc p) f -> p kc f', p=128), writes=[st1], sb=st1)
            S.dma('sp', st2[:, :].rearrange('p (fc d) -> p fc d', fc=2),
                  dr['w_down'][l, g_, e_].rearrange('(fc p) d -> p fc d', p=128), writes=[st2], sb=st2)
            cp(C, 'act', wg[jn][:, :, :], st0[:, :].rearrange('p (kc f) -> p kc f', kc=8), [st0], [wg[jn]])
            cp(C, 'pool', wu[jn][:, :, :], st1[:, :].rearrange('p (kc f) -> p kc f', kc=8), [st1], [wu[jn]])
            cp(C, 'act', wd[jn][:, :, :], st2[:, :].rearrange('p (fc d) -> p fc d', fc=2), [st2], [wd[jn]])
            return wg[jn], wu[jn], wd[jn]

        hcount = 0
        ocount = 0
        for grp in groups:
            pend = load_expert(0)
            for gi, si in enumerate(grp):
                tok0, ntok = ST[si]
                isctx = si == 8
                ntile = ntok // 128
                M = Mc if isctx else Mx
                hxT = hxTs[gi]
                xl = [load_x(si, i) for i in range(ntile)]
                norm_mod_T(C, xl, ntile, hxT, M.gsc, 8, M.mT, 24, [C.pb[6], C.pb[7]])
                for i in range(ntile):
                    gt = gates[gi * 4 + i]
                    pr = C.pb[6 + (i % 2)]
                    mm(C, pr, pr[:, 0:36], [(hxT[:, kc, i * 128:(i + 1) * 128], rw[:, kc, :]) for kc in range(8)], [hxT, rw])
                    tt(C, 'dve', lg[:, :], pr[:, 0:36], rb[:, :], ALU.add, [pr, rb], [lg])
                    S.op('dve', lambda e: e.tensor_reduce(rs[:, 0:1], lg[:, 0:4], mybir.AxisListType.X, ALU.max),
                         reads=[lg], writes=[rs])
                    tsc(C, 'dve', mk[:, :], lg[:, 0:4], rs[:, 0:1], None, ALU.is_equal, None, [lg], [mk], strict=[rs])
                    tsc(C, 'dve', rs[:, 1:2], rs[:, 0:1], -1.0, None, ALU.mult, None, [rs], [rs])
                    act(C, e4[:, :], lg[:, 0:4], AF.Exp, [lg, rs], [e4, rs], bias=rs[:, 1:2], accum=rs[:, 2:3])
                    S.op('dve', lambda e: e.reciprocal(rs[:, 3:4], rs[:, 2:3]), reads=[rs], writes=[rs])
                    for g in range(4):
                        src = lg[:, 4 + 8 * g:12 + 8 * g]
                        if g == 0:
                            tsc(C, 'dve', lfs[:, :], src, mk[:, 0:1], None, ALU.mult, None, [lg], [lfs], strict=[mk])
                        else:
                            stt(C, lfs[:, :], src, mk[:, g:g + 1], lfs[:, :], ALU.mult, ALU.add, [lg, lfs], [lfs], strict=[mk])
                    S.op('dve', lambda e: e.max(top8[:, :], lfs[:, :]), reads=[lfs], writes=[top8])
                    tt(C, 'dve', rs[:, 4:5], top8[:, 1:2], top8[:, 0:1], ALU.subtract, [top8], [rs])
                    act(C, rs[:, 5:6], rs[:, 4:5], AF.Exp, [rs], [rs])
                    tsc(C, 'dve', rs[:, 6:7], rs[:, 5:6], 1.0, None, ALU.add, None, [rs], [rs])
                    S.op('dve', lambda e: e.reciprocal(rs[:, 7:8], rs[:, 6:7]), reads=[rs], writes=[rs])
                    tt(C, 'dve', rs[:, 8:9], rs[:, 7:8], rs[:, 3:4], ALU.mult, [rs], [rs])
                    tt(C, 'dve', rs[:, 9:10], rs[:, 8:9], rs[:, 5:6], ALU.mult, [rs], [rs])
                    tsc(C, 'dve', g8[:, :], lfs[:, :], top8[:, 0:1], rs[:, 8:9], ALU.is_equal, ALU.mult, [lfs], [g8],
                        strict=[top8, rs])
                    tsc(C, 'dve', g8b[:, :], lfs[:, :], top8[:, 1:2], rs[:, 9:10], ALU.is_equal, ALU.mult, [lfs], [g8b],
                        strict=[top8, rs])
                    tt(C, 'dve', g8[:, :], g8[:, :], g8b[:, :], ALU.add, [g8, g8b], [g8])
                    for g in range(4):
                        tsc(C, 'dve', gt[:, 8 * g:8 * g + 8], g8[:, :], mk[:, g:g + 1], None, ALU.mult, None,
                            [g8], [gt], strict=[mk])
            units = [(e, gi) for e in range(32) for gi in range(len(grp))]
            wts = {0: pend}
            hts = {}

            def emit_gu(n):
                e, gi = units[n]
                wg_, wu_, wd_ = wts[e]
                si = grp[gi]
                ntok = ST[si][1]
                hxT = hxTs[gi]
                hT = hidT[n % 2]
                hts[n] = hT
                for fc in range(2):
                    pa, pbb = C.pb[fc], C.pb[2 + fc]
                    mm(C, pa, pa[:, 0:ntok], [(wg_[:, kc, fc * 128:(fc + 1) * 128], hxT[:, kc, 0:ntok]) for kc in range(8)],
                       [wg_, hxT])
                    mm(C, pbb, pbb[:, 0:ntok], [(wu_[:, kc, fc * 128:(fc + 1) * 128], hxT[:, kc, 0:ntok]) for kc in range(8)],
                       [wu_, hxT])
                    sa_ = sa[fc]
                    act(C, sa_[:, 0:ntok], pa[:, 0:ntok], AF.Silu, [pa], [sa_])
                    tt(C, 'dve', hT[:, fc, 0:ntok], sa_[:, 0:ntok], pbb[:, 0:ntok], ALU.mult, [sa_, pbb], [hT])

            def emit_down(n):
                nonlocal ocount
                e, gi = units[n]
                wg_, wu_, wd_ = wts[e]
                si = grp[gi]
                ntile = ST[si][1] // 128
                hT = hts.pop(n)
                for i in range(ntile):
                    ac = accs[gi * 4 + i]
                    gt = gates[gi * 4 + i]
                    for half in range(2):
                        po = C.pb[4 + (ocount % 2)]
                        ocount += 1
                        mm(C, po, po[:, :], [(hT[:, fc, i * 128:(i + 1) * 128], wd_[:, fc, half * 512:(half + 1) * 512])
                                             for fc in range(2)], [hT, wd_])
                        a_ = ac[:, half * 512:(half + 1) * 512]
                        if e == 0:
                            tsc(C, 'dve', a_, po[:, :], gt[:, 0:1], None, ALU.mult, None, [po], [ac], strict=[gt])
                        else:
                            stt(C, a_, po[:, :], gt[:, e:e + 1], a_, ALU.mult, ALU.add, [po, ac], [ac], strict=[gt])

            wts[1] = load_expert(1)
            emit_gu(0)
            for n in range(len(units)):
                if n + 1 < len(units):
                    emit_gu(n + 1)
                emit_down(n)
                e, gi = units[n]
                if gi == len(grp) - 1 and e + 2 < 32:
                    wts[e + 2] = load_expert(e + 2)
                    wts.pop(e, None)
            for gi, si in enumerate(grp):
                tok0, ntok = ST[si]
                isctx = si == 8
                gbc = g2c if isctx else g2x
                for i in range(ntok // 128):
                    ac = accs[gi * 4 + i]
                    xt = load_x(si, i)
                    tt(C, 'pool', ac[:, :], ac[:, :], gbc[:, :], ALU.mult, [ac, gbc], [ac])
                    tt(C, 'pool', xt[:, :], xt[:, :], ac[:, :], ALU.add, [xt, ac], [xt])
                    if last:
                        ss = C.ss[i % 2]
                        rstd = C.rstd[i % 2]
                        rms_rstd(C, (xt[:, :], xt), rstd, ss, C.junk_b[:, :])
                        stt(C, xt[:, :], xt[:, :], rstd[:, 0:1], fg[:, :], ALU.mult, ALU.mult, [xt, fg], [xt], strict=[rstd])
                        S.dma('pool', xdst[tok0 + i * 128:tok0 + (i + 1) * 128, :], xt[:, :], reads=[xt],
                              writes=[dbuf(C, 'out')], sb=xt)
                    else:
                        S.dma('pool', xdst[tok0 + i * 128:tok0 + (i + 1) * 128, :], xt[:, :], reads=[xt],
                              writes=[dbuf(C, 'xm%d#%d' % (l, si))], sb=xt)
        S.barrier()
        S.release(C.phase_bufs)
        C.phase_bufs = []


TAGGED = {'ada_w', 'w_in', 'w_out', 'w_gate', 'w_up', 'w_down'}
TAGN = 64


def declare(C, name, shape, dt, role):
    kind = {'in': 'ExternalInput', 'out': 'ExternalOutput', 'tmp': 'Internal'}[role]
    if name in TAGGED:
        n = 1
        for d_ in shape:
            n *= d_
        flat = C.nc.dram_tensor(name, [n + TAGN], dt, kind=kind).ap()
        letters = 'abcdefg'[:len(shape)]
        pat = '(%s) -> %s' % (' '.join(letters), ' '.join(letters))
        C.dr[name] = flat[0:n].rearrange(pat, **{letters[i]: shape[i] for i in range(1, len(shape))})
        return
    if role == 'tmp' and name.startswith(('Kg', 'Vg', 'Hg')):
        C.dr[name] = C.nc.dram_tensor(name, list(shape), dt, kind=kind, addr_space='Local').ap()
    else:
        C.dr[name] = C.nc.dram_tensor(name, list(shape), dt, kind=kind).ap()


CONST_IN = [('ident', [128, 128]), ('pmat', [128, 128]), ('cosT', [128, NT]), ('sinT', [128, NT]),
            ('n1gT', [128, 16]), ('n2gT', [128, 16]), ('psT', [128, 8]), ('sel', [128, 8]), ('invcnt', [4, NT])]
PARAM_IN = [('w_in', [2, D, 2048]), ('w_out', [2, D, D]), ('pool_w', [2, 4, 128, 128]), ('subln_g', [2, 128]),
            ('lambda_q1', [2, 64]), ('lambda_k1', [2, 64]), ('lambda_q2', [2, 64]), ('lambda_k2', [2, 64]),
            ('router_coarse_w', [2, D, 4]), ('router_coarse_b', [2, 4]), ('router_fine_w', [2, D, 32]),
            ('router_fine_b', [2, 32]), ('w_gate', [2, 4, 8, D, 256]), ('w_up', [2, 4, 8, D, 256]),
            ('w_down', [2, 4, 8, 256, D]), ('final_g', [D])]


_ALLP = [nm for nm, _ in PARAM_IN]
MODE_PARAMS = {0: set(_ALLP), 1: {'w_in'}, 2: set(_ALLP) - {'final_g'}, 3: set(_ALLP) - {'w_in'}}


def layer_tensors(l):
    t = [('uT%d' % l, [512, NT], F32), ('qT%d' % l, [512, NT], BF16), ('Hx%d' % l, [128, 64], F32),
         ('Kc%d' % l, [512, NCTX], BF16), ('Vc%d' % l, [4 * NCTX, VW], BF16)]
    for s in range(8):
        t.append(('Kx%d_%d' % (l, s), [512, 512], BF16))
        t.append(('Vx%d_%d' % (l, s), [2048, VW], BF16))
    return t


def gathered_tensors(l):
    t = [('Hg%d' % l, [4 * 128, 64], F32)]
    for s in range(8):
        t.append(('Kg%d_%d' % (l, s), [4 * 512, 512], BF16))
        t.append(('Vg%d_%d' % (l, s), [4 * 2048, VW], BF16))
    return t


def build(mode):
    nc = bass.Bass("TRN2", target_bir_lowering=False)
    C = Ctx()
    C.nc = nc
    C.dr = {}
    C.db = {}
    C.fused = mode == 0
    with ExitStack() as stack:
        C.S = Sched(nc, stack)
        for nm, shp in CONST_IN:
            declare(C, nm, shp, F32, 'in')
        for nm, shp in PARAM_IN:
            if nm in MODE_PARAMS[mode]:
                declare(C, nm, shp, F32, 'in')
        if mode in (0, 1):
            declare(C, 'x_in', [NT, D], F32, 'in')
            declare(C, 'sT_in', [128, 16], F32, 'in')
            declare(C, 'ada_w', [2, D, 6 * D], F32, 'in')
            declare(C, 'ada_b', [2, 6 * D], F32, 'in')
        declare(C, 'modrow', [2, 2, 6 * D], F32, {0: 'tmp', 1: 'out', 2: 'in', 3: 'in'}[mode])
        for l in range(2):
            prod = 1 if l == 0 else 2
            cons = prod + 1
            for nm, shp, dt in layer_tensors(l):
                if mode == 0:
                    declare(C, nm, shp, dt, 'tmp')
                elif mode == prod:
                    declare(C, nm, shp, dt, 'out')
                elif mode == cons and not nm.startswith(('Kx', 'Vx', 'Hx')):
                    declare(C, nm, shp, dt, 'in')
            for nm, shp, dt in gathered_tensors(l):
                if mode == 0:
                    declare(C, nm, shp, dt, 'tmp')
                elif mode == cons:
                    declare(C, nm, shp, dt, 'in')
        if mode == 0:
            for nm in ['xa0', 'xm0', 'xa1']:
                declare(C, nm, [NT, D], F32, 'tmp')
            declare(C, 'out', [NLAT, D], F32, 'out')
        elif mode == 2:
            declare(C, 'x_in', [NT, D], F32, 'in')
            declare(C, 'xa0', [NT, D], F32, 'out')
            declare(C, 'xm0', [NT, D], F32, 'out')
        elif mode == 3:
            declare(C, 'xm0', [NT, D], F32, 'in')
            declare(C, 'xa1', [NT, D], F32, 'tmp')
            declare(C, 'out', [NLAT, D], F32, 'out')
        setup_common(C, stack)
        C.n1gT = sb(C, stack, 'n1gT_sb', [128, 16], F32)
        C.n2gT = sb(C, stack, 'n2gT_sb', [128, 16], F32)
        C.S.dma('sp', C.n1gT[:, :], C.dr['n1gT'][:, :], writes=[C.n1gT], sb=C.n1gT)
        C.S.dma('sp', C.n2gT[:, :], C.dr['n2gT'][:, :], writes=[C.n2gT], sb=C.n2gT)
        C.phase_bufs = []
        if mode in (0, 1):
            prologue(C)
            phase_a(C, 0, 'x_in', last=False)
        if mode in (0, 2):
            phase_b(C, 0, 'x_in', last=False)
            phase_c(C, 0, last=False)
            phase_a(C, 1, 'xm0', last=True)
        if mode in (0, 3):
            phase_b(C, 1, 'xm0', last=True)
            phase_c(C, 1, last=True)
        C.S.barrier(['sp'])
        C.S.replay()
    return nc


def rope_tables_T(core):
    qq = core % 4
    tok = np.arange(NLAT) + qq * NLAT
    row = (tok // 64).astype(np.float32)
    col = (tok % 64).astype(np.float32)
    inv = (10000.0 ** (-np.arange(16, dtype=np.float32) / 16)).astype(np.float32)
    cosT = np.ones((64, NT), np.float32)
    sinT = np.zeros((64, NT), np.float32)
    for d in range(64):
        f = d % 16
        pos = row if d < 32 else col
        ang = (pos * inv[f]).astype(np.float32)
        sgn = -1.0 if (d % 32) < 16 else 1.0
        cosT[d, :NLAT] = np.cos(ang)
        sinT[d, :NLAT] = sgn * np.sin(ang)
    return np.tile(cosT, (2, 1)), np.tile(sinT, (2, 1))


def pmat_const():
    p = np.zeros((128, 128), np.float32)
    for i in range(128):
        partner = i + 16 if (i % 32) < 16 else i - 16
        p[partner, i] = 1.0
    return p


def invcnt_const(core):
    qq = core % 4
    out = np.zeros((4, NT), np.float32)
    for g, w in enumerate((2, 4, 8, 16)):
        left = w // 2
        right = w - 1 - left
        t = np.arange(NLAT) + qq * NLAT
        lo = np.clip(t - left, 0, L)
        hi = np.clip(t + right + 1, 0, L)
        out[g, :NLAT] = 1.0 / (hi - lo)
        t = np.arange(NCTX)
        lo = np.clip(t - left, 0, NCTX)
        hi = np.clip(t + right + 1, 0, NCTX)
        out[g, NLAT:] = 1.0 / (hi - lo)
    return out


def const_inputs(core, inp):
    cosT, sinT = rope_tables_T(core)
    qq = core % 4
    sel = np.zeros((128, 8), np.float32)
    if qq > 0:
        sel[:, qq - 1] = 1.0
    if qq < 3:
        sel[:, 4 + qq + 1] = 1.0
    m = {
        'ident': np.eye(128, dtype=np.float32),
        'pmat': pmat_const(),
        'cosT': cosT, 'sinT': sinT,
        'n1gT': np.ascontiguousarray(inp['norm1_g'].reshape(2, 8, 128).transpose(2, 0, 1).reshape(128, 16)),
        'n2gT': np.ascontiguousarray(inp['norm2_g'].reshape(2, 8, 128).transpose(2, 0, 1).reshape(128, 16)),
        'psT': np.ascontiguousarray(inp['pool_scale'].reshape(2, 4, 128).transpose(2, 0, 1).reshape(128, 8)),
        'sel': sel,
        'invcnt': invcnt_const(core),
    }
    return m


def first_inputs(core, inp):
    b, qq = core // 4, core % 4
    m = {}
    m['x_in'] = np.ascontiguousarray(np.concatenate([inp['x'][b, qq * NLAT:(qq + 1) * NLAT], inp['ctx'][b]], 0))
    s = np.stack([inp['c'][b], inp['c_ctx']], -1)
    m['sT_in'] = np.ascontiguousarray(s.reshape(8, 128, 2).transpose(1, 0, 2).reshape(128, 16))
    m['ada_w'] = tagged(inp['ada_w'], core)
    m['ada_b'] = inp['ada_b']
    return m


def gather_host(results, l):
    outs = []
    for c in range(NCORES):
        grp = [4 * (c // 4) + r for r in range(4)]
        m = {'Hg%d' % l: np.concatenate([results[r]['Hx%d' % l] for r in grp], 0)}
        for s in range(8):
            m['Kg%d_%d' % (l, s)] = np.concatenate([results[r]['Kx%d_%d' % (l, s)] for r in grp], 0)
            m['Vg%d_%d' % (l, s)] = np.concatenate([results[r]['Vx%d_%d' % (l, s)] for r in grp], 0)
        outs.append(m)
    return outs


_NC_CACHE = {}


def get_nc(mode):
    if mode not in _NC_CACHE:
        _NC_CACHE[mode] = build(mode)
    return _NC_CACHE[mode]


def tagged(a, core):
    return np.concatenate([np.asarray(a, np.float32).ravel(), np.full(TAGN, float(core), np.float32)])


def params_for(mode, inp, core=0):
    return {nm: (tagged(inp[nm], core) if nm in TAGGED else inp[nm]) for nm in _ALLP if nm in MODE_PARAMS[mode]}


def run_multi(inp, cores=None):
    consts = [const_inputs(c, inp) for c in range(NCORES)]
    ids = list(range(NCORES)) if cores is None else cores
    in1 = [dict(consts[c], **first_inputs(c, inp), **params_for(1, inp, c)) for c in ids]
    r1 = run_bass_kernel_spmd(get_nc(1), in1, core_ids=ids).results
    g0 = gather_host(r1, 0)
    in2 = []
    for c in ids:
        m = dict(consts[c], **params_for(2, inp, c))
        m['x_in'] = in1[c]['x_in']
        m['modrow'] = r1[c]['modrow']
        for nm in ['uT0', 'qT0', 'Kc0', 'Vc0']:
            m[nm] = r1[c][nm]
        m.update(g0[c])
        in2.append(m)
    r2 = run_bass_kernel_spmd(get_nc(2), in2, core_ids=ids).results
    g1 = gather_host(r2, 1)
    in3 = []
    for c in ids:
        m = dict(consts[c], **params_for(3, inp, c))
        m['modrow'] = r1[c]['modrow']
        m['xm0'] = r2[c]['xm0']
        for nm in ['uT1', 'qT1', 'Kc1', 'Vc1']:
            m[nm] = r2[c][nm]
        m.update(g1[c])
        in3.append(m)
    r3 = run_bass_kernel_spmd(get_nc(3), in3, core_ids=ids).results
    return r3


def run_fused(inp):
    ids = list(range(NCORES))
    ins = [dict(const_inputs(c, inp), **first_inputs(c, inp), **params_for(0, inp, c)) for c in ids]
    return run_bass_kernel_spmd(get_nc(0), ins, core_ids=ids).results


FUSED = True


def kernel(**inputs):
    inp = {k: np.ascontiguousarray(np.asarray(v, dtype=np.float32)) for k, v in inputs.items()}
    res = run_fused(inp) if FUSED else run_multi(inp)
    out = np.zeros((2, L, D), np.float32)
    for c in range(NCORES):
        b, qq = c // 4, c % 4
        out[b, qq * NLAT:(qq + 1) * NLAT] = np.asarray(res[c]['out'], dtype=np.float32)
    return out
```

```python
import math
from contextlib import ExitStack
import numpy as np
import ml_dtypes
import concourse.bass as bass
import concourse.mybir as mybir
from concourse.bass_utils import run_bass_kernel_spmd

F32 = mybir.dt.float32
BF16 = mybir.dt.bfloat16
AF = mybir.ActivationFunctionType
ALU = mybir.AluOpType

NCORES = 8
D = 1024
L = 16384
NLAT = 4096
NCTX = 256
NT = NLAT + NCTX
VW = 132
EPS = 1e-6
ST = [(s * 512, 512) for s in range(8)] + [(NLAT, NCTX)]
ENGS = ['sp', 'pe', 'act', 'dve', 'pool']


class Buf:
    def __init__(self, name, t=None):
        self.name = name
        self.t = t
        self.w = None
        self.r = {}
        self.dkey = None
        self.small = False

    def __getitem__(self, idx):
        return self.t[idx]


class Sched:
    def __init__(self, nc, stack):
        self.nc = nc
        self.stack = stack
        self.ops = {e: [] for e in ENGS}
        self.sem = {}
        self.cnt = {}
        self.isdma = {}
        self.waited = {e: {} for e in ENGS}
        self.nsem = 0
        self.free_d = []
        for e in ['pe', 'act', 'dve', 'pool']:
            self.newsem(e, False)

    def newsem(self, key, isdma):
        self.nsem += 1
        self.sem[key] = self.stack.enter_context(self.nc.semaphore('s%d' % self.nsem))
        self.cnt[key] = 0
        self.isdma[key] = isdma

    def _deps(self, reads, writes):
        deps = {}

        def add(k, v):
            if deps.get(k, 0) < v:
                deps[k] = v
        for b in reads:
            if b.w is not None:
                add(*b.w)
        for b in writes:
            if b.w is not None:
                add(*b.w)
            for k, v in b.r.items():
                add(k, v)
        return deps

    def _wait(self, eng, deps, strict=()):
        own = 0
        for b in strict:
            if b.w is not None and b.w[0] == eng:
                own = max(own, b.w[1])
        for k, v in deps.items():
            if k == eng:
                if own == 0:
                    continue
                v = own
            if self.isdma[k]:
                v = self.cnt[k]
            if self.waited[eng].get(k, 0) >= v:
                continue
            self.waited[eng][k] = v
            sem = self.sem[k]
            self.ops[eng].append(lambda e, sem=sem, v=v: e.wait_ge(sem, v))

    def _commit(self, key, v, reads, writes):
        for b in writes:
            b.w = (key, v)
            b.r = {}
        for b in reads:
            if b.r.get(key, 0) < v:
                b.r[key] = v

    def op(self, eng, fns, reads=(), writes=(), strict=()):
        if not isinstance(fns, (list, tuple)):
            fns = [fns]
        strict = list(strict) + [b for b in reads if b.small]
        self._wait(eng, self._deps(list(reads) + list(strict), writes), strict)
        self.cnt[eng] += 1
        v = self.cnt[eng]
        sem = self.sem[eng]
        for f in fns[:-1]:
            self.ops[eng].append(f)
        last = fns[-1]
        self.ops[eng].append(lambda e, last=last, sem=sem: last(e).then_inc(sem, 1))
        self._commit(eng, v, reads, writes)

    def dma(self, q, out, in_, reads=(), writes=(), sb=None, **kw):
        if sb.dkey is None:
            if self.free_d:
                sb.dkey = self.free_d.pop()
            else:
                sb.dkey = ('d', len(self.sem))
                self.newsem(sb.dkey, True)
        key = sb.dkey
        self._wait(q, self._deps(reads, writes))
        self.cnt[key] += 16
        v = self.cnt[key]
        sem = self.sem[key]
        self.ops[q].append(lambda e: e.dma_start(out=out, in_=in_, **kw).then_inc(sem, 16))
        self._commit(key, v, reads, writes)

    def cc(self, ins_ap, outs_ap, reads=(), writes=()):
        key = 'cc'
        if key not in self.sem:
            self.newsem(key, True)
        self._wait('pool', self._deps(reads, writes))
        self.cnt[key] += 1
        v = self.cnt[key]
        sem = self.sem[key]
        self.ops['pool'].append(lambda e: e.collective_compute(
            "AllGather", ALU.bypass, replica_groups=[[0, 1, 2, 3], [4, 5, 6, 7]],
            ins=[ins_ap], outs=[outs_ap]).then_inc(sem, 1))
        self._commit(key, v, reads, writes)

    def release(self, bufs):
        for b in bufs:
            if b.dkey is not None:
                self.free_d.append(b.dkey)
                b.dkey = None

    def barrier(self, engines=ENGS):
        for e in engines:
            for k in self.sem:
                if k == e or self.cnt[k] == 0:
                    continue
                v = self.cnt[k]
                if self.waited[e].get(k, 0) >= v:
                    continue
                self.waited[e][k] = v
                sem = self.sem[k]
                self.ops[e].append(lambda en, sem=sem, v=v: en.wait_ge(sem, v))

    def replay(self):
        nc = self.nc
        with nc.Block() as block:
            @block.sync
            def _(e):
                for f in self.ops['sp']:
                    f(e)

            @block.tensor
            def _(e):
                for f in self.ops['pe']:
                    f(e)

            @block.scalar
            def _(e):
                for f in self.ops['act']:
                    f(e)

            @block.vector
            def _(e):
                for f in self.ops['dve']:
                    f(e)

            @block.gpsimd
            def _(e):
                for f in self.ops['pool']:
                    f(e)


class Ctx:
    pass


def dbuf(C, name):
    if name not in C.db:
        C.db[name] = Buf(name)
    return C.db[name]


def sb(C, stack, name, shape, dt):
    if not hasattr(C, 'names'):
        C.names = {}
    k = C.names.get(name, 0)
    C.names[name] = k + 1
    if k:
        name = '%s_r%d' % (name, k)
    t = stack.enter_context(C.nc.sbuf_tensor(name, shape, dt))
    b = Buf(name, t)
    if getattr(C, 'phase_bufs', None) is not None:
        C.phase_bufs.append(b)
    fs = 1
    for d_ in shape[1:]:
        fs *= d_
    b.small = fs < 256
    return b


def ps(C, stack, name, shape, dt):
    t = stack.enter_context(C.nc.psum_tensor(name, shape, dt))
    return Buf(name, t)


def mm(C, out_buf, out_ap, pairs, reads, start=True, stop=True, skip=False):
    fns = []
    n = len(pairs)
    for i, (l_ap, r_ap) in enumerate(pairs):
        st = start and i == 0
        sp_ = stop and i == n - 1
        fns.append(lambda e, l_ap=l_ap, r_ap=r_ap, st=st, sp_=sp_: e.matmul(
            out_ap, l_ap, r_ap, start=st, stop=sp_, skip_group_check=skip))
    C.S.op('pe', fns, reads=reads, writes=[out_buf])


def tr(C, out_buf, out_ap, in_buf, in_ap, ident_ap, extra_reads=()):
    C.S.op('pe', lambda e: e.transpose(out_ap, in_ap, ident_ap),
           reads=[in_buf, C.ident_b] + list(extra_reads), writes=[out_buf])


def act(C, out_ap, in_ap, func, reads, writes, scale=None, bias=None, accum=None, strict=()):
    kw = {}
    if scale is not None:
        kw['scale'] = scale
    if bias is not None:
        kw['bias'] = bias
    if accum is not None:
        kw['accum_out'] = accum
    C.S.op('act', lambda e: e.activation(out_ap, in_ap, func, **kw), reads=reads, writes=writes, strict=strict)


def tsc(C, eng, out_ap, in_ap, s1, s2, op0, op1, reads, writes, accum=None, strict=()):
    if op1 is None:
        C.S.op(eng, lambda e: e.tensor_scalar(out_ap, in_ap, s1, None, op0), reads=reads, writes=writes, strict=strict)
    else:
        C.S.op(eng, lambda e: e.tensor_scalar(out_ap, in_ap, s1, s2, op0, op1), reads=reads, writes=writes, strict=strict)


def stt(C, out_ap, in0, scalar, in1, op0, op1, reads, writes, accum=None, strict=()):
    if accum is None:
        C.S.op('dve', lambda e: e.scalar_tensor_tensor(out_ap, in0, scalar, in1, op0, op1),
               reads=reads, writes=writes, strict=strict)
    else:
        C.S.op('dve', lambda e: e.scalar_tensor_tensor(out_ap, in0, scalar, in1, op0, op1, accum_out=accum),
               reads=reads, writes=writes, strict=strict)


def tt(C, eng, out_ap, in0, in1, op, reads, writes):
    C.S.op(eng, lambda e: e.tensor_tensor(out_ap, in0, in1, op), reads=reads, writes=writes)


def cp(C, eng, out_ap, in_ap, reads, writes):
    if eng == 'act':
        C.S.op('act', lambda e: e.activation(out_ap, in_ap, AF.Copy), reads=reads, writes=writes)
    else:
        C.S.op(eng, lambda e: e.tensor_copy(out_ap, in_ap), reads=reads, writes=writes)


def setup_common(C, stack):
    nc, S = C.nc, C.S
    C.ident_f = sb(C, stack, 'ident_f', [128, 128], F32)
    C.ident_b = sb(C, stack, 'ident_b', [128, 128], BF16)
    C.eps_t = sb(C, stack, 'eps_t', [128, 1], F32)
    S.dma('sp', C.ident_f[:], C.dr['ident'][:, :], reads=[], writes=[C.ident_f], sb=C.ident_f)
    cp(C, 'dve', C.ident_b[:], C.ident_f[:], [C.ident_f], [C.ident_b])
    S.op('dve', lambda e: e.memset(C.eps_t[:], EPS), writes=[C.eps_t])
    C.pb2 = [stack.enter_context(nc.psum_tensor('pb%d' % i, [128, 1024], F32)) for i in range(4)]
    C.pb = []
    for i in range(4):
        for hh in range(2):
            C.pb.append(Buf('pbank%d' % (2 * i + hh), C.pb2[i][:, hh * 512:(hh + 1) * 512]))


def load_modT(C, dst, col, lyr, which, vec, q='sp'):
    src = C.dr['modrow'][lyr, which, vec * 1024:(vec + 1) * 1024].rearrange('(kc p) -> p kc', p=128)
    C.S.dma(q, dst[:, col:col + 8], src, reads=[dbuf(C, 'modrow')], writes=[dst], sb=dst,
            allow_slow_non_contiguous=True)


def load_bc(C, dst_ap, dst_buf, src_row_ap, reads=(), q='sp'):
    C.S.dma(q, dst_ap, src_row_ap.partition_broadcast(128), reads=list(reads), writes=[dst_buf], sb=dst_buf)


def rms_rstd(C, xt, rstd, ss, junk, n=1024, dim=1024):
    stt(C, junk, xt[0], 1.0, xt[0], ALU.mult, ALU.mult, reads=[xt[1]], writes=[ss, C.junk_b], accum=ss[:, 0:1])
    act(C, ss[:, 1:2], ss[:, 0:1], AF.Sqrt, [ss], [ss], scale=1.0 / dim, bias=C.eps_t[:, 0:1], strict=[C.eps_t])
    C.S.op('dve', lambda e: e.reciprocal(rstd[:, 0:1], ss[:, 1:2]), reads=[ss], writes=[rstd])


def norm_mod_T(C, xts, ntile, hxT, gsc, gcol, sh, scol, tpb):
    for i in range(ntile):
        xt = xts[i]
        ss = C.ss[i % 2]
        rstd = C.rstd[i % 2]
        xn = C.xn[i % 2]
        rms_rstd(C, (xt[:, :], xt), rstd, ss, C.junk_b[:, :])
        tsc(C, 'dve', xn[:, :], xt[:, :], rstd[:, 0:1], None, ALU.mult, None, [xt], [xn], strict=[rstd])
        tp = tpb[i % len(tpb)]
        tpv = tp[:, :].bitcast(BF16).rearrange('p (k t) -> p k t', k=8)
        for kc in range(8):
            tr(C, tp, tpv[:, kc, :], xn, xn[:, kc * 128:(kc + 1) * 128], C.ident_b[:])
        for kc in range(8):
            o = hxT[:, kc, i * 128:(i + 1) * 128]
            if kc % 2 == 0:
                act(C, o, tpv[:, kc, :], AF.Identity, [tp], [hxT], strict=[gsc, sh],
                    scale=gsc[:, gcol + kc:gcol + kc + 1], bias=sh[:, scol + kc:scol + kc + 1])
            else:
                tsc(C, 'dve', o, tpv[:, kc, :], gsc[:, gcol + kc:gcol + kc + 1], sh[:, scol + kc:scol + kc + 1],
                    ALU.mult, ALU.add, [tp], [hxT], strict=[gsc, sh])


def load_weight_bf16(C, dst, dst_ap_fn, src_ap_fn, nchunk, stage_bufs, q='sp', cast_eng='pool'):
    for i in range(nchunk):
        stg = stage_bufs[i % len(stage_bufs)]
        src = src_ap_fn(i)
        C.S.dma(q, stg[0](i), src, reads=[], writes=[stg[1]], sb=stg[1])
        cp(C, cast_eng, dst_ap_fn(i), stg[0](i), [stg[1]], [dst])


def mod_vectors(C, lyr, which, stack, pre):
    M = Ctx()
    M.mT = sb(C, stack, pre + 'mT', [128, 48], F32)
    for v in (0, 1, 3, 4):
        load_modT(C, M.mT, v * 8, lyr, which, v)
    M.gsc = sb(C, stack, pre + 'gsc', [128, 16], F32)
    stt(C, M.gsc[:, 0:8], M.mT[:, 8:16], 1.0, C.n1gT[:, lyr * 8:(lyr + 1) * 8], ALU.add, ALU.mult,
        [M.mT, C.n1gT], [M.gsc])
    stt(C, M.gsc[:, 8:16], M.mT[:, 32:40], 1.0, C.n2gT[:, lyr * 8:(lyr + 1) * 8], ALU.add, ALU.mult,
        [M.mT, C.n2gT], [M.gsc])
    return M


def prologue(C):
    S = C.S
    with ExitStack() as stack:
        sT = sb(C, stack, 'sT', [128, 16], F32)
        S.dma('sp', sT[:, :], C.dr['sT_in'][:, :], writes=[sT], sb=sT)
        act(C, sT[:, :], sT[:, :], AF.Silu, [sT], [sT])
        sTv = sT[:, :].rearrange('p (k w) -> p k w', w=2)
        wblk = [sb(C, stack, 'adaw%d' % i, [128, 8, 512], F32) for i in range(2)]
        brow = sb(C, stack, 'brow', [2, 6144], F32)
        mrow = sb(C, stack, 'mrow', [2, 6144], F32)
        n = 0
        for lyr in range(2):
            for w in range(2):
                S.dma('sp', brow[w:w + 1, :], C.dr['ada_b'][lyr:lyr + 1, :], writes=[brow], sb=brow)
            for cb in range(12):
                wb = wblk[n % 2]
                src = C.dr['ada_w'][lyr, :, cb * 512:(cb + 1) * 512].rearrange('(kc p) c -> p kc c', p=128)
                S.dma('sp' if n % 2 == 0 else 'pool', wb[:, :, :], src, writes=[wb], sb=wb)
                pb = C.pb[n % 2]
                mm(C, pb, pb[0:2, :], [(sTv[:, kc, :], wb[:, kc, :]) for kc in range(8)], [sT, wb])
                tt(C, 'dve', mrow[:, cb * 512:(cb + 1) * 512], pb[0:2, :], brow[:, cb * 512:(cb + 1) * 512],
                   ALU.add, [pb, brow], [mrow])
                n += 1
            S.dma('pool', C.dr['modrow'][lyr, :, :], mrow[:, :], reads=[mrow], writes=[dbuf(C, 'modrow')], sb=mrow)
        S.barrier()
        S.release(C.phase_bufs)
        C.phase_bufs = []


def phase_a(C, lyr, xname, last):
    xsrc = C.dr[xname]
    xsrc_bufs = [dbuf(C, '%s#%d' % (xname, i)) for i in range(9)]
    S = C.S
    with ExitStack() as stack:
        w_in = sb(C, stack, 'w_in_sb', [128, 8, 2048], BF16)
        stg = [sb(C, stack, 'stgA%d' % i, [128, 2048], F32) for i in range(2)]
        for kc in range(8):
            st_ = stg[kc % 2]
            S.dma('sp', st_[:, :], C.dr['w_in'][lyr, kc * 128:(kc + 1) * 128, :], writes=[st_], sb=st_)
            cp(C, 'pool', w_in[:, kc, :], st_[:, :], [st_], [w_in])
        pmat = sb(C, stack, 'pmat_sb', [128, 128], BF16)
        S.dma('sp', stg[0][:, 0:128], C.dr['pmat'][:, :], writes=[stg[0]], sb=stg[0])
        cp(C, 'dve', pmat[:, :], stg[0][:, 0:128], [stg[0]], [pmat])
        Mx = mod_vectors(C, lyr, 0, stack, 'ax')
        Mc = mod_vectors(C, lyr, 1, stack, 'ac')
        xts = [[sb(C, stack, 'xtA%d_%d' % (j, i), [128, 1024], F32) for i in range(4)] for j in range(2)]
        hxTs = [sb(C, stack, 'hxTA%d' % j, [128, 8, 512], BF16) for j in range(2)]
        cosb = [sb(C, stack, 'cosA%d' % j, [128, 512], F32) for j in range(2)]
        sinb = [sb(C, stack, 'sinA%d' % j, [128, 512], F32) for j in range(2)]
        uTs = [sb(C, stack, 'uTA%d' % j, [128, 4, 512], F32) for j in range(2)]
        qTs = [sb(C, stack, 'qTA%d' % j, [128, 4, 512], BF16) for j in range(2)]
        kTs = [sb(C, stack, 'kTA%d' % j, [128, 4, 512], BF16) for j in range(2)]
        Vts = [sb(C, stack, 'VtA%d' % j, [128, 4, 4, VW], BF16) for j in range(2)]
        raw = [sb(C, stack, 'rawA%d' % j, [128, 512], BF16) for j in range(2)]
        t1 = [sb(C, stack, 't1A%d' % j, [128, 512], F32) for j in range(2)]
        t2 = [sb(C, stack, 't2A%d' % j, [128, 512], F32) for j in range(2)]
        C.ss = [sb(C, stack, 'ssA%d' % j, [128, 2], F32) for j in range(2)]
        C.rstd = [sb(C, stack, 'rstdA%d' % j, [128, 1], F32) for j in range(2)]
        C.xn = [sb(C, stack, 'xnA%d' % j, [128, 1024], BF16) for j in range(2)]
        C.junk_b = sb(C, stack, 'junkA', [128, 1024], BF16)
        for j in range(2):
            S.op('pool', lambda e, j=j: e.memset(Vts[j][:, :, :, :], 1.0), writes=[Vts[j]])

        def loads(si):
            tok0, ntok = ST[si]
            j = si % 2
            for i in range(ntok // 128):
                S.dma('sp', xts[j][i][:, :], xsrc[tok0 + i * 128: tok0 + (i + 1) * 128, :],
                      reads=[xsrc_bufs[si]], writes=[xts[j][i]], sb=xts[j][i])
            S.dma('sp', cosb[j][:, 0:ntok], C.dr['cosT'][:, tok0:tok0 + ntok], writes=[cosb[j]], sb=cosb[j])
            S.dma('sp', sinb[j][:, 0:ntok], C.dr['sinT'][:, tok0:tok0 + ntok], writes=[sinb[j]], sb=sinb[j])

        loads(0)
        pbi = 0
        for si, (tok0, ntok) in enumerate(ST):
            if si + 1 < len(ST):
                loads(si + 1)
            j = si % 2
            ntile = ntok // 128
            isctx = si == 8
            M = Mc if isctx else Mx
            hxT = hxTs[j]
            norm_mod_T(C, xts[j], ntile, hxT, M.gsc, 0, M.mT, 0, [C.pb[6], C.pb[7]])
            uT, qT, kT, Vt = uTs[j], qTs[j], kTs[j], Vts[j]
            nkb = ntile
            for cc in range(12):
                if last and isctx and cc < 8:
                    continue
                pb = C.pb[pbi % 4]
                pbi += 1
                mm(C, pb, pb[:, 0:ntok],
                   [(w_in[:, kc, cc * 128:(cc + 1) * 128], hxT[:, kc, 0:ntok]) for kc in range(8)], [w_in, hxT])
                if cc < 4:
                    cp(C, 'act', uT[:, cc, 0:ntok], pb[:, 0:ntok], [pb], [uT])
                    continue
                h = cc % 4
                rw = raw[cc % 2]
                cp(C, 'act', rw[:, 0:ntok], pb[:, 0:ntok], [pb], [rw])
                pw = C.pb[4 + (cc % 2)]
                mm(C, pw, pw[:, 0:ntok], [(pmat[:, :], rw[:, 0:ntok])], [pmat, rw])
                a1, a2 = t1[cc % 2], t2[cc % 2]
                tt(C, 'pool', a1[:, 0:ntok], rw[:, 0:ntok], cosb[j][:, 0:ntok], ALU.mult, [rw, cosb[j]], [a1])
                tt(C, 'dve', a2[:, 0:ntok], pw[:, 0:ntok], sinb[j][:, 0:ntok], ALU.mult, [pw, sinb[j]], [a2])
                if cc < 8:
                    tt(C, 'dve', qT[:, h, 0:ntok], a1[:, 0:ntok], a2[:, 0:ntok], ALU.add, [a1, a2], [qT])
                else:
                    o = kT[:, h, 0:ntok].rearrange('d (kb p) -> d p kb', kb=nkb)
                    i1 = a1[:, 0:ntok].rearrange('d (p kb) -> d p kb', kb=nkb)
                    i2 = a2[:, 0:ntok].rearrange('d (p kb) -> d p kb', kb=nkb)
                    tt(C, 'dve', o, i1, i2, ALU.add, [a1, a2], [kT])
            for i in range(ntile):
                pb = C.pb[pbi % 4]
                pbi += 1
                mm(C, pb, pb[:, :],
                   [(hxT[:, kc, i * 128:(i + 1) * 128], w_in[:, kc, 1536:2048]) for kc in range(8)], [w_in, hxT])
                cp(C, 'act' if i % 2 == 0 else 'dve', Vt[:, i, :, 0:128],
                   pb[:, :].rearrange('p (h e) -> p h e', h=4), [pb], [Vt])
            dr = C.dr
            l = lyr
            if not (last and isctx):
                S.dma('pool', dr['uT%d' % l].rearrange('(c p) t -> p c t', p=128)[:, :, tok0:tok0 + ntok],
                      uT[:, :, 0:ntok], reads=[uT], writes=[dbuf(C, 'uT%d#%d' % (l, si))], sb=uT)
                S.dma('pool', dr['qT%d' % l].rearrange('(c p) t -> p c t', p=128)[:, :, tok0:tok0 + ntok],
                      qT[:, :, 0:ntok], reads=[qT], writes=[dbuf(C, 'qT%d#%d' % (l, si))], sb=qT)
                hx = dr['Hx%d' % l].rearrange('p (c t) -> p c t', c=4)
                if si == 0:
                    S.dma('pool', hx[:, :, 0:8], uT[:, :, 0:8], reads=[uT], writes=[dbuf(C, 'Hx%d' % l)], sb=uT)
                if si == 7:
                    S.dma('pool', hx[:, :, 8:16], uT[:, :, 504:512], reads=[uT], writes=[dbuf(C, 'Hx%d' % l)], sb=uT)
                    if C.fused:
                        S.cc(dr['Hx%d' % l][:, :], dr['Hg%d' % l][:, :], reads=[dbuf(C, 'Hx%d' % l)],
                             writes=[dbuf(C, 'Hg%d' % l)])
            if not isctx:
                kn, vn = 'Kx%d_%d' % (l, si), 'Vx%d_%d' % (l, si)
                S.dma('pool', dr[kn].rearrange('(c p) t -> p c t', p=128), kT[:, :, 0:ntok],
                      reads=[kT], writes=[dbuf(C, kn)], sb=kT)
                for i in range(ntile):
                    S.dma('pool', dr[vn].rearrange('(h t) e -> t h e', h=4)[i * 128:(i + 1) * 128, :, :],
                          Vt[:, i, :, :], reads=[Vt], writes=[dbuf(C, vn)], sb=Vt)
                if C.fused:
                    S.cc(dr[kn][:, :], dr['Kg%d_%d' % (l, si)][:, :], reads=[dbuf(C, kn)],
                         writes=[dbuf(C, 'Kg%d_%d' % (l, si))])
                    S.cc(dr[vn][:, :], dr['Vg%d_%d' % (l, si)][:, :], reads=[dbuf(C, vn)],
                         writes=[dbuf(C, 'Vg%d_%d' % (l, si))])
            else:
                S.dma('pool', dr['Kc%d' % l].rearrange('(c p) t -> p c t', p=128), kT[:, :, 0:ntok],
                      reads=[kT], writes=[dbuf(C, 'Kc%d' % l)], sb=kT)
                for i in range(ntile):
                    S.dma('pool', dr['Vc%d' % l].rearrange('(h t) e -> t h e', h=4)[i * 128:(i + 1) * 128, :, :],
                          Vt[:, i, :, :], reads=[Vt], writes=[dbuf(C, 'Vc%d' % l)], sb=Vt)
        S.barrier()
        S.release(C.phase_bufs)
        C.phase_bufs = []


def load_w_bf16_rows(C, dst, src2d, nk, width, stg, q='sp', cast='pool'):
    for kc in range(nk):
        st_ = stg[kc % len(stg)]
        C.S.dma(q, st_[:, 0:width], src2d[kc * 128:(kc + 1) * 128, :], writes=[st_], sb=st_)
        cp(C, cast, dst[:, kc, :], st_[:, 0:width], [st_], [dst])


def phase_b(C, lyr, xname, last):
    S = C.S
    l = lyr
    dr = C.dr
    lam_init = 0.8 - 0.6 * math.exp(-0.3 * lyr)
    sts = list(range(8)) if last else list(range(9))
    with ExitStack() as stack:
        stg = [sb(C, stack, 'stgB%d' % i, [128, 1024], F32) for i in range(2)]
        w_out = sb(C, stack, 'w_out_sb', [128, 8, 1024], BF16)
        load_w_bf16_rows(C, w_out, dr['w_out'][l], 8, 1024, stg)
        pool_w = sb(C, stack, 'pool_w_sb', [128, 4, 128], BF16)
        S.dma('sp', stg[0][:, 0:512].rearrange('p (g e) -> p g e', g=4), dr['pool_w'][l].rearrange('g c e -> c g e'),
              writes=[stg[0]], sb=stg[0])
        cp(C, 'dve', pool_w[:, :, :], stg[0][:, 0:512].rearrange('p (g e) -> p g e', g=4), [stg[0]], [pool_w])
        psT = sb(C, stack, 'psT_sb', [128, 8], F32)
        S.dma('sp', psT[:, :], dr['psT'][:, :], writes=[psT], sb=psT)
        sel = sb(C, stack, 'sel_sb', [128, 8], F32)
        S.dma('sp', sel[:, :], dr['sel'][:, :], writes=[sel], sb=sel)
        subg = sb(C, stack, 'subg', [128, 128], F32)
        load_bc(C, subg[:, :], subg, dr['subln_g'][l, :])
        tsc(C, 'dve', subg[:, :], subg[:, :], 1.0 - lam_init, None, ALU.mult, None, [subg], [subg])
        lamb = sb(C, stack, 'lamb', [128, 4, 64], F32)
        for i, nm in enumerate(['lambda_q1', 'lambda_k1', 'lambda_q2', 'lambda_k2']):
            load_bc(C, lamb[:, i, :], lamb, dr[nm][l, :])
        lsc = sb(C, stack, 'lsc', [128, 8], F32)
        ljunk = sb(C, stack, 'ljunk', [128, 64], F32)
        stt(C, ljunk[:, :], lamb[:, 0, :], 1.0, lamb[:, 1, :], ALU.mult, ALU.mult, [lamb], [ljunk, lsc], accum=lsc[:, 0:1])
        stt(C, ljunk[:, :], lamb[:, 2, :], 1.0, lamb[:, 3, :], ALU.mult, ALU.mult, [lamb], [ljunk, lsc], accum=lsc[:, 1:2])
        act(C, lsc[:, 2:4], lsc[:, 0:2], AF.Exp, [lsc], [lsc])
        tt(C, 'dve', lsc[:, 4:5], lsc[:, 2:3], lsc[:, 3:4], ALU.subtract, [lsc], [lsc])
        nlam = sb(C, stack, 'nlam', [128, 1], F32)
        tsc(C, 'dve', nlam[:, :], lsc[:, 4:5], lam_init, -1.0, ALU.add, ALU.mult, [lsc], [nlam])
        g1x = sb(C, stack, 'g1x', [128, 1024], F32)
        load_bc(C, g1x[:, :], g1x, dr['modrow'][l, 0, 2048:3072], reads=[dbuf(C, 'modrow')])
        g1c = None
        if not last:
            g1c = sb(C, stack, 'g1c', [128, 1024], F32)
            load_bc(C, g1c[:, :], g1c, dr['modrow'][l, 1, 2048:3072], reads=[dbuf(C, 'modrow')])
        hg = sb(C, stack, 'hg_sb', [128, 4, 64], F32)
        S.dma('sp', hg[:, :, :], dr['Hg%d' % l].rearrange('(r p) f -> p r f', p=128), reads=[dbuf(C, 'Hg%d' % l)],
              writes=[hg], sb=hg)
        hgv = hg[:, :, :].rearrange('p r (c t) -> p r c t', c=4)
        halo = sb(C, stack, 'halo', [128, 2, 4, 8], F32)
        for r in range(4):
            for side in range(2):
                src = hgv[:, r, :, 8:16] if side == 0 else hgv[:, r, :, 0:8]
                scol = sel[:, side * 4 + r:side * 4 + r + 1]
                if r == 0:
                    tsc(C, 'dve', halo[:, side, :, :], src, scol, None, ALU.mult, None, [hg, sel], [halo])
                else:
                    stt(C, halo[:, side, :, :], src, scol, halo[:, side, :, :], ALU.mult, ALU.add, [hg, sel, halo], [halo])
        qTt = [sb(C, stack, 'qTB%d' % j, [128, 4, 512], BF16) for j in range(2)]
        NB = 4
        kch = [sb(C, stack, 'kch%d' % j, [128, 512], BF16) for j in range(NB)]
        vch = [sb(C, stack, 'vch%d' % j, [128, 4, VW], BF16) for j in range(NB)]
        Eb = [sb(C, stack, 'Eb%d' % j, [128, 2, 512], BF16) for j in range(3)]
        attn_tm = sb(C, stack, 'attn_tm', [128, 4, 4, 128], BF16)
        catT = sb(C, stack, 'catT', [128, 8, 512], BF16)
        uTe = [sb(C, stack, 'uTe%d' % j, [128, 4, 528], F32) for j in range(2)]
        invc = [sb(C, stack, 'invc%d' % j, [128, 4, 512], F32) for j in range(2)]
        s2 = sb(C, stack, 'ps2', [128, 4, 528], F32)
        s4 = sb(C, stack, 'ps4', [128, 3, 528], F32)
        s8 = sb(C, stack, 'ps8', [128, 2, 528], F32)
        s16 = sb(C, stack, 'ps16', [128, 1, 528], F32)
        ptmp = sb(C, stack, 'ptmp', [128, 512], F32)
        pooledT = sb(C, stack, 'pooledT', [128, 4, 512], BF16)
        xts = [[sb(C, stack, 'xtB%d_%d' % (j, i), [128, 1024], F32) for i in range(4)] for j in range(2)]
        tmpo = [sb(C, stack, 'tmpo%d' % j, [128, 512], F32) for j in range(2)]
        o32 = [sb(C, stack, 'o32_%d' % j, [128, 128], F32) for j in range(2)]
        fsc = [sb(C, stack, 'fsc%d' % j, [128, 8], F32) for j in range(2)]
        fjunk = sb(C, stack, 'fjunk', [128, 128], BF16)
        xsrc = dr[xname]
        xdst = dr['xa%d' % l]
        accb = [C.pb[4], C.pb[5], C.pb[6]]
        misc = C.pb[7]

        def acc_ap(idx):
            return accb[idx // 3][:, (idx % 3) * 132:(idx % 3) * 132 + 129]

        def loads(si):
            tok0, ntok = ST[si]
            j = si % 2
            isctx = si == 8
            S.dma('sp', qTt[j][:, :, 0:ntok], dr['qT%d' % l].rearrange('(c p) t -> p c t', p=128)[:, :, tok0:tok0 + ntok],
                  reads=[dbuf(C, 'qT%d#%d' % (l, si))], writes=[qTt[j]], sb=qTt[j])
            ut = dr['uT%d' % l].rearrange('(c p) t -> p c t', p=128)
            ue = uTe[j]
            lo = 0 if (si == 0 or isctx) else 8
            hi = 0 if (si == 7 or isctx) else 8
            rd = [dbuf(C, 'uT%d#%d' % (l, si))]
            if lo:
                rd.append(dbuf(C, 'uT%d#%d' % (l, si - 1)))
            if hi:
                rd.append(dbuf(C, 'uT%d#%d' % (l, si + 1)))
            S.dma('sp', ue[:, :, 8 - lo:8 + ntok + hi], ut[:, :, tok0 - lo:tok0 + ntok + hi], reads=rd, writes=[ue], sb=ue)
            if isctx:
                S.op('pool', lambda e: e.memset(ue[:, :, 0:8], 0.0), writes=[ue])
                S.op('pool', lambda e: e.memset(ue[:, :, 8 + ntok:16 + ntok], 0.0), writes=[ue])
            else:
                if si == 0:
                    cp(C, 'pool', ue[:, :, 0:8], halo[:, 0, :, :], [halo], [ue])
                if si == 7:
                    cp(C, 'pool', ue[:, :, 8 + ntok:16 + ntok], halo[:, 1, :, :], [halo], [ue])
            S.dma('sp', invc[j][:, :, 0:ntok], dr['invcnt'][:, tok0:tok0 + ntok].partition_broadcast(128),
                  writes=[invc[j]], sb=invc[j])
            for i in range(ntok // 128):
                S.dma('sp', xts[j][i][:, :], xsrc[tok0 + i * 128:tok0 + (i + 1) * 128, :],
                      reads=[dbuf(C, '%s#%d' % (xname, si))], writes=[xts[j][i]], sb=xts[j][i])

        nchunk_issued = [0]

        def chunk_list(si):
            cl = []
            if si != 8:
                for r in range(4):
                    for jj in range(8):
                        cl.append(('lat', r, jj))
            cl.append(('ctx', 0, 0))
            return cl

        def load_chunk(h, ch):
            n = nchunk_issued[0]
            nchunk_issued[0] += 1
            kb_, vb_ = kch[n % NB], vch[n % NB]
            kind, r, jj = ch
            if kind == 'lat':
                kn, vn = 'Kg%d_%d' % (l, jj), 'Vg%d_%d' % (l, jj)
                S.dma('sp', kb_[:, :], dr[kn][r * 512 + h * 128:r * 512 + (h + 1) * 128, :], reads=[dbuf(C, kn)],
                      writes=[kb_], sb=kb_)
                S.dma('sp', vb_[:, :, :],
                      dr[vn][r * 2048 + h * 512:r * 2048 + (h + 1) * 512, :].rearrange('(p kb) e -> p kb e', kb=4),
                      reads=[dbuf(C, vn)], writes=[vb_], sb=vb_)
            else:
                S.dma('sp', kb_[:, 0:256], dr['Kc%d' % l][h * 128:(h + 1) * 128, :], reads=[dbuf(C, 'Kc%d' % l)],
                      writes=[kb_], sb=kb_)
                S.dma('sp', vb_[:, 0:2, :],
                      dr['Vc%d' % l][h * 256:(h + 1) * 256, :].rearrange('(p kb) e -> p kb e', kb=2),
                      reads=[dbuf(C, 'Vc%d' % l)], writes=[vb_], sb=vb_)
            return kb_, vb_

        loads(sts[0])
        ucount = [0]
        for sidx, si in enumerate(sts):
            if sidx + 1 < len(sts):
                loads(sts[sidx + 1])
            tok0, ntok = ST[si]
            j = si % 2
            isctx = si == 8
            ntile = ntok // 128
            qT = qTt[j]
            chunks = chunk_list(si)
            work = [(h, ci) for h in range(4) for ci in range(len(chunks))]
            loaded = {}
            PRE = 3
            for wi in range(min(PRE, len(work))):
                loaded[wi] = load_chunk(work[wi][0], chunks[work[wi][1]])
            units = []
            for wi, (h, ci) in enumerate(work):
                nkb = 4 if chunks[ci][0] == 'lat' else 2
                for kb in range(nkb):
                    units.append((wi, h, ci, kb))

            def emit_S(u):
                wi, h, ci, kb = units[u]
                kb_, vb_ = loaded[wi]
                uu = ucount[0] + u
                b0, b1 = C.pb[(uu % 2) * 2], C.pb[(uu % 2) * 2 + 1]
                o0, o1 = b0[:, 0:ntok], b1[:, 0:ntok]
                l0, l1 = kb_[0:64, kb * 128:(kb + 1) * 128], kb_[64:128, kb * 128:(kb + 1) * 128]
                r0, r1 = qT[0:64, h, 0:ntok], qT[64:128, h, 0:ntok]
                fns = [lambda e, o0=o0, l0=l0, r0=r0: e.matmul(o0, l0, r0, start=True, stop=True),
                       lambda e, o1=o1, l1=l1, r1=r1: e.matmul(o1, l1, r1, start=True, stop=True)]
                S.op('pe', fns, reads=[kb_, qT], writes=[b0, b1])

            started = {}
            emit_S(0)
            if len(units) > 1:
                emit_S(1)
            for u, (wi, h, ci, kb) in enumerate(units):
                if kb == 0 and wi + PRE < len(work) and (wi + PRE) not in loaded:
                    loaded[wi + PRE] = load_chunk(work[wi + PRE][0], chunks[work[wi + PRE][1]])
                uu = ucount[0] + u
                b0, b1 = C.pb[(uu % 2) * 2], C.pb[(uu % 2) * 2 + 1]
                E = Eb[uu % 3]
                pin = C.pb2[uu % 2][:, :].rearrange('p (m q) -> p m q', m=2)[:, :, 0:ntok]
                act(C, E[:, :, 0:ntok], pin, AF.Exp, [b0, b1], [E], scale=0.125)
                if u + 2 < len(units):
                    emit_S(u + 2)
                kb_, vb_ = loaded[wi]
                fns = []
                for m in range(2):
                    for qb in range(ntile):
                        idx = m * 4 + qb
                        bank = idx // 3
                        st = (h, bank) not in started
                        started[(h, bank)] = True
                        oa, la, ra = acc_ap(idx), E[:, m, qb * 128:(qb + 1) * 128], vb_[:, kb, 0:129]
                        fns.append(lambda e, oa=oa, la=la, ra=ra, st=st: e.matmul(
                            oa, la, ra, start=st, stop=False, skip_group_check=True))
                S.op('pe', fns, reads=[E, vb_], writes=accb)
                last_of_head = (u + 1 == len(units)) or units[u + 1][1] != h
                if last_of_head:
                    for qb in range(ntile):
                        f = fsc[qb % 2]
                        o = o32[qb % 2]
                        a0, a1 = acc_ap(qb), acc_ap(4 + qb)
                        S.op('dve', lambda e, f=f, a0=a0: e.reciprocal(f[:, 0:1], a0[:, 128:129]), reads=accb, writes=[f])
                        S.op('dve', lambda e, f=f, a1=a1: e.reciprocal(f[:, 1:2], a1[:, 128:129]), reads=accb, writes=[f])
                        tt(C, 'dve', f[:, 2:3], f[:, 1:2], nlam[:, 0:1], ALU.mult, [f, nlam], [f])
                        tsc(C, 'dve', o[:, :], a0[:, 0:128], f[:, 0:1], None, ALU.mult, None, accb, [o], strict=[f])
                        stt(C, o[:, :], a1[:, 0:128], f[:, 2:3], o[:, :], ALU.mult, ALU.add, accb + [o], [o], strict=[f])
                        stt(C, fjunk[:, :], o[:, :], 1.0, o[:, :], ALU.mult, ALU.mult, [o], [fjunk, f], accum=f[:, 3:4])
                        act(C, f[:, 4:5], f[:, 3:4], AF.Sqrt, [f], [f], scale=1.0 / 128, bias=C.eps_t[:, 0:1],
                            strict=[C.eps_t])
                        S.op('dve', lambda e, f=f: e.reciprocal(f[:, 5:6], f[:, 4:5]), reads=[f], writes=[f])
                        stt(C, attn_tm[:, qb, h, :], o[:, :], f[:, 5:6], subg[:, :], ALU.mult, ALU.mult,
                            [o, subg], [attn_tm], strict=[f])
            ucount[0] += len(units)
            mv = misc[:, :].bitcast(BF16).rearrange('p (k t) -> p k t', k=8)
            for qb in range(ntile):
                for h in range(4):
                    tr(C, misc, mv[:, h, :], attn_tm, attn_tm[:, qb, h, :], C.ident_b[:])
                cp(C, 'dve' if qb % 2 == 0 else 'act', catT[:, 4:8, qb * 128:(qb + 1) * 128], mv[:, 0:4, :], [misc], [catT])
            ue = uTe[j]
            W = ntok + 16
            tt(C, 'pool', s2[:, :, 0:W - 1], ue[:, :, 0:W - 1], ue[:, :, 1:W], ALU.add, [ue], [s2])
            tt(C, 'pool', s4[:, :, 0:W - 3], s2[:, 1:4, 0:W - 3], s2[:, 1:4, 2:W - 1], ALU.add, [s2], [s4])
            tt(C, 'pool', s8[:, :, 0:W - 7], s4[:, 1:3, 0:W - 7], s4[:, 1:3, 4:W - 3], ALU.add, [s4], [s8])
            tt(C, 'pool', s16[:, :, 0:W - 15], s8[:, 1:2, 0:W - 15], s8[:, 1:2, 8:W - 7], ALU.add, [s8], [s16])
            wsrc = [(s2, 0, 7), (s4, 0, 6), (s8, 0, 4), (s16, 0, 0)]
            for g in range(4):
                buf_, gi, off = wsrc[g]
                tt(C, 'pool', ptmp[:, 0:ntok], buf_[:, gi, off:off + ntok], invc[j][:, g, 0:ntok], ALU.mult,
                   [buf_, invc[j]], [ptmp])
                tt(C, 'pool', pooledT[:, g, 0:ntok], ptmp[:, 0:ntok], ue[:, g, 8:8 + ntok], ALU.subtract,
                   [ptmp, ue], [pooledT])
            for g in range(4):
                pbk = C.pb[g % 4]
                mm(C, pbk, pbk[:, 0:ntok], [(pool_w[:, g, :], pooledT[:, g, 0:ntok])], [pool_w, pooledT])
                tsc(C, 'dve', catT[:, g, 0:ntok], pbk[:, 0:ntok], psT[:, l * 4 + g:l * 4 + g + 1], None, ALU.mult, None,
                    [pbk, psT], [catT])
            gbc = g1c if isctx else g1x
            k = 0
            for i in range(ntile):
                xt = xts[j][i]
                for half in range(2):
                    pbk = C.pb[k % 4]
                    tm = tmpo[k % 2]
                    k += 1
                    mm(C, pbk, pbk[:, :], [(catT[:, kc, i * 128:(i + 1) * 128], w_out[:, kc, half * 512:(half + 1) * 512])
                                           for kc in range(8)], [catT, w_out])
                    tt(C, 'dve', tm[:, :], pbk[:, :], gbc[:, half * 512:(half + 1) * 512], ALU.mult, [pbk, gbc], [tm])
                    tt(C, 'dve', xt[:, half * 512:(half + 1) * 512], xt[:, half * 512:(half + 1) * 512], tm[:, :], ALU.add,
                       [xt, tm], [xt])
                S.dma('pool', xdst[tok0 + i * 128:tok0 + (i + 1) * 128, :], xt[:, :], reads=[xt],
                      writes=[dbuf(C, 'xa%d#%d' % (l, si))], sb=xt)
        S.barrier()
        S.release(C.phase_bufs)
        C.phase_bufs = []


def phase_c(C, lyr, last):
    S = C.S
    l = lyr
    dr = C.dr
    sts = list(range(8)) if last else list(range(9))
    GS = 3
    groups = [sts[i:i + GS] for i in range(0, len(sts), GS)]
    with ExitStack() as stack:
        Mx = mod_vectors(C, lyr, 0, stack, 'cx')
        Mc = mod_vectors(C, lyr, 1, stack, 'cc') if not last else None
        g2x = sb(C, stack, 'g2x', [128, 1024], F32)
        load_bc(C, g2x[:, :], g2x, dr['modrow'][l, 0, 5120:6144], reads=[dbuf(C, 'modrow')])
        g2c = None
        if not last:
            g2c = sb(C, stack, 'g2c', [128, 1024], F32)
            load_bc(C, g2c[:, :], g2c, dr['modrow'][l, 1, 5120:6144], reads=[dbuf(C, 'modrow')])
        fg = None
        if last:
            fg = sb(C, stack, 'fg', [128, 1024], F32)
            load_bc(C, fg[:, :], fg, dr['final_g'][:])
        rstg = sb(C, stack, 'rstg', [128, 8, 36], F32)
        S.dma('sp', rstg[:, :, 0:4], dr['router_coarse_w'][l].rearrange('(kc p) g -> p kc g', p=128), writes=[rstg], sb=rstg)
        S.dma('sp', rstg[:, :, 4:36], dr['router_fine_w'][l].rearrange('(kc p) g -> p kc g', p=128), writes=[rstg], sb=rstg)
        rw = sb(C, stack, 'rw', [128, 8, 36], BF16)
        cp(C, 'dve', rw[:, :, :], rstg[:, :, :], [rstg], [rw])
        rb = sb(C, stack, 'rb', [128, 36], F32)
        load_bc(C, rb[:, 0:4], rb, dr['router_coarse_b'][l, :])
        load_bc(C, rb[:, 4:36], rb, dr['router_fine_b'][l, :])
        NW = 2
        wst = [sb(C, stack, 'wst%d' % t, [128, 2048], F32) for t in range(3)]
        wg = [sb(C, stack, 'wg%d' % j, [128, 8, 256], BF16) for j in range(NW)]
        wu = [sb(C, stack, 'wu%d' % j, [128, 8, 256], BF16) for j in range(NW)]
        wd = [sb(C, stack, 'wd%d' % j, [128, 2, 1024], BF16) for j in range(NW)]
        xts = [sb(C, stack, 'xtC%d' % i, [128, 1024], F32) for i in range(4)]
        hxTs = [sb(C, stack, 'hxTC%d' % j, [128, 8, 512], BF16) for j in range(GS)]
        accs = [sb(C, stack, 'accC%d' % i, [128, 1024], F32) for i in range(4 * GS)]
        gates = [sb(C, stack, 'gates%d' % i, [128, 32], F32) for i in range(4 * GS)]
        hidT = [sb(C, stack, 'hidT%d' % j, [128, 2, 512], BF16) for j in range(2)]
        sa = [sb(C, stack, 'sa%d' % j, [128, 512], F32) for j in range(2)]
        C.ss = [sb(C, stack, 'ssC%d' % j, [128, 2], F32) for j in range(2)]
        C.rstd = [sb(C, stack, 'rstdC%d' % j, [128, 1], F32) for j in range(2)]
        C.xn = [sb(C, stack, 'xnC%d' % j, [128, 1024], BF16) for j in range(2)]
        C.junk_b = sb(C, stack, 'junkC', [128, 1024], BF16)
        lg = sb(C, stack, 'lg', [128, 36], F32)
        rs = sb(C, stack, 'rsc', [128, 16], F32)
        lfs = sb(C, stack, 'lfs', [128, 8], F32)
        top8 = sb(C, stack, 'top8', [128, 8], F32)
        g8 = sb(C, stack, 'g8', [128, 8], F32)
        g8b = sb(C, stack, 'g8b', [128, 8], F32)
        mk = sb(C, stack, 'mk', [128, 4], F32)
        e4 = sb(C, stack, 'e4', [128, 4], F32)
        xsrc = dr['xa%d' % l]
        xdst = dr['out'] if last else dr['xm%d' % l]
        nx = [0]

        def load_x(si, i):
            tok0, ntok = ST[si]
            xt = xts[nx[0] % 4]
            nx[0] += 1
            S.dma('sp', xt[:, :], xsrc[tok0 + i * 128:tok0 + (i + 1) * 128, :],
                  reads=[dbuf(C, 'xa%d#%d' % (l, si))], writes=[xt], sb=xt)
            return xt

        nw = [0]

        def load_expert(e):
            n = nw[0]
            nw[0] += 1
            jn = n % NW
            g_, e_ = e // 8, e % 8
            st0, st1, st2 = wst
            S.dma('sp', st0[:, :].rearrange('p (kc f) -> p kc f', kc=8),
                  dr['w_gate'][l, g_, e_].rearrange('(kc p) f -> p kc f', p=128), writes=[st0], sb=st0)
            S.dma('sp', st1[:, :].rearrange('p (kc f) -> p kc f', kc=8),
                  dr['w_up'][l, g_, e_].rearrange('(kc p) f -> p kc f', p=128), writes=[st1], sb=st1)
            S.dma('sp', st2[:, :].rearrange('p (fc d) -> p fc d', fc=2),
                  dr['w_down'][l, g_, e_].rearrange('(fc p) d -> p fc d', p=128), writes=[st2], sb=st2)
            cp(C, 'act', wg[jn][:, :, :], st0[:, :].rearrange('p (kc f) -> p kc f', kc=8), [st0], [wg[jn]])
            cp(C, 'pool', wu[jn][:, :, :], st1[:, :].rearrange('p (kc f) -> p kc f', kc=8), [st1], [wu[jn]])
            cp(C, 'act', wd[jn][:, :, :], st2[:, :].rearrange('p (fc d) -> p fc d', fc=2), [st2], [wd[jn]])
            return wg[jn], wu[jn], wd[jn]

        hcount = 0
        ocount = 0
        for grp in groups:
            pend = load_expert(0)
            for gi, si in enumerate(grp):
                tok0, ntok = ST[si]
                isctx = si == 8
                ntile = ntok // 128
                M = Mc if isctx else Mx
                hxT = hxTs[gi]
                xl = [load_x(si, i) for i in range(ntile)]
                norm_mod_T(C, xl, ntile, hxT, M.gsc, 8, M.mT, 24, [C.pb[6], C.pb[7]])
                for i in range(ntile):
                    gt = gates[gi * 4 + i]
                    pr = C.pb[6 + (i % 2)]
                    mm(C, pr, pr[:, 0:36], [(hxT[:, kc, i * 128:(i + 1) * 128], rw[:, kc, :]) for kc in range(8)], [hxT, rw])
                    tt(C, 'dve', lg[:, :], pr[:, 0:36], rb[:, :], ALU.add, [pr, rb], [lg])
                    S.op('dve', lambda e: e.tensor_reduce(rs[:, 0:1], lg[:, 0:4], mybir.AxisListType.X, ALU.max),
                         reads=[lg], writes=[rs])
                    tsc(C, 'dve', mk[:, :], lg[:, 0:4], rs[:, 0:1], None, ALU.is_equal, None, [lg], [mk], strict=[rs])
                    tsc(C, 'dve', rs[:, 1:2], rs[:, 0:1], -1.0, None, ALU.mult, None, [rs], [rs])
                    act(C, e4[:, :], lg[:, 0:4], AF.Exp, [lg, rs], [e4, rs], bias=rs[:, 1:2], accum=rs[:, 2:3])
                    S.op('dve', lambda e: e.reciprocal(rs[:, 3:4], rs[:, 2:3]), reads=[rs], writes=[rs])
                    for g in range(4):
                        src = lg[:, 4 + 8 * g:12 + 8 * g]
                        if g == 0:
                            tsc(C, 'dve', lfs[:, :], src, mk[:, 0:1], None, ALU.mult, None, [lg], [lfs], strict=[mk])
                        else:
                            stt(C, lfs[:, :], src, mk[:, g:g + 1], lfs[:, :], ALU.mult, ALU.add, [lg, lfs], [lfs], strict=[mk])
                    S.op('dve', lambda e: e.max(top8[:, :], lfs[:, :]), reads=[lfs], writes=[top8])
                    tt(C, 'dve', rs[:, 4:5], top8[:, 1:2], top8[:, 0:1], ALU.subtract, [top8], [rs])
                    act(C, rs[:, 5:6], rs[:, 4:5], AF.Exp, [rs], [rs])
                    tsc(C, 'dve', rs[:, 6:7], rs[:, 5:6], 1.0, None, ALU.add, None, [rs], [rs])
                    S.op('dve', lambda e: e.reciprocal(rs[:, 7:8], rs[:, 6:7]), reads=[rs], writes=[rs])
                    tt(C, 'dve', rs[:, 8:9], rs[:, 7:8], rs[:, 3:4], ALU.mult, [rs], [rs])
                    tt(C, 'dve', rs[:, 9:10], rs[:, 8:9], rs[:, 5:6], ALU.mult, [rs], [rs])
                    tsc(C, 'dve', g8[:, :], lfs[:, :], top8[:, 0:1], rs[:, 8:9], ALU.is_equal, ALU.mult, [lfs], [g8],
                        strict=[top8, rs])
                    tsc(C, 'dve', g8b[:, :], lfs[:, :], top8[:, 1:2], rs[:, 9:10], ALU.is_equal, ALU.mult, [lfs], [g8b],
                        strict=[top8, rs])
                    tt(C, 'dve', g8[:, :], g8[:, :], g8b[:, :], ALU.add, [g8, g8b], [g8])
                    for g in range(4):
                        tsc(C, 'dve', gt[:, 8 * g:8 * g + 8], g8[:, :], mk[:, g:g + 1], None, ALU.mult, None,
                            [g8], [gt], strict=[mk])
            units = [(e, gi) for e in range(32) for gi in range(len(grp))]
            wts = {0: pend}
            hts = {}

            def emit_gu(n):
                e, gi = units[n]
                wg_, wu_, wd_ = wts[e]
                si = grp[gi]
                ntok = ST[si][1]
                hxT = hxTs[gi]
                hT = hidT[n % 2]
                hts[n] = hT
                for fc in range(2):
                    pa, pbb = C.pb[fc], C.pb[2 + fc]
                    mm(C, pa, pa[:, 0:ntok], [(wg_[:, kc, fc * 128:(fc + 1) * 128], hxT[:, kc, 0:ntok]) for kc in range(8)],
                       [wg_, hxT])
                    mm(C, pbb, pbb[:, 0:ntok], [(wu_[:, kc, fc * 128:(fc + 1) * 128], hxT[:, kc, 0:ntok]) for kc in range(8)],
                       [wu_, hxT])
                    sa_ = sa[fc]
                    act(C, sa_[:, 0:ntok], pa[:, 0:ntok], AF.Silu, [pa], [sa_])
                    tt(C, 'dve', hT[:, fc, 0:ntok], sa_[:, 0:ntok], pbb[:, 0:ntok], ALU.mult, [sa_, pbb], [hT])

            def emit_down(n):
                nonlocal ocount
                e, gi = units[n]
                wg_, wu_, wd_ = wts[e]
                si = grp[gi]
                ntile = ST[si][1] // 128
                hT = hts.pop(n)
                for i in range(ntile):
                    ac = accs[gi * 4 + i]
                    gt = gates[gi * 4 + i]
                    for half in range(2):
                        po = C.pb[4 + (ocount % 2)]
                        ocount += 1
                        mm(C, po, po[:, :], [(hT[:, fc, i * 128:(i + 1) * 128], wd_[:, fc, half * 512:(half + 1) * 512])
                                             for fc in range(2)], [hT, wd_])
                        a_ = ac[:, half * 512:(half + 1) * 512]
                        if e == 0:
                            tsc(C, 'dve', a_, po[:, :], gt[:, 0:1], None, ALU.mult, None, [po], [ac], strict=[gt])
                        else:
                            stt(C, a_, po[:, :], gt[:, e:e + 1], a_, ALU.mult, ALU.add, [po, ac], [ac], strict=[gt])

            wts[1] = load_expert(1)
            emit_gu(0)
            for n in range(len(units)):
                if n + 1 < len(units):
                    emit_gu(n + 1)
                emit_down(n)
                e, gi = units[n]
                if gi == len(grp) - 1 and e + 2 < 32:
                    wts[e + 2] = load_expert(e + 2)
                    wts.pop(e, None)
            for gi, si in enumerate(grp):
                tok0, ntok = ST[si]
                isctx = si == 8
                gbc = g2c if isctx else g2x
                for i in range(ntok // 128):
                    ac = accs[gi * 4 + i]
                    xt = load_x(si, i)
                    tt(C, 'pool', ac[:, :], ac[:, :], gbc[:, :], ALU.mult, [ac, gbc], [ac])
                    tt(C, 'pool', xt[:, :], xt[:, :], ac[:, :], ALU.add, [xt, ac], [xt])
                    if last:
                        ss = C.ss[i % 2]
                        rstd = C.rstd[i % 2]
                        rms_rstd(C, (xt[:, :], xt), rstd, ss, C.junk_b[:, :])
                        stt(C, xt[:, :], xt[:, :], rstd[:, 0:1], fg[:, :], ALU.mult, ALU.mult, [xt, fg], [xt], strict=[rstd])
                        S.dma('pool', xdst[tok0 + i * 128:tok0 + (i + 1) * 128, :], xt[:, :], reads=[xt],
                              writes=[dbuf(C, 'out')], sb=xt)
                    else:
                        S.dma('pool', xdst[tok0 + i * 128:tok0 + (i + 1) * 128, :], xt[:, :], reads=[xt],
                              writes=[dbuf(C, 'xm%d#%d' % (l, si))], sb=xt)
        S.barrier()
        S.release(C.phase_bufs)
        C.phase_bufs = []


TAGGED = {'ada_w', 'w_in', 'w_out', 'w_gate', 'w_up', 'w_down'}
TAGN = 64


def declare(C, name, shape, dt, role):
    kind = {'in': 'ExternalInput', 'out': 'ExternalOutput', 'tmp': 'Internal'}[role]
    if name in TAGGED:
        n = 1
        for d_ in shape:
            n *= d_
        flat = C.nc.dram_tensor(name, [n + TAGN], dt, kind=kind).ap()
        letters = 'abcdefg'[:len(shape)]
        pat = '(%s) -> %s' % (' '.join(letters), ' '.join(letters))
        C.dr[name] = flat[0:n].rearrange(pat, **{letters[i]: shape[i] for i in range(1, len(shape))})
        return
    if role == 'tmp' and name.startswith(('Kg', 'Vg', 'Hg')):
        C.dr[name] = C.nc.dram_tensor(name, list(shape), dt, kind=kind, addr_space='Local').ap()
    else:
        C.dr[name] = C.nc.dram_tensor(name, list(shape), dt, kind=kind).ap()


CONST_IN = [('ident', [128, 128]), ('pmat', [128, 128]), ('cosT', [128, NT]), ('sinT', [128, NT]),
            ('n1gT', [128, 16]), ('n2gT', [128, 16]), ('psT', [128, 8]), ('sel', [128, 8]), ('invcnt', [4, NT])]
PARAM_IN = [('w_in', [2, D, 2048]), ('w_out', [2, D, D]), ('pool_w', [2, 4, 128, 128]), ('subln_g', [2, 128]),
            ('lambda_q1', [2, 64]), ('lambda_k1', [2, 64]), ('lambda_q2', [2, 64]), ('lambda_k2', [2, 64]),
            ('router_coarse_w', [2, D, 4]), ('router_coarse_b', [2, 4]), ('router_fine_w', [2, D, 32]),
            ('router_fine_b', [2, 32]), ('w_gate', [2, 4, 8, D, 256]), ('w_up', [2, 4, 8, D, 256]),
            ('w_down', [2, 4, 8, 256, D]), ('final_g', [D])]


_ALLP = [nm for nm, _ in PARAM_IN]
MODE_PARAMS = {0: set(_ALLP), 1: {'w_in'}, 2: set(_ALLP) - {'final_g'}, 3: set(_ALLP) - {'w_in'}}


def layer_tensors(l):
    t = [('uT%d' % l, [512, NT], F32), ('qT%d' % l, [512, NT], BF16), ('Hx%d' % l, [128, 64], F32),
         ('Kc%d' % l, [512, NCTX], BF16), ('Vc%d' % l, [4 * NCTX, VW], BF16)]
    for s in range(8):
        t.append(('Kx%d_%d' % (l, s), [512, 512], BF16))
        t.append(('Vx%d_%d' % (l, s), [2048, VW], BF16))
    return t


def gathered_tensors(l):
    t = [('Hg%d' % l, [4 * 128, 64], F32)]
    for s in range(8):
        t.append(('Kg%d_%d' % (l, s), [4 * 512, 512], BF16))
        t.append(('Vg%d_%d' % (l, s), [4 * 2048, VW], BF16))
    return t


def build(mode):
    nc = bass.Bass("TRN2", target_bir_lowering=False)
    C = Ctx()
    C.nc = nc
    C.dr = {}
    C.db = {}
    C.fused = mode == 0
    with ExitStack() as stack:
        C.S = Sched(nc, stack)
        for nm, shp in CONST_IN:
            declare(C, nm, shp, F32, 'in')
        for nm, shp in PARAM_IN:
            if nm in MODE_PARAMS[mode]:
                declare(C, nm, shp, F32, 'in')
        if mode in (0, 1):
            declare(C, 'x_in', [NT, D], F32, 'in')
            declare(C, 'sT_in', [128, 16], F32, 'in')
            declare(C, 'ada_w', [2, D, 6 * D], F32, 'in')
            declare(C, 'ada_b', [2, 6 * D], F32, 'in')
        declare(C, 'modrow', [2, 2, 6 * D], F32, {0: 'tmp', 1: 'out', 2: 'in', 3: 'in'}[mode])
        for l in range(2):
            prod = 1 if l == 0 else 2
            cons = prod + 1
            for nm, shp, dt in layer_tensors(l):
                if mode == 0:
                    declare(C, nm, shp, dt, 'tmp')
                elif mode == prod:
                    declare(C, nm, shp, dt, 'out')
                elif mode == cons and not nm.startswith(('Kx', 'Vx', 'Hx')):
                    declare(C, nm, shp, dt, 'in')
            for nm, shp, dt in gathered_tensors(l):
                if mode == 0:
                    declare(C, nm, shp, dt, 'tmp')
                elif mode == cons:
                    declare(C, nm, shp, dt, 'in')
        if mode == 0:
            for nm in ['xa0', 'xm0', 'xa1']:
                declare(C, nm, [NT, D], F32, 'tmp')
            declare(C, 'out', [NLAT, D], F32, 'out')
        elif mode == 2:
            declare(C, 'x_in', [NT, D], F32, 'in')
            declare(C, 'xa0', [NT, D], F32, 'out')
            declare(C, 'xm0', [NT, D], F32, 'out')
        elif mode == 3:
            declare(C, 'xm0', [NT, D], F32, 'in')
            declare(C, 'xa1', [NT, D], F32, 'tmp')
            declare(C, 'out', [NLAT, D], F32, 'out')
        setup_common(C, stack)
        C.n1gT = sb(C, stack, 'n1gT_sb', [128, 16], F32)
        C.n2gT = sb(C, stack, 'n2gT_sb', [128, 16], F32)
        C.S.dma('sp', C.n1gT[:, :], C.dr['n1gT'][:, :], writes=[C.n1gT], sb=C.n1gT)
        C.S.dma('sp', C.n2gT[:, :], C.dr['n2gT'][:, :], writes=[C.n2gT], sb=C.n2gT)
        C.phase_bufs = []
        if mode in (0, 1):
            prologue(C)
            phase_a(C, 0, 'x_in', last=False)
        if mode in (0, 2):
            phase_b(C, 0, 'x_in', last=False)
            phase_c(C, 0, last=False)
            phase_a(C, 1, 'xm0', last=True)
        if mode in (0, 3):
            phase_b(C, 1, 'xm0', last=True)
            phase_c(C, 1, last=True)
        C.S.barrier(['sp'])
        C.S.replay()
    return nc


def rope_tables_T(core):
    qq = core % 4
    tok = np.arange(NLAT) + qq * NLAT
    row = (tok // 64).astype(np.float32)
    col = (tok % 64).astype(np.float32)
    inv = (10000.0 ** (-np.arange(16, dtype=np.float32) / 16)).astype(np.float32)
    cosT = np.ones((64, NT), np.float32)
    sinT = np.zeros((64, NT), np.float32)
    for d in range(64):
        f = d % 16
        pos = row if d < 32 else col
        ang = (pos * inv[f]).astype(np.float32)
        sgn = -1.0 if (d % 32) < 16 else 1.0
        cosT[d, :NLAT] = np.cos(ang)
        sinT[d, :NLAT] = sgn * np.sin(ang)
    return np.tile(cosT, (2, 1)), np.tile(sinT, (2, 1))


def pmat_const():
    p = np.zeros((128, 128), np.float32)
    for i in range(128):
        partner = i + 16 if (i % 32) < 16 else i - 16
        p[partner, i] = 1.0
    return p


def invcnt_const(core):
    qq = core % 4
    out = np.zeros((4, NT), np.float32)
    for g, w in enumerate((2, 4, 8, 16)):
        left = w // 2
        right = w - 1 - left
        t = np.arange(NLAT) + qq * NLAT
        lo = np.clip(t - left, 0, L)
        hi = np.clip(t + right + 1, 0, L)
        out[g, :NLAT] = 1.0 / (hi - lo)
        t = np.arange(NCTX)
        lo = np.clip(t - left, 0, NCTX)
        hi = np.clip(t + right + 1, 0, NCTX)
        out[g, NLAT:] = 1.0 / (hi - lo)
    return out


def const_inputs(core, inp):
    cosT, sinT = rope_tables_T(core)
    qq = core % 4
    sel = np.zeros((128, 8), np.float32)
    if qq > 0:
        sel[:, qq - 1] = 1.0
    if qq < 3:
        sel[:, 4 + qq + 1] = 1.0
    m = {
        'ident': np.eye(128, dtype=np.float32),
        'pmat': pmat_const(),
        'cosT': cosT, 'sinT': sinT,
        'n1gT': np.ascontiguousarray(inp['norm1_g'].reshape(2, 8, 128).transpose(2, 0, 1).reshape(128, 16)),
        'n2gT': np.ascontiguousarray(inp['norm2_g'].reshape(2, 8, 128).transpose(2, 0, 1).reshape(128, 16)),
        'psT': np.ascontiguousarray(inp['pool_scale'].reshape(2, 4, 128).transpose(2, 0, 1).reshape(128, 8)),
        'sel': sel,
        'invcnt': invcnt_const(core),
    }
    return m


def first_inputs(core, inp):
    b, qq = core // 4, core % 4
    m = {}
    m['x_in'] = np.ascontiguousarray(np.concatenate([inp['x'][b, qq * NLAT:(qq + 1) * NLAT], inp['ctx'][b]], 0))
    s = np.stack([inp['c'][b], inp['c_ctx']], -1)
    m['sT_in'] = np.ascontiguousarray(s.reshape(8, 128, 2).transpose(1, 0, 2).reshape(128, 16))
    m['ada_w'] = tagged(inp['ada_w'], core)
    m['ada_b'] = inp['ada_b']
    return m


def gather_host(results, l):
    outs = []
    for c in range(NCORES):
        grp = [4 * (c // 4) + r for r in range(4)]
        m = {'Hg%d' % l: np.concatenate([results[r]['Hx%d' % l] for r in grp], 0)}
        for s in range(8):
            m['Kg%d_%d' % (l, s)] = np.concatenate([results[r]['Kx%d_%d' % (l, s)] for r in grp], 0)
            m['Vg%d_%d' % (l, s)] = np.concatenate([results[r]['Vx%d_%d' % (l, s)] for r in grp], 0)
        outs.append(m)
    return outs


_NC_CACHE = {}


def get_nc(mode):
    if mode not in _NC_CACHE:
        _NC_CACHE[mode] = build(mode)
    return _NC_CACHE[mode]


def tagged(a, core):
    return np.concatenate([np.asarray(a, np.float32).ravel(), np.full(TAGN, float(core), np.float32)])


def params_for(mode, inp, core=0):
    return {nm: (tagged(inp[nm], core) if nm in TAGGED else inp[nm]) for nm in _ALLP if nm in MODE_PARAMS[mode]}


def run_multi(inp, cores=None):
    consts = [const_inputs(c, inp) for c in range(NCORES)]
    ids = list(range(NCORES)) if cores is None else cores
    in1 = [dict(consts[c], **first_inputs(c, inp), **params_for(1, inp, c)) for c in ids]
    r1 = run_bass_kernel_spmd(get_nc(1), in1, core_ids=ids).results
    g0 = gather_host(r1, 0)
    in2 = []
    for c in ids:
        m = dict(consts[c], **params_for(2, inp, c))
        m['x_in'] = in1[c]['x_in']
        m['modrow'] = r1[c]['modrow']
        for nm in ['uT0', 'qT0', 'Kc0', 'Vc0']:
            m[nm] = r1[c][nm]
        m.update(g0[c])
        in2.append(m)
    r2 = run_bass_kernel_spmd(get_nc(2), in2, core_ids=ids).results
    g1 = gather_host(r2, 1)
    in3 = []
    for c in ids:
        m = dict(consts[c], **params_for(3, inp, c))
        m['modrow'] = r1[c]['modrow']
        m['xm0'] = r2[c]['xm0']
        for nm in ['uT1', 'qT1', 'Kc1', 'Vc1']:
            m[nm] = r2[c][nm]
        m.update(g1[c])
        in3.append(m)
    r3 = run_bass_kernel_spmd(get_nc(3), in3, core_ids=ids).results
    return r3


def run_fused(inp):
    ids = list(range(NCORES))
    ins = [dict(const_inputs(c, inp), **first_inputs(c, inp), **params_for(0, inp, c)) for c in ids]
    return run_bass_kernel_spmd(get_nc(0), ins, core_ids=ids).results


FUSED = True


def kernel(**inputs):
    inp = {k: np.ascontiguousarray(np.asarray(v, dtype=np.float32)) for k, v in inputs.items()}
    res = run_fused(inp) if FUSED else run_multi(inp)
    out = np.zeros((2, L, D), np.float32)
    for c in range(NCORES):
        b, qq = c // 4, c % 4
        out[b, qq * NLAT:(qq + 1) * NLAT] = np.asarray(res[c]['out'], dtype=np.float32)
    return out
```
